# Optimizing a Trainium2 kernel written in Bass

```python
import math
import jax, jax.numpy as jnp
from jax import lax
import numpy as np

D_MODEL = 2048
BATCH = 4
SEQ = 2048
DEPTH = 2

N_MIXERS = 2
ATT_HEADS = 16
ATT_KV_HEADS = 4
ATT_HEAD_DIM = D_MODEL // ATT_HEADS
IDX_HEADS = 16
IDX_HEAD_DIM = 64
TOPK_MAX = 256
Q_BLOCK = 128
ML_HEADS = 8
ML_V_DIM = D_MODEL // ML_HEADS
ML_QK_DIM = ML_V_DIM // 2
ML_CHUNK = 64
GATE_SOFTCAP = 15.0
D_FF = 4 * D_MODEL
ROPE_THETA = 500000.0
ROT_FRAC = 4
EPS = 1e-6

N_ATT_LAYERS = (DEPTH + 1) // 2
N_ML_LAYERS = DEPTH // 2

ATT_SIZES = [ATT_HEADS * ATT_HEAD_DIM, ATT_KV_HEADS * ATT_HEAD_DIM, ATT_KV_HEADS * ATT_HEAD_DIM,
             IDX_HEADS * IDX_HEAD_DIM, IDX_HEAD_DIM, IDX_HEADS]
ATT_IN = sum(ATT_SIZES)
ML_SIZES = [ML_HEADS * ML_QK_DIM, ML_HEADS * ML_QK_DIM, ML_HEADS * ML_V_DIM, D_MODEL, ML_HEADS, ML_HEADS]
ML_IN = sum(ML_SIZES)

kernel_name = "hybrid_dsa_mlstm_trunk"


def _split_points(sizes):
    return np.cumsum(sizes)[:-1].tolist()


def rms_norm(x, g):
    xf = x.astype(jnp.float32)
    y = xf * lax.rsqrt(jnp.mean(xf * xf, axis=-1, keepdims=True) + EPS)
    return (y * g.astype(jnp.float32)).astype(x.dtype)


def rope_partial(x, pos):
    rot = x.shape[-1] // ROT_FRAC
    half = rot // 2
    inv_freq = ROPE_THETA ** (-2.0 * jnp.arange(half, dtype=jnp.float32) / rot)
    ang = pos.astype(jnp.float32)[:, None] * inv_freq[None, :]
    cos = jnp.cos(ang)[:, None, :]
    sin = jnp.sin(ang)[:, None, :]
    xf = x.astype(jnp.float32)
    x1 = xf[..., :half]
    x2 = xf[..., half:rot]
    out = jnp.concatenate([x1 * cos - x2 * sin, x1 * sin + x2 * cos, xf[..., rot:]], axis=-1)
    return out.astype(x.dtype)


def dsa_attention(h, w_in, q_gain, k_gain, w_out):
    B, T, _ = h.shape
    H, G, Dh = ATT_HEADS, ATT_KV_HEADS, ATT_HEAD_DIM
    R = H // G
    Hi, Di = IDX_HEADS, IDX_HEAD_DIM
    proj = h @ w_in
    q, k, v, qi, ki, wi = jnp.split(proj, _split_points(ATT_SIZES), axis=-1)
    pos = jnp.arange(T)
    q = rope_partial(rms_norm(q.reshape(B, T, H, Dh), q_gain), pos)
    k = rope_partial(rms_norm(k.reshape(B, T, G, Dh), k_gain), pos)
    v = v.reshape(B, T, G, Dh)
    qi = rope_partial(qi.reshape(B, T, Hi, Di), pos)
    ki = rope_partial(ki.reshape(B, T, 1, Di), pos)[:, :, 0].astype(jnp.float32)
    wi = wi.astype(jnp.float32) * (Hi ** -0.5 * Di ** -0.5)
    topk = min(TOPK_MAX, T // 4)
    nb = T // Q_BLOCK

    def to_blocks(a):
        return jnp.moveaxis(a.reshape((B, nb, Q_BLOCK) + a.shape[2:]), 1, 0)

    key_pos = jnp.arange(T)

    def block_fn(args):
        q_b, qi_b, wi_b, t0 = args
        tpos = t0 + jnp.arange(Q_BLOCK)
        dots = jnp.einsum('bthd,bsd->bths', qi_b.astype(jnp.float32), ki)
        score = jnp.einsum('bths,bth->bts', jax.nn.relu(dots), wi_b)
        causal = key_pos[None, :] <= tpos[:, None]
        score = jnp.where(causal[None], score, -jnp.inf)
        _, idx = lax.top_k(score, topk)
        valid = idx <= tpos[None, :, None]
        k_sel = jax.vmap(lambda kb, ib: kb[ib])(k, idx).astype(jnp.float32)
        v_sel = jax.vmap(lambda vb, ib: vb[ib])(v, idx).astype(jnp.float32)
        qg = q_b.reshape(B, Q_BLOCK, G, R, Dh).astype(jnp.float32)
        logits = jnp.einsum('btgrd,btkgd->btgrk', qg, k_sel) * (Dh ** -0.5)
        logits = jnp.where(valid[:, :, None, None, :], logits, -jnp.inf)
        p = jax.nn.softmax(logits, axis=-1)
        o = jnp.einsum('btgrk,btkgd->btgrd', p, v_sel)
        return o.reshape(B, Q_BLOCK, H * Dh).astype(h.dtype)

    starts = jnp.arange(nb, dtype=jnp.int32) * Q_BLOCK
    out = lax.map(block_fn, (to_blocks(q), to_blocks(qi), to_blocks(wi), starts))
    out = jnp.moveaxis(out, 0, 1).reshape(B, T, H * Dh)
    return out @ w_out


def mlstm_chunkwise(q, k, v, ig, logf):
    B, H, T, dk = q.shape
    dv = v.shape[-1]
    L = ML_CHUNK
    NC = T // L
    q = q.reshape(B, H, NC, L, dk)
    k = k.reshape(B, H, NC, L, dk)
    v = v.reshape(B, H, NC, L, dv)
    ig = ig.reshape(B, H, NC, L)
    b = jnp.cumsum(logf.reshape(B, H, NC, L), axis=-1)
    a = b[..., -1]
    g = a[..., None] - b + ig
    m_loc = jnp.max(g, axis=-1)
    wgt = jnp.exp(g - m_loc[..., None])
    C_loc = jnp.einsum('bhcl,bhcld,bhcle->bhcde', wgt, k, v)
    n_loc = jnp.einsum('bhcl,bhcld->bhcd', wgt, k)

    def step(carry, xs):
        C, n, m = carry
        a_c, m_c, C_c, n_c = xs
        m_new = jnp.maximum(a_c + m, m_c)
        s_old = jnp.exp(a_c + m - m_new)
        s_new = jnp.exp(m_c - m_new)
        C2 = s_old[..., None, None] * C + s_new[..., None, None] * C_c
        n2 = s_old[..., None] * n + s_new[..., None] * n_c
        return (C2, n2, m_new), (C, n, m)

    init = (jnp.zeros((B, H, dk, dv), jnp.float32), jnp.zeros((B, H, dk), jnp.float32),
            jnp.zeros((B, H), jnp.float32))
    xs = (jnp.moveaxis(a, 2, 0), jnp.moveaxis(m_loc, 2, 0), jnp.moveaxis(C_loc, 2, 0), jnp.moveaxis(n_loc, 2, 0))
    _, (C_st, n_st, m_st) = lax.scan(step, init, xs)
    C_st = jnp.moveaxis(C_st, 0, 2)
    n_st = jnp.moveaxis(n_st, 0, 2)
    m_st = jnp.moveaxis(m_st, 0, 2)

    causal = jnp.tril(jnp.ones((L, L), dtype=bool))
    Dm = b[..., :, None] - b[..., None, :] + ig[..., None, :]
    Dm = jnp.where(causal, Dm, -jnp.inf)
    inter = b + m_st[..., None]
    m_t = jnp.maximum(inter, jnp.max(Dm, axis=-1))
    S = jnp.einsum('bhcld,bhcsd->bhcls', q, k) * jnp.exp(Dm - m_t[..., None])
    s_inter = jnp.exp(inter - m_t)
    num = jnp.einsum('bhcls,bhcse->bhcle', S, v) + s_inter[..., None] * jnp.einsum('bhcld,bhcde->bhcle', q, C_st)
    den = jnp.sum(S, axis=-1) + s_inter * jnp.einsum('bhcld,bhcd->bhcl', q, n_st)
    h_t = num / jnp.maximum(jnp.abs(den), jnp.exp(-m_t))[..., None]
    return h_t.reshape(B, H, T, dv)


def mlstm_mixer(h, w_in, b_gate, h_gain, w_out):
    B, T, _ = h.shape
    Hm, dk, dv = ML_HEADS, ML_QK_DIM, ML_V_DIM
    proj = h @ w_in
    q, k, v, o, ig, fg = jnp.split(proj, _split_points(ML_SIZES), axis=-1)
    q = q.reshape(B, T, Hm, dk).transpose(0, 2, 1, 3).astype(jnp.float32)
    k = k.reshape(B, T, Hm, dk).transpose(0, 2, 1, 3).astype(jnp.float32) * (dk ** -0.5)
    v = v.reshape(B, T, Hm, dv).transpose(0, 2, 1, 3).astype(jnp.float32)
    gates = jnp.concatenate([ig, fg], axis=-1).astype(jnp.float32) + b_gate.astype(jnp.float32)
    gates = GATE_SOFTCAP * jnp.tanh(gates / GATE_SOFTCAP)
    ig_pre = gates[..., :Hm].transpose(0, 2, 1)
    logf = jax.nn.log_sigmoid(gates[..., Hm:]).transpose(0, 2, 1)
    h_t = mlstm_chunkwise(q, k, v, ig_pre, logf)
    h_t = rms_norm(h_t, h_gain)
    h_t = h_t.transpose(0, 2, 1, 3).reshape(B, T, Hm * dv).astype(h.dtype)
    return (jax.nn.sigmoid(o) * h_t) @ w_out


def setup_inputs(seed: int = 0) -> dict:
    key = jax.random.key(seed)
    ks = jax.random.split(key, 16)

    def nrm(k, shape, scale):
        return jax.random.normal(k, shape, jnp.float32) * scale

    Hm = ML_HEADS
    b_in = nrm(ks[9], (N_ML_LAYERS, Hm), 0.1)
    b_f = 3.0 + nrm(ks[10], (N_ML_LAYERS, Hm), 0.1)
    return {
        "x": nrm(ks[0], (BATCH, SEQ, D_MODEL), 1.0),
        "norm_mix": 1.0 + nrm(ks[1], (DEPTH, D_MODEL), 0.1),
        "norm_ffn": 1.0 + nrm(ks[2], (DEPTH, D_MODEL), 0.1),
        "att_w_in": nrm(ks[3], (N_ATT_LAYERS, D_MODEL, ATT_IN), D_MODEL ** -0.5),
        "att_q_gain": 1.0 + nrm(ks[4], (N_ATT_LAYERS, ATT_HEAD_DIM), 0.1),
        "att_k_gain": 1.0 + nrm(ks[5], (N_ATT_LAYERS, ATT_HEAD_DIM), 0.1),
        "att_w_out": nrm(ks[6], (N_ATT_LAYERS, ATT_HEADS * ATT_HEAD_DIM, D_MODEL), (ATT_HEADS * ATT_HEAD_DIM) ** -0.5),
        "ml_w_in": nrm(ks[7], (N_ML_LAYERS, D_MODEL, ML_IN), D_MODEL ** -0.5),
        "ml_b_gate": jnp.concatenate([b_in, b_f], axis=-1),
        "ml_h_gain": 1.0 + nrm(ks[11], (N_ML_LAYERS, ML_V_DIM), 0.1),
        "ml_w_out": nrm(ks[12], (N_ML_LAYERS, ML_HEADS * ML_V_DIM, D_MODEL), (ML_HEADS * ML_V_DIM) ** -0.5),
        "ffn_w_up": nrm(ks[13], (DEPTH, D_MODEL, D_FF), D_MODEL ** -0.5),
        "ffn_w_down": nrm(ks[14], (DEPTH, D_FF, D_MODEL), D_FF ** -0.5),
    }


def reference(x, norm_mix, norm_ffn, att_w_in, att_q_gain, att_k_gain, att_w_out,
              ml_w_in, ml_b_gate, ml_h_gain, ml_w_out, ffn_w_up, ffn_w_down):
    h = x
    for i in range(DEPTH):
        j = i // N_MIXERS
        hn = rms_norm(h, norm_mix[i])
        if i % N_MIXERS == 0:
            y = dsa_attention(hn, att_w_in[j], att_q_gain[j], att_k_gain[j], att_w_out[j])
        else:
            y = mlstm_mixer(hn, ml_w_in[j], ml_b_gate[j], ml_h_gain[j], ml_w_out[j])
        h = h + y
        hn = rms_norm(h, norm_ffn[i])
        h = h + jnp.square(jax.nn.relu(hn @ ffn_w_up[i])) @ ffn_w_down[i]
    return h
```

```python
import sys, time
import numpy as np
from contextlib import ExitStack
import ml_dtypes
import concourse.bass as bass
import concourse.mybir as mybir
from concourse.bass_utils import run_bass_kernel_spmd


F32 = mybir.dt.float32
BF16 = mybir.dt.bfloat16
ALU = mybir.AluOpType
AF = mybir.ActivationFunctionType
AX = mybir.AxisListType

ENGS = ["tensor", "vector", "scalar", "gpsimd", "sync"]


class T:
    __slots__ = ("h", "name")

    def __init__(self, h, name):
        self.h = h
        self.name = name

    def __getitem__(self, k):
        return self.h[k]


class Op:
    __slots__ = ("eng", "fn", "deps", "odeps", "sig", "sem", "val", "dma", "idx", "cc", "region", "cost", "users", "tail", "nwait", "rt", "fin", "st", "prevdma")

    def __init__(self, eng, fn, dma):
        self.cc = False
        self.eng = eng
        self.fn = fn
        self.deps = set()
        self.odeps = set()
        self.sig = False
        self.sem = None
        self.val = 0
        self.dma = dma
        self.prevdma = None


DEF_COST = {"tensor": 0.25, "vector": 0.7, "scalar": 0.7, "gpsimd": 1.0, "sync": 0.1}
DMA_ISSUE = {"sync": 0.15, "gpsimd": 1.2}
DMA_LAT = 5.0
SYNC_LAT = 1.2


class Prog:
    def __init__(self, nc, stack, n_dma_sems=24, schedule=True):
        self.nc = nc
        self.stack = stack
        self.schedule = schedule
        self.regions = [[]]
        self.region_sched = [schedule]
        self.last_w = {}
        self.readers = {}
        self.engsem = {e: stack.enter_context(nc.semaphore("s_" + e)) for e in ENGS}
        self.dma_pool = {}
        for q in ("sync", "gpsimd"):
            self.dma_pool[q] = [stack.enter_context(nc.semaphore("d_%s%d" % (q, i))) for i in range(n_dma_sems)]
        self.n = 0
        self.final_wait = None

    def add(self, eng, fn, reads=(), writes=(), dma=False, cc=False, cost=None):
        op = Op(eng, fn, dma or cc)
        op.cc = cc
        op.idx = self.n
        self.n += 1
        op.region = len(self.regions) - 1
        op.cost = cost if cost is not None else DEF_COST[eng]
        deps = set()
        for r in reads:
            w = self.last_w.get(r)
            if w is not None:
                deps.add(w)
        for w_ in writes:
            lw = self.last_w.get(w_)
            if lw is not None:
                deps.add(lw)
            for rd in self.readers.get(w_, ()):
                deps.add(rd)
        if eng == "tensor" and not dma:
            for d_ in deps:
                if d_.eng == "tensor" and not d_.dma:
                    op.odeps.add(d_)
                else:
                    op.deps.add(d_)
        else:
            op.deps = deps
        if cc:
            op.sem = self.stack.enter_context(self.nc.semaphore("cc%d" % op.idx))
            op.val = 1
        for r in reads:
            self.readers.setdefault(r, []).append(op)
        for w_ in writes:
            self.last_w[w_] = op
            self.readers[w_] = []
        self.regions[-1].append(op)
        return op

    def barrier(self, schedule=None):
        self.regions.append([])
        self.region_sched.append(self.schedule if schedule is None else schedule)
        self.last_w = {}
        self.readers = {}

    def wait_all_dma(self, eng="sync"):
        self.final_wait = eng

    def _schedule(self, ops, do_sched):
        import heapq
        order = {e: [] for e in ENGS}
        if not do_sched:
            for op in ops:
                order[op.eng].append(op)
            return order
        for op in ops:
            op.users = []
            op.nwait = len(op.deps) + len(op.odeps)
            op.rt = 0.0
        for op in ops:
            for d_ in op.deps:
                d_.users.append((op, True))
            for d_ in op.odeps:
                d_.users.append((op, False))
        for op in reversed(ops):
            t = 0.0
            for u, sy in op.users:
                t = max(t, u.tail + (SYNC_LAT if sy else 0.0))
            op.tail = t + (DMA_LAT if op.dma else op.cost)
        pend = {e: [] for e in ENGS}
        avail = {e: [] for e in ENGS}
        tfree = {e: 0.0 for e in ENGS}
        for op in ops:
            if op.nwait == 0:
                heapq.heappush(pend[op.eng], (0.0, op.idx, op))
        left = len(ops)
        while left:
            best = None
            for e in ENGS:
                pe, av = pend[e], avail[e]
                t = tfree[e]
                while pe and pe[0][0] <= t:
                    _, _, o = heapq.heappop(pe)
                    heapq.heappush(av, (-o.tail, o.idx, o))
                if av:
                    cand = (t, av[0][0], e, True)
                elif pe:
                    cand = (pe[0][0], -pe[0][2].tail, e, False)
                else:
                    continue
                if best is None or cand[:2] < best[:2]:
                    best = cand
            start, _, e, from_av = best
            if from_av:
                _, _, op = heapq.heappop(avail[e])
            else:
                _, _, op = heapq.heappop(pend[e])
            order[e].append(op)
            left -= 1
            if op.dma:
                busy = DMA_ISSUE.get(e, 0.2)
                fin = start + busy + DMA_LAT
            else:
                busy = op.cost
                fin = start + busy
            tfree[e] = start + busy
            for u, sy in op.users:
                r = fin + SYNC_LAT if sy else start
                if r > u.rt:
                    u.rt = r
                u.nwait -= 1
                if u.nwait == 0:
                    heapq.heappush(pend[u.eng], (u.rt, u.idx, u))
        return order

    def emit(self):
        nc = self.nc
        queues = {e: [] for e in ENGS}
        bar_deps = []
        for ri, ops in enumerate(self.regions):
            order = self._schedule(ops, self.region_sched[ri])
            lasts = set()
            for e in ENGS:
                if order[e]:
                    for op in reversed(order[e]):
                        if not op.dma and op.fn is not None:
                            lasts.add(op)
                            break
                for op in order[e]:
                    if op.dma:
                        lasts.add(op)
            bar_deps.append(lasts)
            for e in ENGS:
                first = True
                for op in order[e]:
                    if first and len(bar_deps) > 1:
                        op.deps = set(op.deps) | bar_deps[-2]
                    first = False
                    queues[e].append(op)
        if self.final_wait is not None:
            op = Op(self.final_wait, None, False)
            op.deps = set(o for r in self.regions for o in r if o.dma)
            queues[self.final_wait].append(op)
        for q, pool in self.dma_pool.items():
            k = 0
            hist = []
            for op in queues[q]:
                if op.dma and not op.cc:
                    op.sem = pool[k % len(pool)]
                    op.val = 16 * (k // len(pool) + 1)
                    if k >= len(pool):
                        op.prevdma = hist[k - len(pool)]
                    hist.append(op)
                    k += 1
        pos = {}
        for e in ENGS:
            for i, op in enumerate(queues[e]):
                pos[op] = i
        for e in ENGS:
            for op in queues[e]:
                best = {}
                nd = set()
                for d in op.deps:
                    if d.dma:
                        nd.add(d)
                    else:
                        b_ = best.get(d.eng)
                        if b_ is None or pos[d] > pos[b_]:
                            best[d.eng] = d
                for d in best.values():
                    d.sig = True
                    nd.add(d)
                op.deps = nd
        for e in ENGS:
            cnt = 0
            for op in queues[e]:
                if op.dma:
                    continue
                if op.sig:
                    cnt += 1
                    op.sem = self.engsem[e]
                    op.val = cnt
        self.queues = queues
        with nc.Block() as block:
            def mk(e):
                def body(eng):
                    waited = {}
                    for op in queues[e]:
                        need = {}
                        for d in op.deps:
                            if need.get(d.sem, 0) < d.val:
                                need[d.sem] = d.val
                        if op.prevdma is not None:
                            p_ = op.prevdma
                            if need.get(p_.sem, 0) < p_.val:
                                need[p_.sem] = p_.val
                        for s, v in need.items():
                            if waited.get(s, 0) < v:
                                eng.wait_ge(s, v)
                                waited[s] = v
                        if op.fn is None:
                            continue
                        ins = op.fn(eng)
                        if op.cc:
                            ins.then_inc(op.sem, 1)
                        elif op.dma:
                            ins.then_inc(op.sem, 16)
                        elif op.sig:
                            ins.then_inc(op.sem, 1)
                return body
            block.tensor(mk("tensor"))
            block.vector(mk("vector"))
            block.scalar(mk("scalar"))
            block.gpsimd(mk("gpsimd"))
            block.sync(mk("sync"))


SBW = 52000


class Arena:
    def __init__(self, nc, st):
        self.sb_t = st.enter_context(nc.sbuf_tensor("arena_sb", [128, SBW], F32))
        self.ps_t = st.enter_context(nc.psum_tensor("arena_ps", [128, 4096], F32))
        self.reset()

    def reset(self):
        self.off = 0
        self.pbank = 0

    @staticmethod
    def _shape(v, shape):
        if len(shape) == 2:
            return v
        if len(shape) == 3:
            return v.rearrange("p (a b) -> p a b", a=shape[1])
        if len(shape) == 4:
            return v.rearrange("p (a b c) -> p a b c", a=shape[1], b=shape[2])
        raise ValueError(shape)

    def sb(self, name, shape, dt):
        p = shape[0]
        n = int(np.prod(shape[1:]))
        words = n if dt == F32 else (n + 1) // 2
        words = (words + 7) // 8 * 8
        assert self.off + words <= SBW, ("SBUF arena overflow", name, self.off, words)
        base = self.sb_t[0:p, self.off:self.off + words]
        self.off += words
        v = base if dt == F32 else base.bitcast(BF16)
        return T(self._shape(v[:, 0:n], shape), name)

    def ps(self, name, shape, dt):
        p = shape[0]
        n = int(np.prod(shape[1:]))
        nbytes = n * (4 if dt == F32 else 2)
        banks = (nbytes + 2047) // 2048
        assert self.pbank + banks <= 8, ("PSUM arena overflow", name)
        base = self.ps_t[0:p, self.pbank * 512:(self.pbank + banks) * 512]
        self.pbank += banks
        v = base if dt == F32 else base.bitcast(BF16)
        return T(self._shape(v[:, 0:n], shape), name)


D = 2048
EPS = 1e-6
NEG = -1.0e30
REPL = -3.0e38


def merge_gens(gens):
    gens = [g for g in gens if g is not None]
    while gens:
        for g in list(gens):
            try:
                next(g)
            except StopIteration:
                gens.remove(g)


def build_att(P, nc, sb, ps, d):
    def V(fn, r, w):
        return P.add("vector", fn, reads=r, writes=w)

    def A(fn, r, w):
        return P.add("scalar", fn, reads=r, writes=w)

    def G(fn, r, w):
        return P.add("gpsimd", fn, reads=r, writes=w)

    def TE(fn, r, w):
        return P.add("tensor", fn, reads=r, writes=w)

    def LD(t, src, q="sync", key=None):
        return P.add(q, lambda e: e.dma_start(out=t[:] if key is None else key[1], in_=src), writes=[t if key is None else key[0]], dma=True)

    ident = sb("ident", [128, 128], BF16)
    LD(ident, d["ident"])
    epsT = sb("epsT", [128, 1], F32)
    V(lambda e: e.memset(epsT[:], EPS), [], [epsT])
    g_bc = sb("g_bc", [128, D], F32)
    LD(g_bc, d["gmix"].partition_broadcast(128))
    gq_bc = sb("gq_bc", [128, 128], F32)
    LD(gq_bc, d["gq"].partition_broadcast(128))
    gk_bc = sb("gk_bc", [128, 128], F32)
    LD(gk_bc, d["gk"].partition_broadcast(128))
    tabs = {}
    for nm, shp in (("cos_k", [128, 16, 16]), ("sin_k", [128, 16, 16]), ("cos_ki", [128, 16, 8]), ("sin_ki", [128, 16, 8]),
                    ("cos_q", [128, 8, 16]), ("sin_q", [128, 8, 16]), ("cos_qi", [128, 8, 8]), ("sin_qi", [128, 8, 8])):
        tabs[nm] = sb(nm, shp, F32)
        LD(tabs[nm], d[nm])
    cbias = sb("cbias", [128, 256], F32)
    LD(cbias, d["cbias"])

    NW = 2
    wt = [sb("wt%d" % i, [128, 16, 512], BF16) for i in range(NW)]
    wki = sb("wki", [128, 16, 80], BF16)
    xt = [sb("xt%d" % i, [128, D], F32) for i in range(2)]
    hb = [sb("hb%d" % i, [128, D], BF16) for i in range(2)]
    ss = [sb("ss%d" % i, [128, 1], F32) for i in range(2)]
    rstd = [sb("rstd%d" % i, [128, 1], F32) for i in range(2)]
    hnT = sb("hnT", [128, 16, 512], BF16)
    kT = sb("kT", [128, 4, 2048], BF16)
    Vt = sb("Vt", [128, 16, 4, 128], BF16)
    ones_bf = sb("ones_bf", [128, 128], BF16)
    rsum = sb("rsum", [128, 512], F32)
    kiT = sb("kiT", [128, 2048], BF16)
    qT = sb("qT", [128, 16, 512], BF16)
    qiT = sb("qiT", [128, 8, 512], BF16)
    wi_sb = sb("wi_sb", [128, 4, 16], F32)
    score = sb("score", [128, 2048], F32)
    work = sb("work", [128, 2048], F32)
    mask = sb("mask", [128, 2048], BF16)
    maskT = sb("maskT", [128, 16, 128], BF16)
    scoreb = [score, work]
    bis = [[sb("bis%d_%d" % (k, j), [128, 1], F32) for j in range(6)] for k in range(2)]
    stepsb = [sb("steps%d" % k, [128, 24], F32) for k in range(2)]
    ckrow = sb("ckrow", [128, 24], F32)
    for k_ in range(24):
        V(lambda e, k_=k_: e.memset(ckrow[:, k_:k_ + 1], 2.0 ** -(k_ + 1)), [], [ckrow])
    tmpA = [sb("tmpA%d" % i, [128, 512], F32) for i in range(2)]
    sqt = sb("sqt", [128, 512], F32)
    osb = sqt
    st1 = sb("st1", [128, 8], F32)
    st2 = sb("st2", [128, 8], F32)
    rtmp = [sb("rtmp%d" % i, [128, 64], F32) for i in range(4)]
    ob = [sb("ob%d" % i, [128, 512], BF16) for i in range(2)]
    relu_t = [sb("relu%d" % i, [128, 512], F32) for i in range(2)]
    pT = [sb("pT%d" % i, [128, 512], BF16) for i in range(2)]
    pTm = [sb("pTm%d" % i, [128, 512], BF16) for i in range(2)]
    ot = [sb("ot%d" % i, [128, 512], F32) for i in range(2)]
    xres = [sb("xres%d" % i, [128, 512], F32) for i in range(2)]
    ps_tr = [ps("tr%d" % i, [128, 4, 128], BF16) for i in range(2)]
    ps_mm = [ps("mm%d" % i, [128, 512], F32) for i in range(2)]
    ps_s = [ps("s%d" % i, [128, 512], F32) for i in range(2)]
    ps_oa = ps("oa", [128, 512], F32)
    ps_os = ps("os", [128, 512], F32)

    cnt = {"w": 0, "x": 0, "tr": 0, "mm": 0, "tA": 0, "ob": 0, "relu": 0, "p": 0, "s": 0, "ot": 0}

    def nxt(k, n=2):
        v = cnt[k] % n
        cnt[k] += 1
        return v

    def load_w(dram, r0, c0, ncols=512, dst=None):
        w = dst if dst is not None else wt[nxt("w", NW)]
        for q in range(4):
            src = dram[r0 + q * 512:r0 + (q + 1) * 512, c0:c0 + ncols].rearrange("(c p) n -> p c n", p=128)
            P.add("gpsimd", lambda e, w=w, q=q, src=src: e.dma_start(out=w[:, q * 4:(q + 1) * 4, 0:ncols], in_=src),
                  writes=[(w, q)], dma=True)
        return w

    def wres(w):
        return [(w, q) for q in range(4)]

    def norm_tile(src_ap):
        k = nxt("x")
        x_, h_, s_, r_ = xt[k], hb[k], ss[k], rstd[k]
        P.add("sync", lambda e: e.dma_start(out=x_[:], in_=src_ap), writes=[x_], dma=True)
        A(lambda e: e.activation(out=h_[:], in_=x_[:], func=AF.Square, accum_out=s_[:]), [x_], [h_, s_])
        A(lambda e: e.activation(out=s_[:], in_=s_[:], func=AF.Sqrt, bias=epsT[:], scale=1.0 / D), [s_, epsT], [s_])
        V(lambda e: e.reciprocal(out=r_[:], in_=s_[:]), [s_], [r_])
        V(lambda e: e.scalar_tensor_tensor(out=h_[:], in0=x_[:], scalar=r_[:], in1=g_bc[:], op0=ALU.mult, op1=ALU.mult),
          [x_, r_, g_bc], [h_])
        return h_

    def tr16(h_, dstT, col0, dkey):
        for j in range(4):
            pt = ps_tr[nxt("tr")]
            for k in range(4):
                dc = j * 4 + k
                TE(lambda e, pt=pt, k=k, dc=dc: e.transpose(out=pt[:, k, :], in_=h_[:, dc * 128:(dc + 1) * 128], identity=ident[:]),
                   [h_, ident], [pt])
            if j % 2 == 0:
                A(lambda e, pt=pt, j=j: e.copy(out=dstT[:, j * 4:(j + 1) * 4, col0:col0 + 128], in_=pt[:]), [pt], [(dstT, dkey, j)])
            else:
                V(lambda e, pt=pt, j=j: e.tensor_copy(out=dstT[:, j * 4:(j + 1) * 4, col0:col0 + 128], in_=pt[:]), [pt], [(dstT, dkey, j)])

    def tres(dstT, dkey):
        return [(dstT, dkey, j) for j in range(4)]

    def proj(pm, ncols, i, w, c_lo):
        for dc in range(16):
            TE(lambda e, dc=dc: e.matmul(pm[:, 0:ncols], lhsT=hnT[:, dc, i * 128:(i + 1) * 128], rhs=w[:, dc, c_lo:c_lo + ncols],
                                         start=(dc == 0), stop=(dc == 15)),
               tres(hnT, i) + [(w, dc // 4)], [pm])

    def normrope(pm, nh, hd, hf, gain, cos_ap, sin_ap, tabres, out, dup=False):
        n = nh * hd
        tA = tmpA[nxt("tA")]
        tA3 = tA[:, 0:n].rearrange("p (a b) -> p a b", a=nh)
        A(lambda e: e.copy(out=tA[:, 0:n], in_=pm[:, 0:n]), [pm], [tA])
        if gain is not None:
            V(lambda e: e.tensor_tensor(out=sqt[:, 0:n], in0=tA[:, 0:n], in1=tA[:, 0:n], op=ALU.mult), [tA], [sqt])
            V(lambda e: e.reduce_sum(out=st1[:, 0:nh], in_=sqt[:, 0:n].rearrange("p (a b) -> p a b", a=nh), axis=AX.X), [sqt], [st1])
            A(lambda e: e.activation(out=st1[:, 0:nh], in_=st1[:, 0:nh], func=AF.Sqrt, bias=epsT[:], scale=1.0 / hd), [st1, epsT], [st1])
            V(lambda e: e.reciprocal(out=st2[:, 0:nh], in_=st1[:, 0:nh]), [st1], [st2])
            V(lambda e: e.tensor_tensor(out=tA3, in0=tA3, in1=st2[:, 0:nh].unsqueeze(2).to_broadcast([128, nh, hd]), op=ALU.mult), [tA, st2], [tA])
            V(lambda e: e.tensor_tensor(out=tA3, in0=tA3, in1=gain[:, 0:hd].unsqueeze(1).to_broadcast([128, nh, hd]), op=ALU.mult), [tA, gain], [tA])
        x1 = tA3[:, :, 0:hf]
        x2 = tA3[:, :, hf:2 * hf]
        cb = cos_ap.unsqueeze(1).to_broadcast([128, nh, hf])
        sn = sin_ap.unsqueeze(1).to_broadcast([128, nh, hf])
        r3 = [r[:, 0:nh * hf].rearrange("p (a b) -> p a b", a=nh) for r in rtmp]
        G(lambda e: e.tensor_tensor(out=r3[0], in0=x1, in1=cb, op=ALU.mult), [tA] + tabres, [rtmp[0]])
        G(lambda e: e.tensor_tensor(out=r3[1], in0=x2, in1=sn, op=ALU.mult), [tA] + tabres, [rtmp[1]])
        G(lambda e: e.tensor_tensor(out=r3[2], in0=x1, in1=sn, op=ALU.mult), [tA] + tabres, [rtmp[2]])
        G(lambda e: e.tensor_tensor(out=r3[3], in0=x2, in1=cb, op=ALU.mult), [tA] + tabres, [rtmp[3]])
        reps = 2 if dup else 1
        for rp in range(reps):
            o3 = out[:, rp * n:(rp + 1) * n].rearrange("p (a b) -> p a b", a=nh)
            G(lambda e, o3=o3: e.tensor_tensor(out=o3[:, :, 0:hf], in0=r3[0], in1=r3[1], op=ALU.subtract), [rtmp[0], rtmp[1]], [(out, "a", rp)])
            G(lambda e, o3=o3: e.tensor_tensor(out=o3[:, :, hf:2 * hf], in0=r3[2], in1=r3[3], op=ALU.add), [rtmp[2], rtmp[3]], [(out, "b", rp)])
            A(lambda e, o3=o3: e.copy(out=o3[:, :, 2 * hf:hd], in_=tA3[:, :, 2 * hf:hd]), [tA], [(out, "c", rp)])

    def obres(out, dup=False):
        return [(out, k, rp) for k in "abc" for rp in range(2 if dup else 1)]

    V(lambda e: e.memset(ones_bf[:], 1.0), [], [ones_bf])
    w_in = d["w_in"]
    wk = load_w(w_in, 0, 2048)
    wv = load_w(w_in, 0, 2560)
    load_w(w_in, 0, 4096, ncols=80, dst=wki)
    for kb in range(4):
        for i in range(4):
            tI = kb * 4 + i
            h_ = norm_tile(d["xk"][tI * 128:(tI + 1) * 128, :])
            tr16(h_, hnT, i * 128, i)
        for i in range(4):
            tI = kb * 4 + i
            pm = ps_mm[nxt("mm")]
            proj(pm, 512, i, wk, 0)
            o_ = ob[nxt("ob")]
            normrope(pm, 4, 128, 16, gk_bc, tabs["cos_k"][:, tI, :], tabs["sin_k"][:, tI, :], [tabs["cos_k"], tabs["sin_k"]], o_)
            pt = ps_tr[nxt("tr")]
            for h in range(4):
                TE(lambda e, pt=pt, h=h, o_=o_: e.transpose(out=pt[:, h, :], in_=o_[:, h * 128:(h + 1) * 128], identity=ident[:]),
                   obres(o_) + [ident], [pt])
            V(lambda e, pt=pt, tI=tI: e.tensor_copy(out=kT[:, :, tI * 128:(tI + 1) * 128], in_=pt[:]), [pt], [(kT, tI)])
            pm = ps_mm[nxt("mm")]
            proj(pm, 512, i, wv, 0)
            A(lambda e, pm=pm, tI=tI: e.copy(out=Vt[:, tI, :, :], in_=pm[:].rearrange("p (a b) -> p a b", a=4)), [pm], [(Vt, tI)])
            pm = ps_mm[nxt("mm")]
            proj(pm, 64, i, wki, 0)
            o_ = ob[nxt("ob")]
            normrope(pm, 1, 64, 8, None, tabs["cos_ki"][:, tI, :], tabs["sin_ki"][:, tI, :], [tabs["cos_ki"], tabs["sin_ki"]], o_, dup=True)
            pt = ps_tr[nxt("tr")]
            TE(lambda e, pt=pt, o_=o_: e.transpose(out=pt[:, 0, :], in_=o_[:, 0:128], identity=ident[:]), obres(o_, True) + [ident], [pt])
            V(lambda e, pt=pt, tI=tI: e.tensor_copy(out=kiT[:, tI * 128:(tI + 1) * 128], in_=pt[:, 0, :]), [pt], [(kiT, tI)])

    SCALE = 128 ** -0.5

    KBIS = 22

    def index_steps(half, i):
        s_ = half * 4 + i
        nkt = 2 * s_ + 2
        S = nkt * 128
        nch = (S + 511) // 512
        sc = scoreb[s_ % 2]
        lo, Wd, nmid, cntt, gg, rmx = bis[s_ % 2]
        steps = stepsb[s_ % 2]
        for h in range(16):
            pair = h // 2
            off = (h % 2) * 64
            for c in range(nch):
                cols = min(512, S - c * 512)
                pm = ps_mm[nxt("mm")]
                TE(lambda e, pm=pm, cols=cols, c=c, off=off, pair=pair: e.matmul(pm[:, 0:cols], lhsT=qiT[off:off + 64, pair, i * 128:(i + 1) * 128],
                                                                                 rhs=kiT[off:off + 64, c * 512:c * 512 + cols], start=True, stop=True),
                   [(qiT, i, pair)] + [(kiT, t) for t in range(c * 4, min(c * 4 + 4, nkt))], [pm])
                rl = relu_t[nxt("relu")]
                A(lambda e, pm=pm, rl=rl, cols=cols: e.activation(out=rl[:, 0:cols], in_=pm[:, 0:cols], func=AF.Relu), [pm], [rl])
                if h == 0:
                    V(lambda e, rl=rl, cols=cols, c=c: e.tensor_scalar(out=sc[:, c * 512:c * 512 + cols], in0=rl[:, 0:cols], scalar1=wi_sb[:, i, 0:1], scalar2=None, op0=ALU.mult),
                      [rl, (wi_sb, i)], [(sc, c)])
                else:
                    V(lambda e, rl=rl, cols=cols, c=c, h=h: e.scalar_tensor_tensor(out=sc[:, c * 512:c * 512 + cols], in0=rl[:, 0:cols], scalar=wi_sb[:, i, h:h + 1],
                                                                                 in1=sc[:, c * 512:c * 512 + cols], op0=ALU.mult, op1=ALU.add),
                      [rl, (wi_sb, i), (sc, c)], [(sc, c)])
                yield
        sres = [(sc, c) for c in range(4)]
        V(lambda e: e.reduce_max(out=rmx[:], in_=sc[:, 0:S], axis=AX.X), sres, [rmx])
        V(lambda e: e.tensor_reduce(out=lo[:], in_=sc[:, 0:S], axis=AX.X, op=ALU.min), sres, [lo])
        V(lambda e: e.tensor_tensor(out=Wd[:], in0=rmx[:], in1=lo[:], op=ALU.subtract), [rmx, lo], [Wd])
        V(lambda e: e.tensor_scalar(out=steps[:], in0=ckrow[:], scalar1=Wd[:, 0:1], scalar2=None, op0=ALU.mult), [ckrow, Wd], [steps])
        V(lambda e: e.scalar_tensor_tensor(out=nmid[:], in0=lo[:], scalar=-1.0, in1=steps[:, 0:1], op0=ALU.mult, op1=ALU.subtract), [lo, steps], [nmid])
        V(lambda e: e.tensor_tensor(out=sc[:, S - 256:S], in0=sc[:, S - 256:S], in1=cbias[:], op=ALU.add), sres + [cbias], sres)
        yield
        for k in range(KBIS):
            A(lambda e: e.activation(out=mask[:, 0:S], in_=sc[:, 0:S], func=AF.Sign, bias=nmid[:], scale=1.0, accum_out=cntt[:]), sres + [nmid], [mask, cntt])
            V(lambda e: e.tensor_scalar(out=gg[:], in0=cntt[:], scalar1=510.5 - S, scalar2=0.5, op0=ALU.is_lt, op1=ALU.subtract), [cntt], [gg])
            V(lambda e, k=k: e.scalar_tensor_tensor(out=nmid[:], in0=gg[:], scalar=steps[:, k:k + 1], in1=nmid[:], op0=ALU.mult, op1=ALU.add), [gg, steps, nmid], [nmid])
            yield
        V(lambda e: e.scalar_tensor_tensor(out=lo[:], in0=steps[:, KBIS - 1:KBIS], scalar=-0.5, in1=nmid[:], op0=ALU.mult, op1=ALU.subtract), [steps, nmid], [lo])
        V(lambda e: e.tensor_scalar(out=mask[:, 0:S], in0=sc[:, 0:S], scalar1=lo[:, 0:1], scalar2=None, op0=ALU.is_ge), sres + [lo], [mask])
        yield
        for j0 in range(0, nkt, 4):
            pt = ps_tr[nxt("tr")]
            nj = min(4, nkt - j0)
            for jj in range(nj):
                j = j0 + jj
                TE(lambda e, pt=pt, jj=jj, j=j: e.transpose(out=pt[:, jj, :], in_=mask[:, j * 128:(j + 1) * 128], identity=ident[:]), [mask, ident], [pt])
            V(lambda e, pt=pt, j0=j0, nj=nj: e.tensor_copy(out=maskT[:, j0:j0 + nj, :], in_=pt[:, 0:nj, :]), [pt], [(maskT, j0 // 4)])
            yield

    def attn_steps(half, i):
        s_ = half * 4 + i
        nkt = 2 * s_ + 2
        for g in range(4):
            for j in range(nkt):
                pS = ps_s[nxt("s")]
                TE(lambda e, pS=pS, g=g, j=j: e.matmul(pS[:].rearrange("p (a b) -> p a b", a=4), lhsT=kT[:, g, j * 128:(j + 1) * 128],
                                                       rhs=qT[:, 4 * g:4 * g + 4, i * 128:(i + 1) * 128], start=True, stop=True),
                   [(kT, j), (qT, i, g)], [pS])
                k = nxt("p")
                p_, pm_ = pT[k], pTm[k]
                A(lambda e, pS=pS, p_=p_: e.activation(out=p_[:], in_=pS[:], func=AF.Exp, scale=SCALE), [pS], [p_])
                G(lambda e, p_=p_, pm_=pm_, j=j: e.tensor_tensor(out=pm_[:].rearrange("p (a b) -> p a b", a=4), in0=p_[:].rearrange("p (a b) -> p a b", a=4),
                                                                in1=maskT[:, j, :].unsqueeze(1).to_broadcast([128, 4, 128]), op=ALU.mult),
                  [p_, (maskT, j // 4)], [pm_])
                TE(lambda e, pm_=pm_, j=j, g=g: e.matmul(ps_oa[:], lhsT=Vt[:, j, g, :], rhs=pm_[:], start=(j == 0), stop=(j == nkt - 1)),
                   [pm_, (Vt, j)], [ps_oa])
                TE(lambda e, pm_=pm_, j=j: e.matmul(ps_os[:], lhsT=ones_bf[:], rhs=pm_[:], start=(j == 0), stop=(j == nkt - 1)),
                   [pm_, ones_bf], [ps_os])
                yield
            V(lambda e: e.reciprocal(out=rsum[:], in_=ps_os[:]), [ps_os], [rsum])
            V(lambda e, g=g: e.tensor_tensor(out=hnT[:, 4 * g:4 * g + 4, i * 128:(i + 1) * 128], in0=ps_oa[:].rearrange("p (a b) -> p a b", a=4),
                                             in1=rsum[:].rearrange("p (a b) -> p a b", a=4), op=ALU.mult),
              [ps_oa, rsum], [(hnT, i, g)])
            yield

    w_out = d["w_out"]
    for half in range(2):
        for i in range(4):
            s_ = half * 4 + i
            h_ = norm_tile(d["xq"][s_ * 128:(s_ + 1) * 128, :])
            tr16(h_, hnT, i * 128, i)
        for u in range(4):
            w = load_w(w_in, 0, u * 512)
            for i in range(4):
                s_ = half * 4 + i
                pm = ps_mm[nxt("mm")]
                proj(pm, 512, i, w, 0)
                o_ = ob[nxt("ob")]
                normrope(pm, 4, 128, 16, gq_bc, tabs["cos_q"][:, s_, :], tabs["sin_q"][:, s_, :], [tabs["cos_q"], tabs["sin_q"]], o_)
                pt = ps_tr[nxt("tr")]
                for h in range(4):
                    TE(lambda e, pt=pt, h=h, o_=o_: e.transpose(out=pt[:, h, :], in_=o_[:, h * 128:(h + 1) * 128], identity=ident[:]),
                       obres(o_) + [ident], [pt])
                V(lambda e, pt=pt, u=u, i=i: e.tensor_copy(out=qT[:, 4 * u:4 * u + 4, i * 128:(i + 1) * 128], in_=pt[:]), [pt], [(qT, i, u)])
        for u in range(2):
            w = load_w(w_in, 0, 3072 + u * 512)
            for i in range(4):
                s_ = half * 4 + i
                pm = ps_mm[nxt("mm")]
                proj(pm, 512, i, w, 0)
                o_ = ob[nxt("ob")]
                normrope(pm, 8, 64, 8, None, tabs["cos_qi"][:, s_, :], tabs["sin_qi"][:, s_, :], [tabs["cos_qi"], tabs["sin_qi"]], o_)
                pt = ps_tr[nxt("tr")]
                for pr in range(4):
                    TE(lambda e, pt=pt, pr=pr, o_=o_: e.transpose(out=pt[:, pr, :], in_=o_[:, pr * 128:(pr + 1) * 128], identity=ident[:]),
                       obres(o_) + [ident], [pt])
                V(lambda e, pt=pt, u=u, i=i: e.tensor_copy(out=qiT[:, 4 * u:4 * u + 4, i * 128:(i + 1) * 128], in_=pt[:]), [pt],
                  [(qiT, i, 4 * u + k) for k in range(4)])
        for i in range(4):
            pm = ps_mm[nxt("mm")]
            proj(pm, 16, i, wki, 64)
            V(lambda e, pm=pm, i=i: e.tensor_scalar(out=wi_sb[:, i, :], in0=pm[:, 0:16], scalar1=1.0 / 32.0, scalar2=None, op0=ALU.mult), [pm], [(wi_sb, i)])
        prev = None
        for i in range(4):
            merge_gens([index_steps(half, i), prev])
            prev = attn_steps(half, i)
        merge_gens([prev])
        for u in range(4):
            w = load_w(w_out, 0, u * 512)
            for i in range(4):
                s_ = half * 4 + i
                pm = ps_mm[nxt("mm")]
                proj(pm, 512, i, w, 0)
                k = nxt("ot")
                xr, o = xres[k], ot[k]
                P.add("sync", lambda e, xr=xr, s_=s_, u=u: e.dma_start(out=xr[:], in_=d["xq"][s_ * 128:(s_ + 1) * 128, u * 512:(u + 1) * 512]), writes=[xr], dma=True)
                V(lambda e, pm=pm, xr=xr, o=o: e.tensor_tensor(out=o[:], in0=pm[:], in1=xr[:], op=ALU.add), [pm, xr], [o])
                P.add("sync", lambda e, o=o, s_=s_, u=u: e.dma_start(out=d["out"][s_ * 128:(s_ + 1) * 128, u * 512:(u + 1) * 512], in_=o[:]),
                      reads=[o], writes=[("out", s_, u)], dma=True)


def rope_tables(pos, rot):
    half = rot // 2
    inv = 500000.0 ** (-2.0 * np.arange(half, dtype=np.float32) / rot)
    ang = pos.astype(np.float32)[:, None] * inv[None, :].astype(np.float32)
    return np.cos(ang).astype(np.float32), np.sin(ang).astype(np.float32)


def att_consts(p):
    import ml_dtypes
    c = {}
    kpos = np.arange(2048)
    ck, sk = rope_tables(kpos, 32)
    c["cos_k"] = np.ascontiguousarray(ck.reshape(16, 128, 16).transpose(1, 0, 2))
    c["sin_k"] = np.ascontiguousarray(sk.reshape(16, 128, 16).transpose(1, 0, 2))
    ck, sk = rope_tables(kpos, 16)
    c["cos_ki"] = np.ascontiguousarray(ck.reshape(16, 128, 8).transpose(1, 0, 2))
    c["sin_ki"] = np.ascontiguousarray(sk.reshape(16, 128, 8).transpose(1, 0, 2))
    qpos = np.concatenate([np.arange(128) + (2 * i + p) * 128 for i in range(8)])
    cq, sq = rope_tables(qpos, 32)
    c["cos_q"] = np.ascontiguousarray(cq.reshape(8, 128, 16).transpose(1, 0, 2))
    c["sin_q"] = np.ascontiguousarray(sq.reshape(8, 128, 16).transpose(1, 0, 2))
    cq, sq = rope_tables(qpos, 16)
    c["cos_qi"] = np.ascontiguousarray(cq.reshape(8, 128, 8).transpose(1, 0, 2))
    c["sin_qi"] = np.ascontiguousarray(sq.reshape(8, 128, 8).transpose(1, 0, 2))
    t = np.arange(128)[:, None]
    col = np.arange(256)[None, :]
    c["cbias"] = np.where(col <= t + 128 * p, 0.0, NEG).astype(np.float32)
    c["ident"] = np.eye(128).astype(ml_dtypes.bfloat16)
    return c


ATT_SHAPES = {"xk": ([2048, D], F32), "xq": ([1024, D], F32), "gmix": ([D], F32), "gq": ([128], F32), "gk": ([128], F32),
              "w_in": ([D, 4176], F32), "w_out": ([D, D], F32),
              "cos_k": ([128, 16, 16], F32), "sin_k": ([128, 16, 16], F32), "cos_ki": ([128, 16, 8], F32), "sin_ki": ([128, 16, 8], F32),
              "cos_q": ([128, 8, 16], F32), "sin_q": ([128, 8, 16], F32), "cos_qi": ([128, 8, 8], F32), "sin_qi": ([128, 8, 8], F32),
              "cbias": ([128, 256], F32), "ident": ([128, 128], BF16)}


def q_tiles(x_b, p):
    return np.concatenate([x_b[(2 * i + p) * 128:(2 * i + p + 1) * 128] for i in range(8)], axis=0)


D = 2048
DFF = 8192
EPS = 1e-6


def build_ffn(P, nc, sb, ps, h_dram, g_dram, wu_dram, wd_dram, out_dram, ident_dram, ntok):
    NB = ntok // 512
    ident = sb("ident", [128, 128], BF16)
    g_bc = sb("g_bc", [128, D], F32)
    h_t = [sb("h_t%d" % i, [128, D], F32) for i in range(4)]
    hn = [sb("hn%d" % i, [128, D], BF16) for i in range(2)]
    sq = sb("sq", [128, D], BF16)
    ss = [sb("ss%d" % i, [128, 1], F32) for i in range(2)]
    rstd = [sb("rstd%d" % i, [128, 1], F32) for i in range(2)]
    hnT = sb("hnT", [128, 16, 512], BF16)
    rT = sb("rT", [128, 64, 512], BF16)
    NW = 3
    wt = [sb("wt%d" % i, [128, 16, 512], BF16) for i in range(NW)]
    ot = [sb("ot%d" % i, [128, 512], F32) for i in range(2)]
    relu_t = [sb("relu%d" % i, [128, 512], F32) for i in range(2)]
    ps_tr = [ps("ps_tr%d" % i, [128, 4, 128], BF16) for i in range(2)]
    ps_u = [ps("ps_u%d" % i, [128, 512], F32) for i in range(2)]
    ps_d = [ps("ps_d%d" % i, [128, 512], F32) for i in range(4)]

    epsT = sb("epsT", [128, 1], F32)
    P.add("vector", lambda e: e.memset(epsT[:], EPS), writes=[epsT])
    P.add("sync", lambda e: e.dma_start(out=ident[:], in_=ident_dram), writes=[ident], dma=True)
    P.add("sync", lambda e: e.dma_start(out=g_bc[:], in_=g_dram.partition_broadcast(128)), writes=[g_bc], dma=True)

    wcnt = [0]

    def load_w(dram, r0, c0):
        i = wcnt[0] % NW
        wcnt[0] += 1
        w = wt[i]
        for q in range(4):
            src = dram[r0 + q * 512:r0 + (q + 1) * 512, c0:c0 + 512].rearrange("(c p) n -> p c n", p=128)
            P.add("gpsimd", lambda e, w=w, q=q, src=src: e.dma_start(out=w[:, q * 4:(q + 1) * 4, :], in_=src),
                  writes=[(w, q)], dma=True)
        return w

    trc = [0]
    uc = [0]
    oc = [0]
    for tb in range(NB):
        for i in range(4):
            t0 = tb * 512 + i * 128
            ht = h_t[i]
            P.add("sync", lambda e, ht=ht, t0=t0: e.dma_start(out=ht[:], in_=h_dram[t0:t0 + 128, :]), writes=[ht], dma=True)
            s_ = ss[i % 2]
            r_ = rstd[i % 2]
            hb = hn[i % 2]
            P.add("scalar", lambda e, ht=ht, s_=s_: e.activation(out=sq[:], in_=ht[:], func=AF.Square, accum_out=s_[:]),
                  reads=[ht], writes=[sq, s_])
            P.add("scalar", lambda e, s_=s_: e.activation(out=s_[:], in_=s_[:], func=AF.Sqrt, bias=epsT[:], scale=1.0 / D),
                  reads=[s_, epsT], writes=[s_])
            P.add("vector", lambda e, s_=s_, r_=r_: e.reciprocal(out=r_[:], in_=s_[:]),
                  reads=[s_], writes=[r_])
            P.add("vector", lambda e, ht=ht, r_=r_, hb=hb: e.scalar_tensor_tensor(out=hb[:], in0=ht[:], scalar=r_[:], in1=g_bc[:], op0=ALU.mult, op1=ALU.mult),
                  reads=[ht, r_, g_bc], writes=[hb])
            for j in range(4):
                pt = ps_tr[trc[0] % 2]
                trc[0] += 1
                for k in range(4):
                    dc = j * 4 + k
                    P.add("tensor", lambda e, pt=pt, k=k, hb=hb, dc=dc: e.transpose(out=pt[:, k, :], in_=hb[:, dc * 128:(dc + 1) * 128], identity=ident[:]),
                          reads=[hb, ident], writes=[pt])
                eng = "scalar" if (j % 2 == 0) else "vector"
                if eng == "scalar":
                    P.add("scalar", lambda e, pt=pt, j=j, i=i: e.copy(out=hnT[:, j * 4:(j + 1) * 4, i * 128:(i + 1) * 128], in_=pt[:]),
                          reads=[pt], writes=[(hnT, i, j)])
                else:
                    P.add("vector", lambda e, pt=pt, j=j, i=i: e.tensor_copy(out=hnT[:, j * 4:(j + 1) * 4, i * 128:(i + 1) * 128], in_=pt[:]),
                          reads=[pt], writes=[(hnT, i, j)])
        hnT_res = [(hnT, i, j) for i in range(4) for j in range(4)]
        for fg in range(16):
            w = load_w(wu_dram, 0, fg * 512)
            for fcl in range(4):
                fc = fg * 4 + fcl
                pu = ps_u[uc[0] % 2]
                uc[0] += 1
                for dc in range(16):
                    P.add("tensor", lambda e, pu=pu, w=w, fcl=fcl, dc=dc: e.matmul(pu[:], lhsT=w[:, dc, fcl * 128:(fcl + 1) * 128], rhs=hnT[:, dc, :], start=(dc == 0), stop=(dc == 15)),
                          reads=[(w, dc // 4)] + hnT_res, writes=[pu])
                ru = relu_t[uc[0] % 2]
                P.add("scalar", lambda e, pu=pu, ru=ru: e.activation(out=ru[:], in_=pu[:], func=AF.Relu),
                      reads=[pu], writes=[ru])
                P.add("vector", lambda e, ru=ru, fc=fc: e.tensor_tensor(out=rT[:, fc, :], in0=ru[:], in1=ru[:], op=ALU.mult),
                      reads=[ru], writes=[(rT, fc)])
        for db in range(4):
            for fq in range(4):
                w = load_w(wd_dram, fq * 2048, db * 512)
                for i in range(4):
                    for k in range(16):
                        fc = fq * 16 + k
                        P.add("tensor", lambda e, i=i, w=w, k=k, fc=fc, fq=fq: e.matmul(ps_d[i][:], lhsT=rT[:, fc, i * 128:(i + 1) * 128], rhs=w[:, k, :], start=(fq == 0 and k == 0), stop=(fq == 3 and k == 15)),
                              reads=[(w, k // 4), (rT, fc)], writes=[ps_d[i]])
            for i in range(4):
                o = ot[oc[0] % 2]
                oc[0] += 1
                t0 = tb * 512 + i * 128
                P.add("vector", lambda e, o=o, i=i, db=db: e.tensor_tensor(out=o[:], in0=ps_d[i][:], in1=h_t[i][:, db * 512:(db + 1) * 512], op=ALU.add),
                      reads=[ps_d[i], h_t[i]], writes=[o])
                P.add("sync", lambda e, o=o, t0=t0, db=db: e.dma_start(out=out_dram[t0:t0 + 128, db * 512:(db + 1) * 512], in_=o[:]),
                      reads=[o], writes=[("out", t0, db)], dma=True)


D = 2048
EPS = 1e-6


def build_ml(P, nc, sb, ps, d):
    def V(fn, r, w):
        return P.add("vector", fn, reads=r, writes=w)

    def A(fn, r, w):
        return P.add("scalar", fn, reads=r, writes=w)

    def G(fn, r, w):
        return P.add("gpsimd", fn, reads=r, writes=w)

    def TE(fn, r, w):
        return P.add("tensor", fn, reads=r, writes=w)

    def LD(t, src):
        return P.add("sync", lambda e: e.dma_start(out=t[:], in_=src), writes=[t], dma=True)

    ident = sb("ident", [128, 128], BF16)
    LD(ident, d["ident"])
    i8 = sb("i8", [8, 8], F32)
    LD(i8, d["i8"])
    hsel = sb("hsel", [8, 8, 128], F32)
    LD(hsel, d["hsel"])
    bmask = sb("bmask", [128, 128], BF16)
    LD(bmask, d["bmask"])
    epsT = sb("epsT", [128, 1], F32)
    V(lambda e: e.memset(epsT[:], EPS), [], [epsT])
    one8 = sb("one8", [8, 1], F32)
    V(lambda e: e.memset(one8[:], 1.0), [], [one8])
    g_bc = sb("g_bc", [128, D], F32)
    LD(g_bc, d["gmix"].partition_broadcast(128))
    hg_bc = sb("hg_bc", [128, 256], F32)
    LD(hg_bc, d["hgain"].partition_broadcast(128))
    bi = sb("bi", [8, 1], F32)
    bf = sb("bf", [8, 1], F32)
    LD(bi, d["bgate"][0:8].rearrange("(a b) -> a b", b=1))
    LD(bf, d["bgate"][8:16].rearrange("(a b) -> a b", b=1))
    V(lambda e: e.tensor_scalar(out=bi[:], in0=bi[:], scalar1=1.0 / 15.0, scalar2=None, op0=ALU.mult), [bi], [bi])
    V(lambda e: e.tensor_scalar(out=bf[:], in0=bf[:], scalar1=1.0 / 15.0, scalar2=None, op0=ALU.mult), [bf], [bf])

    NW = 2
    wt = [sb("wt%d" % i, [128, 16, 512], BF16) for i in range(NW)]
    wg = sb("wg", [128, 16, 16], BF16)
    xt = [sb("xt%d" % i, [128, D], F32) for i in range(2)]
    hb = [sb("hb%d" % i, [128, D], BF16) for i in range(2)]
    ss = [sb("ss%d" % i, [128, 1], F32) for i in range(2)]
    rstd = [sb("rstd%d" % i, [128, 1], F32) for i in range(2)]
    hnT = sb("hnT", [128, 16, 512], BF16)

    class V3:
        def __init__(self, t):
            self.t = t

        def __getitem__(self, k):
            return self.t[:].rearrange("p (h v) -> p h v", h=8)[k]
    sq, sq3 = xt[0], V3(xt[0])
    hsb, hsb3 = xt[1], V3(xt[1])
    gated = hb[0]
    k_tok = sb("k_tok", [128, 4, 1024], BF16)
    v_tok = sb("v_tok", [128, 4, 2048], BF16)
    kT = sb("kT", [128, 8, 512], BF16)
    qT = sb("qT", [128, 8, 512], BF16)
    qA = sb("qA", [128, 8, 512], BF16)
    qB = sb("qB", [128, 8, 512], BF16)
    o_sig = sb("o_sig", [128, 4, 2048], BF16)
    ig_r = sb("ig_r", [8, 512], F32)
    fg_r = sb("fg_r", [8, 512], F32)
    lf = sb("lf", [8, 512], F32)
    lf2 = sb("lf2", [8, 512], F32)
    u_r = sb("u_r", [8, 512], F32)
    e_r = sb("e_r", [8, 512], F32)
    thr_r = sb("thr_r", [8, 512], F32)
    ux = sb("ux", [8, 8], F32)
    Mx = sb("Mx", [8, 8], F32)
    fpre = sb("fpre", [8, 8], F32)
    f_r = sb("f_r", [8, 8], F32)
    mcur = sb("mcur", [8, 1], F32)
    V(lambda e: e.memset(mcur[:], 0.0), [], [mcur])
    if "pflag" in d:
        flg = sb("flg", [128, 2], F32)
        LD(flg, d["pflag"])
    e_tok = sb("e_tok", [128, 4, 8], F32)
    e_bf = sb("e_bf", [128, 4, 8], BF16)
    thr_tok = sb("thr_tok", [128, 4, 8], F32)
    F_bc = sb("F_bc", [128, 8, 8], F32)
    Cs = sb("Cs", [128, 8, 256], F32)
    ns = sb("ns", [128, 8], F32)
    V(lambda e: e.memset(Cs[:], 0.0), [], [(Cs, 0), (Cs, 1)])
    V(lambda e: e.memset(ns[:], 0.0), [], [(ns, 0), (ns, 1)])
    Cbf = [sb("Cbf%d" % i, [128, 4, 256], BF16) for i in range(2)]
    nbf = [sb("nbf%d" % i, [128, 4], BF16) for i in range(2)]
    ve = [sb("ve%d" % i, [128, 4, 256], BF16) for i in range(2)]
    Sm = sb("Sm", [128, 4, 128], BF16)
    dtmp = sb("dtmp", [128, 4], F32)
    rd = sb("rd", [128, 4], F32)
    st1 = sb("st1", [128, 8], F32)
    st2 = sb("st2", [128, 8], F32)
    ot = [sb("ot%d" % i, [128, 512], F32) for i in range(2)]
    xres = [sb("xres%d" % i, [128, 512], F32) for i in range(2)]
    ps_tr = ps("tr", [128, 4, 128], BF16)
    ps_mm = [ps("mm%d" % i, [128, 512], F32) for i in range(2)]
    psN = ps("N", [128, 4, 256], F32)
    psC = ps("C", [128, 4, 256], F32)
    pss = ps("small", [128, 128], F32)

    cnt = {"w": 0, "x": 0, "mm": 0, "ot": 0, "ve": 0}

    def nxt(k, n=2):
        v = cnt[k] % n
        cnt[k] += 1
        return v

    def load_w(dram, c0, ncols=512, dst=None):
        w = dst if dst is not None else wt[nxt("w", NW)]
        for q in range(4):
            src = dram[q * 512:(q + 1) * 512, c0:c0 + ncols].rearrange("(c p) n -> p c n", p=128)
            P.add("gpsimd", lambda e, w=w, q=q, src=src: e.dma_start(out=w[:, q * 4:(q + 1) * 4, 0:ncols], in_=src),
                  writes=[(w, q)], dma=True)
        return w

    def norm_tile(src_ap):
        k = nxt("x")
        x_, h_, s_, r_ = xt[k], hb[k], ss[k], rstd[k]
        P.add("sync", lambda e: e.dma_start(out=x_[:], in_=src_ap), writes=[x_], dma=True)
        A(lambda e: e.activation(out=h_[:], in_=x_[:], func=AF.Square, accum_out=s_[:]), [x_], [h_, s_])
        A(lambda e: e.activation(out=s_[:], in_=s_[:], func=AF.Sqrt, bias=epsT[:], scale=1.0 / D), [s_, epsT], [s_])
        V(lambda e: e.reciprocal(out=r_[:], in_=s_[:]), [s_], [r_])
        V(lambda e: e.scalar_tensor_tensor(out=h_[:], in0=x_[:], scalar=r_[:], in1=g_bc[:], op0=ALU.mult, op1=ALU.mult),
          [x_, r_, g_bc], [h_])
        return h_

    def tr16(h_, hres, i):
        for j in range(4):
            for k in range(4):
                dc = j * 4 + k
                TE(lambda e, k=k, dc=dc: e.transpose(out=ps_tr[:, k, :], in_=h_[:, dc * 128:(dc + 1) * 128], identity=ident[:]),
                   hres + [ident], [ps_tr])
            if j % 2 == 0:
                A(lambda e, j=j: e.copy(out=hnT[:, j * 4:(j + 1) * 4, i * 128:(i + 1) * 128], in_=ps_tr[:]), [ps_tr], [(hnT, i, j)])
            else:
                V(lambda e, j=j: e.tensor_copy(out=hnT[:, j * 4:(j + 1) * 4, i * 128:(i + 1) * 128], in_=ps_tr[:]), [ps_tr], [(hnT, i, j)])

    def tres(i):
        return [(hnT, i, j) for j in range(4)]

    def allT():
        return [(hnT, i, j) for i in range(4) for j in range(4)]

    def proj_tok(pm, ncols, i, w, c_lo):
        for dc in range(16):
            TE(lambda e, dc=dc: e.matmul(pm[:, 0:ncols], lhsT=hnT[:, dc, i * 128:(i + 1) * 128], rhs=w[:, dc, c_lo:c_lo + ncols],
                                         start=(dc == 0), stop=(dc == 15)),
               tres(i) + [(w, dc // 4)], [pm])

    def proj_feat(pm, m, w, c_lo):
        for dc in range(16):
            TE(lambda e, dc=dc: e.matmul(pm[0:m, :], lhsT=w[:, dc, c_lo:c_lo + m], rhs=hnT[:, dc, :], start=(dc == 0), stop=(dc == 15)),
               allT() + [(w, dc // 4)], [pm])

    w_in = d["w_in"]
    w_out = d["w_out"]
    KS = 128 ** -0.5
    for blk in range(4):
        own = blk >= 2
        r0 = (blk % 2) * 512
        if blk == 2 and "pflag" in d:
            for hg_ in range(2):
                hs_ = slice(4 * hg_, 4 * hg_ + 4)
                V(lambda e, hs_=hs_: e.tensor_scalar(out=Cs[:, hs_, :], in0=Cs[:, hs_, :], scalar1=flg[:, 1:2], scalar2=None, op0=ALU.mult), [(Cs, hg_), flg], [(Cs, hg_)])
                V(lambda e, hs_=hs_: e.tensor_scalar(out=ns[:, hs_], in0=ns[:, hs_], scalar1=flg[:, 1:2], scalar2=None, op0=ALU.mult), [(ns, hg_), flg], [(ns, hg_)])
        for i in range(4):
            if own:
                src_ap = d["ho"][r0 + i * 128:r0 + (i + 1) * 128, :]
            elif "hp_tile" in d:
                src_ap = d["hp_tile"](blk * 4 + i)
            else:
                src_ap = d["hp"][r0 + i * 128:r0 + (i + 1) * 128, :]
            h_ = norm_tile(src_ap)
            tr16(h_, [h_], i)
        load_w(w_in, 6144, ncols=16, dst=wg)
        for gi, (dst, bb) in enumerate(((ig_r, bi), (fg_r, bf))):
            pm = ps_mm[nxt("mm")]
            proj_feat(pm, 8, wg, gi * 8)
            A(lambda e, pm=pm, dst=dst, bb=bb: e.activation(out=dst[:], in_=pm[0:8, :], func=AF.Tanh, bias=bb[:], scale=1.0 / 15.0), [pm, bb], [dst])
            V(lambda e, dst=dst: e.tensor_scalar(out=dst[:], in0=dst[:], scalar1=15.0, scalar2=None, op0=ALU.mult), [dst], [dst])
        A(lambda e: e.activation(out=lf2[:], in_=fg_r[:], func=AF.Exp, scale=-1.0), [fg_r], [lf2])
        A(lambda e: e.activation(out=lf[:], in_=lf2[:], func=AF.Ln, bias=one8[:], scale=1.0), [lf2, one8], [lf])
        V(lambda e: e.tensor_scalar(out=lf[:], in0=lf[:], scalar1=-1.0, scalar2=None, op0=ALU.mult), [lf], [lf])
        bufs = [lf, lf2]
        for si, dd in enumerate((1, 2, 4, 8, 16, 32)):
            s_, d_ = bufs[si % 2], bufs[(si + 1) % 2]
            s3 = s_[:].rearrange("p (c l) -> p c l", l=64)
            d3 = d_[:].rearrange("p (c l) -> p c l", l=64)
            V(lambda e, s3=s3, d3=d3, dd=dd: e.tensor_copy(out=d3[:, :, 0:dd], in_=s3[:, :, 0:dd]), [s_], [d_])
            V(lambda e, s3=s3, d3=d3, dd=dd: e.tensor_tensor(out=d3[:, :, dd:64], in0=s3[:, :, dd:64], in1=s3[:, :, 0:64 - dd], op=ALU.add), [s_], [d_])
        lf3 = lf[:].rearrange("p (c l) -> p c l", l=64)
        lres = [lf]
        V(lambda e: e.tensor_tensor(out=u_r[:], in0=ig_r[:], in1=lf[:], op=ALU.subtract), [ig_r] + lres, [u_r])
        u3 = u_r[:].rearrange("p (c l) -> p c l", l=64)
        V(lambda e: e.reduce_max(out=ux[:], in_=u3, axis=AX.X), [u_r], [ux])
        for c in range(8):
            V(lambda e, c=c: e.tensor_tensor(out=Mx[:, c:c + 1], in0=mcur[:], in1=ux[:, c:c + 1], op=ALU.max), [mcur, ux], [(Mx, c)])
            V(lambda e, c=c: e.tensor_tensor(out=fpre[:, c:c + 1], in0=mcur[:], in1=Mx[:, c:c + 1], op=ALU.subtract), [mcur, (Mx, c)], [(fpre, c)])
            V(lambda e, c=c: e.tensor_tensor(out=mcur[:], in0=lf3[:, c, 63:64], in1=Mx[:, c:c + 1], op=ALU.add), lres + [(Mx, c)], [mcur])
        Mres = [(Mx, c) for c in range(8)]
        A(lambda e: e.activation(out=f_r[:], in_=fpre[:], func=AF.Exp), [(fpre, c) for c in range(8)], [f_r])
        V(lambda e: e.tensor_tensor(out=u3, in0=u3, in1=Mx[:, 0:8].unsqueeze(2).to_broadcast([8, 8, 64]), op=ALU.subtract), [u_r] + Mres, [u_r])
        A(lambda e: e.activation(out=e_r[:], in_=u_r[:], func=AF.Exp), [u_r], [e_r])
        if own:
            V(lambda e: e.tensor_tensor(out=lf3, in0=lf3, in1=Mx[:, 0:8].unsqueeze(2).to_broadcast([8, 8, 64]), op=ALU.add), lres + Mres, lres)
            A(lambda e: e.activation(out=thr_r[:], in_=lf[:], func=AF.Exp, scale=-1.0), lres, [thr_r])
        for t in range(4):
            TE(lambda e, t=t: e.matmul(pss[:, 0:8], lhsT=e_r[:, t * 128:(t + 1) * 128], rhs=i8[:], start=True, stop=True), [e_r, i8], [pss])
            V(lambda e, t=t: e.tensor_copy(out=e_tok[:, t, :], in_=pss[:, 0:8]), [pss], [(e_tok, t)])
            A(lambda e, t=t: e.copy(out=e_bf[:, t, :], in_=pss[:, 0:8]), [pss], [(e_bf, t)])
            if own:
                TE(lambda e, t=t: e.matmul(pss[:, 8:16], lhsT=thr_r[:, t * 128:(t + 1) * 128], rhs=i8[:], start=True, stop=True), [thr_r, i8], [pss])
                V(lambda e, t=t: e.tensor_copy(out=thr_tok[:, t, :], in_=pss[:, 8:16]), [pss], [(thr_tok, t)])
        for h in range(8):
            TE(lambda e, h=h: e.matmul(pss[:, 32 + h * 8:40 + h * 8], lhsT=hsel[:, h, :], rhs=f_r[:], start=True, stop=True), [hsel, f_r], [pss])
        V(lambda e: e.tensor_copy(out=F_bc[:], in_=pss[:, 32:96].rearrange("p (h c) -> p h c", h=8)), [pss], [F_bc])
        for u in range(2):
            w = load_w(w_in, 1024 + u * 512)
            for i in range(4):
                pm = ps_mm[nxt("mm")]
                proj_tok(pm, 512, i, w, 0)
                A(lambda e, pm=pm, i=i, u=u: e.mul(out=k_tok[:, i, u * 512:(u + 1) * 512], in_=pm[:], mul=KS), [pm], [(k_tok, i, u)])
            if own:
                for hh in range(4):
                    pm = ps_mm[nxt("mm")]
                    proj_feat(pm, 128, w, hh * 128)
                    A(lambda e, pm=pm, u=u, hh=hh: e.mul(out=kT[:, 4 * u + hh, :], in_=pm[:], mul=KS), [pm], [(kT, 4 * u + hh)])
        if own:
            for u in range(2):
                w = load_w(w_in, u * 512)
                for hh in range(4):
                    pm = ps_mm[nxt("mm")]
                    proj_feat(pm, 128, w, hh * 128)
                    h = 4 * u + hh
                    A(lambda e, pm=pm, h=h: e.copy(out=qT[:, h, :], in_=pm[:]), [pm], [(qT, h)])
            qres = [(qT, h) for h in range(8)]
            q4 = lambda t_: t_[:].rearrange("p h (t a l) -> p h t a l", t=4, a=2)
            A(lambda e: e.copy(out=qA[:], in_=qT[:]), qres, [qA])
            G(lambda e: e.memset(q4(qA)[:, :, :, 1, :], 0.0), [qA], [qA])
            A(lambda e: e.copy(out=qB[:], in_=qT[:]), qres, [qB])
            G(lambda e: e.memset(q4(qB)[:, :, :, 0, :], 0.0), [qB], [qB])
        for u in range(4):
            w = load_w(w_in, 2048 + u * 512)
            for i in range(4):
                pm = ps_mm[nxt("mm")]
                proj_tok(pm, 512, i, w, 0)
                V(lambda e, pm=pm, i=i, u=u: e.tensor_copy(out=v_tok[:, i, u * 512:(u + 1) * 512], in_=pm[:]), [pm], [(v_tok, i, u)])
        if own:
            for u in range(4):
                w = load_w(w_in, 4096 + u * 512)
                for i in range(4):
                    pm = ps_mm[nxt("mm")]
                    proj_tok(pm, 512, i, w, 0)
                    A(lambda e, pm=pm, i=i, u=u: e.activation(out=o_sig[:, i, u * 512:(u + 1) * 512], in_=pm[:], func=AF.Sigmoid), [pm], [(o_sig, i, u)])
        def do_half(t, hg, half, ve_, own=own):
            hs = slice(4 * hg, 4 * hg + 4)
            c = 2 * t + half
            pl = slice(64 * half, 64 * half + 64)
            cb, nb = Cbf[half], nbf[half]
            V(lambda e, hs=hs, c=c: e.tensor_tensor(out=Cs[:, hs, :], in0=Cs[:, hs, :], in1=F_bc[:, hs, c:c + 1].to_broadcast([128, 4, 256]), op=ALU.mult),
              [(Cs, hg), F_bc], [(Cs, hg)])
            V(lambda e, hs=hs, c=c: e.tensor_tensor(out=ns[:, hs], in0=ns[:, hs], in1=F_bc[:, hs, c], op=ALU.mult), [(ns, hg), F_bc], [(ns, hg)])
            if own:
                A(lambda e, cb=cb, hs=hs: e.copy(out=cb[:], in_=Cs[:, hs, :]), [(Cs, hg)], [cb])
                A(lambda e, nb=nb, hs=hs: e.copy(out=nb[:], in_=ns[:, hs]), [(ns, hg)], [nb])
            for hh in range(4):
                h = 4 * hg + hh
                TE(lambda e, hh=hh, h=h, pl=pl, ve_=ve_: e.matmul(psC[:, hh, :], lhsT=k_tok[pl, t, h * 128:(h + 1) * 128], rhs=ve_[pl, hh, :], start=True, stop=True),
                   [(k_tok, t, h // 4), ve_], [psC])
                TE(lambda e, hh=hh, h=h, pl=pl: e.matmul(pss[:, 16 + hh:17 + hh], lhsT=k_tok[pl, t, h * 128:(h + 1) * 128], rhs=e_bf[pl, t, h:h + 1], start=True, stop=True),
                   [(k_tok, t, h // 4), (e_bf, t)], [pss])
            V(lambda e, hs=hs: e.tensor_tensor(out=Cs[:, hs, :], in0=Cs[:, hs, :], in1=psC[:], op=ALU.add), [(Cs, hg), psC], [(Cs, hg)])
            V(lambda e, hs=hs: e.tensor_tensor(out=ns[:, hs], in0=ns[:, hs], in1=pss[:, 16:20], op=ALU.add), [(ns, hg), pss], [(ns, hg)])

        def do_group(t, hg, own=own):
            tc_ = slice(t * 128, (t + 1) * 128)
            hs = slice(4 * hg, 4 * hg + 4)
            ve_ = ve[nxt("ve")]
            V(lambda e, ve_=ve_, t=t, hs=hs: e.tensor_tensor(out=ve_[:], in0=v_tok[:, t, hg * 1024:(hg + 1) * 1024].rearrange("p (h v) -> p h v", h=4),
                                                            in1=e_tok[:, t, hs].unsqueeze(2).to_broadcast([128, 4, 256]), op=ALU.mult),
              [(v_tok, t, 2 * hg), (v_tok, t, 2 * hg + 1), (e_tok, t)], [ve_])
            if own:
                for hh in range(4):
                    h = 4 * hg + hh
                    TE(lambda e, hh=hh, h=h: e.matmul(psN[:, hh, 0:128], lhsT=kT[:, h, tc_], rhs=qT[:, h, tc_], start=True, stop=True),
                       [(kT, h), (qT, h)], [psN])
                V(lambda e: e.tensor_tensor(out=Sm[:], in0=psN[:, :, 0:128], in1=bmask[:, :].unsqueeze(1).to_broadcast([128, 4, 128]), op=ALU.mult), [psN, bmask], [Sm])
            for half in range(2):
                do_half(t, hg, half, ve_)
            if own:
                for hh in range(4):
                    h = 4 * hg + hh
                    TE(lambda e, hh=hh, ve_=ve_: e.matmul(psN[:, hh, :], lhsT=Sm[:, hh, :], rhs=ve_[:, hh, :], start=True, stop=False), [Sm, ve_], [psN])
                    TE(lambda e, hh=hh, h=h: e.matmul(psN[:, hh, :], lhsT=qA[:, h, tc_], rhs=Cbf[0][:, hh, :], start=False, stop=False), [qA, Cbf[0]], [psN])
                    TE(lambda e, hh=hh, h=h: e.matmul(psN[:, hh, :], lhsT=qB[:, h, tc_], rhs=Cbf[1][:, hh, :], start=False, stop=True), [qB, Cbf[1]], [psN])
                    TE(lambda e, hh=hh, h=h: e.matmul(pss[:, 24 + hh:25 + hh], lhsT=Sm[:, hh, :], rhs=e_bf[:, t, h:h + 1], start=True, stop=False), [Sm, (e_bf, t)], [pss])
                    TE(lambda e, hh=hh, h=h: e.matmul(pss[:, 24 + hh:25 + hh], lhsT=qA[:, h, tc_], rhs=nbf[0][:, hh:hh + 1], start=False, stop=False), [qA, nbf[0]], [pss])
                    TE(lambda e, hh=hh, h=h: e.matmul(pss[:, 24 + hh:25 + hh], lhsT=qB[:, h, tc_], rhs=nbf[1][:, hh:hh + 1], start=False, stop=True), [qB, nbf[1]], [pss])
                A(lambda e: e.activation(out=dtmp[:], in_=pss[:, 24:28], func=AF.Abs), [pss], [dtmp])
                V(lambda e, hs=hs: e.tensor_tensor(out=dtmp[:], in0=dtmp[:], in1=thr_tok[:, t, hs], op=ALU.max), [dtmp, (thr_tok, t)], [dtmp])
                V(lambda e: e.reciprocal(out=rd[:], in_=dtmp[:]), [dtmp], [rd])
                V(lambda e, hs=hs: e.tensor_tensor(out=hsb3[:, hs, :], in0=psN[:], in1=rd[:, :].unsqueeze(2).to_broadcast([128, 4, 256]), op=ALU.mult),
                  [psN, rd], [hsb])

        def do_tile(t, own=own):
            for hg in range(2):
                do_group(t, hg)
            if own:
                hres = [hsb]
                V(lambda e: e.tensor_tensor(out=sq[:], in0=hsb[:], in1=hsb[:], op=ALU.mult), hres, [sq])
                V(lambda e: e.reduce_sum(out=st1[:], in_=sq3[:], axis=AX.X), [sq], [st1])
                A(lambda e: e.activation(out=st1[:], in_=st1[:], func=AF.Sqrt, bias=epsT[:], scale=1.0 / 256), [st1, epsT], [st1])
                V(lambda e: e.reciprocal(out=st2[:], in_=st1[:]), [st1], [st2])
                V(lambda e: e.tensor_tensor(out=hsb3[:], in0=hsb3[:], in1=st2[:, :].unsqueeze(2).to_broadcast([128, 8, 256]), op=ALU.mult), hres + [st2], hres)
                V(lambda e: e.tensor_tensor(out=hsb3[:], in0=hsb3[:], in1=hg_bc[:, :].unsqueeze(1).to_broadcast([128, 8, 256]), op=ALU.mult), hres + [hg_bc], hres)
                V(lambda e, t=t: e.tensor_tensor(out=gated[:], in0=hsb[:], in1=o_sig[:, t, :], op=ALU.mult),
                  hres + [(o_sig, t, u) for u in range(4)], [gated])
                tr16(gated, [gated], t)

        for t in range(4):
            do_tile(t)
        if own:
            for u in range(4):
                w = load_w(w_out, u * 512)
                for i in range(4):
                    pm = ps_mm[nxt("mm")]
                    proj_tok(pm, 512, i, w, 0)
                    k = nxt("ot")
                    xr, o = xres[k], ot[k]
                    rr = r0 + i * 128
                    P.add("sync", lambda e, xr=xr, rr=rr, u=u: e.dma_start(out=xr[:], in_=d["ho"][rr:rr + 128, u * 512:(u + 1) * 512]), writes=[xr], dma=True)
                    V(lambda e, pm=pm, xr=xr, o=o: e.tensor_tensor(out=o[:], in0=pm[:], in1=xr[:], op=ALU.add), [pm, xr], [o])
                    P.add("sync", lambda e, o=o, rr=rr, u=u: e.dma_start(out=d["out"][rr:rr + 128, u * 512:(u + 1) * 512], in_=o[:]),
                          reads=[o], writes=[("out", rr, u)], dma=True)


def ml_consts():
    import ml_dtypes
    c = {}
    c["ident"] = np.eye(128).astype(ml_dtypes.bfloat16)
    c["i8"] = np.eye(8).astype(np.float32)
    hs = np.zeros((8, 8, 128), np.float32)
    for h in range(8):
        hs[h, h, :] = 1.0
    c["hsel"] = hs
    s = np.arange(128)[:, None]
    l = np.arange(128)[None, :]
    c["bmask"] = ((s <= l) & (s // 64 == l // 64)).astype(ml_dtypes.bfloat16)
    return c


ML_SHAPES = {"hp": ([1024, D], F32), "ho": ([1024, D], F32), "gmix": ([D], F32), "hgain": ([256], F32), "bgate": ([16], F32),
             "w_in": ([D, 6160], F32), "w_out": ([D, D], F32), "ident": ([128, 128], BF16), "i8": ([8, 8], F32),
             "hsel": ([8, 8, 128], F32), "bmask": ([128, 128], BF16)}


def rb(g):
    i, p = g // 2, g % 2
    return (i // 2) * 4 + p * 2 + (i % 2)


def build_select(P, nc, sb, ps, hg, ho, pflag):
    fl = sb("fl", [128, 2], F32)
    P.add("sync", lambda e: e.dma_start(out=fl[:], in_=pflag), writes=[fl], dma=True)
    c0 = [sb("c0_%d" % i, [128, D], F32) for i in range(3)]
    c1 = [sb("c1_%d" % i, [128, D], F32) for i in range(3)]
    for j in range(8):
        a, b = c0[j % 3], c1[j % 3]
        P.add("sync", lambda e, a=a, j=j: e.dma_start(out=a[:], in_=hg[rb(j) * 128:(rb(j) + 1) * 128, :]), reads=[("hg", rb(j) // 4)], writes=[a], dma=True)
        P.add("sync", lambda e, b=b, j=j: e.dma_start(out=b[:], in_=hg[rb(8 + j) * 128:(rb(8 + j) + 1) * 128, :]), reads=[("hg", rb(8 + j) // 4)], writes=[b], dma=True)
        P.add("vector", lambda e, a=a: e.tensor_scalar(out=a[:], in0=a[:], scalar1=fl[:, 0:1], scalar2=None, op0=ALU.mult), reads=[a, fl], writes=[a])
        P.add("vector", lambda e, a=a, b=b: e.scalar_tensor_tensor(out=b[:], in0=b[:], scalar=fl[:, 1:2], in1=a[:], op0=ALU.mult, op1=ALU.add), reads=[a, b, fl], writes=[b])
        P.add("sync", lambda e, b=b, j=j: e.dma_start(out=ho[j * 128:(j + 1) * 128, :], in_=b[:]), reads=[b], writes=[("ho", j)], dma=True)


IN_SHAPES = {
    "xk": ([2048, D], F32), "xq": ([1024, D], F32), "pflag": ([128, 2], F32),
    "norm_mix": ([2, D], F32), "norm_ffn": ([2, D], F32),
    "att_w_in": ([D, 4176], F32), "att_q_gain": ([128], F32), "att_k_gain": ([128], F32), "att_w_out": ([D, D], F32),
    "ml_w_in": ([D, 6160], F32), "ml_b_gate": ([16], F32), "ml_h_gain": ([256], F32), "ml_w_out": ([D, D], F32),
    "ffn_w_up": ([2, D, DFF], F32), "ffn_w_down": ([2, DFF, D], F32),
    "cos_k": ([128, 16, 16], F32), "sin_k": ([128, 16, 16], F32), "cos_ki": ([128, 16, 8], F32), "sin_ki": ([128, 16, 8], F32),
    "cos_q": ([128, 8, 16], F32), "sin_q": ([128, 8, 16], F32), "cos_qi": ([128, 8, 8], F32), "sin_qi": ([128, 8, 8], F32),
    "cbias": ([128, 256], F32), "ident": ([128, 128], BF16), "i8": ([8, 8], F32), "hsel": ([8, 8, 128], F32), "bmask": ([128, 128], BF16),
}
PAIRS = [[0, 1], [2, 3], [4, 5], [6, 7]]


SCHED_ATT = True
SCHED_ML = True


def build_fused(use_cc=True):
    nc = bass.Bass("TRN2", target_bir_lowering=False)
    di = {k: nc.dram_tensor(k, s, dt, kind="ExternalInput").ap() for k, (s, dt) in IN_SHAPES.items()}
    out = nc.dram_tensor("out", [1024, D], F32, kind="ExternalOutput").ap()
    h_a = nc.dram_tensor("h_a_i", [1024, D], F32).ap()
    h_0 = nc.dram_tensor("h_0_i", [1024, D], F32).ap()
    hg = nc.dram_tensor("hg_i", [2048, D], F32).ap()
    hp = nc.dram_tensor("hp_i", [1024, D], F32).ap()
    ho = nc.dram_tensor("ho_i", [1024, D], F32).ap()
    h_m = nc.dram_tensor("h_m_i", [1024, D], F32).ap()
    with ExitStack() as st:
        P = Prog(nc, st, schedule=SCHED_ATT)
        ar = Arena(nc, st)
        d = {k: di[k] for k in ("xk", "xq", "cos_k", "sin_k", "cos_ki", "sin_ki", "cos_q", "sin_q", "cos_qi", "sin_qi", "cbias", "ident")}
        d.update({"gmix": di["norm_mix"][0], "gq": di["att_q_gain"], "gk": di["att_k_gain"], "w_in": di["att_w_in"], "w_out": di["att_w_out"], "out": h_a})
        build_att(P, nc, ar.sb, ar.ps, d)
        P.barrier(schedule=False)
        ar.reset()
        build_ffn(P, nc, ar.sb, ar.ps, h_a, di["norm_ffn"][0], di["ffn_w_up"][0], di["ffn_w_down"][0], h_0, di["ident"], 1024)
        if use_cc:
            for j in range(4):
                P.add("gpsimd", lambda e, j=j: e.collective_compute("AllGather", ALU.bypass, replica_groups=PAIRS,
                                                                    ins=[h_0[j * 256:(j + 1) * 256, :]], outs=[hg[j * 512:(j + 1) * 512, :]]),
                      reads=[("out", j * 256 + t * 128, db) for t in range(2) for db in range(4)], writes=[("hg", j)], cc=True)
        else:
            allout = [("out", t * 128, db) for t in range(8) for db in range(4)]
            P.add("sync", lambda e: e.dma_start(out=hg[0:1024, :], in_=h_0), reads=allout, writes=["hg"], dma=True)
            P.add("sync", lambda e: e.dma_start(out=hg[1024:2048, :], in_=h_0), reads=allout, writes=["hg2"], dma=True)
        P.barrier(schedule=False)
        ar.reset()
        build_select(P, nc, ar.sb, ar.ps, hg, ho, di["pflag"])
        P.barrier(schedule=SCHED_ML)
        ar.reset()
        d = {"hp_tile": (lambda t: hg[rb(t) * 128:(rb(t) + 1) * 128, :]), "pflag": di["pflag"], "ho": ho, "gmix": di["norm_mix"][1], "hgain": di["ml_h_gain"], "bgate": di["ml_b_gate"], "w_in": di["ml_w_in"],
             "w_out": di["ml_w_out"], "ident": di["ident"], "i8": di["i8"], "hsel": di["hsel"], "bmask": di["bmask"], "out": h_m}
        build_ml(P, nc, ar.sb, ar.ps, d)
        P.barrier(schedule=False)
        ar.reset()
        build_ffn(P, nc, ar.sb, ar.ps, h_m, di["norm_ffn"][1], di["ffn_w_up"][1], di["ffn_w_down"][1], out, di["ident"], 1024)
        P.wait_all_dma("sync")
        P.emit()
    return nc


def core_inputs(c, x, shared):
    b, p = c // 2, c % 2
    m = dict(shared)
    m["xk"] = x[b]
    m["xq"] = q_tiles(x[b], p)
    fl = np.zeros((128, 2), np.float32)
    fl[:, p] = 1.0
    m["pflag"] = fl
    m.update(att_consts(p))
    m.update(ml_consts())
    return m


_NC = {}


def kernel(x, norm_mix, norm_ffn, att_w_in, att_q_gain, att_k_gain, att_w_out,
           ml_w_in, ml_b_gate, ml_h_gain, ml_w_out, ffn_w_up, ffn_w_down):
    f32 = lambda a: np.ascontiguousarray(np.asarray(a, dtype=np.float32))
    x = f32(x)
    shared = {"norm_mix": f32(norm_mix), "norm_ffn": f32(norm_ffn), "att_w_in": f32(att_w_in)[0], "att_q_gain": f32(att_q_gain)[0],
              "att_k_gain": f32(att_k_gain)[0], "att_w_out": f32(att_w_out)[0], "ml_w_in": f32(ml_w_in)[0], "ml_b_gate": f32(ml_b_gate)[0],
              "ml_h_gain": f32(ml_h_gain)[0], "ml_w_out": f32(ml_w_out)[0], "ffn_w_up": f32(ffn_w_up), "ffn_w_down": f32(ffn_w_down)}
    if "nc" not in _NC:
        _NC["nc"] = build_fused()
    cores = list(range(8))
    in_maps = [core_inputs(c, x, shared) for c in cores]
    res = run_bass_kernel_spmd(_NC["nc"], in_maps, core_ids=cores)
    out = np.empty_like(x)
    for c in cores:
        b, p = c // 2, c % 2
        out[b, p * 1024:(p + 1) * 1024] = np.asarray(res.results[c]["out"])
    return out
```

```python
import sys, time
import numpy as np
from contextlib import ExitStack
import ml_dtypes
import concourse.bass as bass
import concourse.mybir as mybir
from concourse.bass_utils import run_bass_kernel_spmd


F32 = mybir.dt.float32
BF16 = mybir.dt.bfloat16
ALU = mybir.AluOpType
AF = mybir.ActivationFunctionType
AX = mybir.AxisListType

ENGS = ["tensor", "vector", "scalar", "gpsimd", "sync"]


class T:
    __slots__ = ("h", "name")

    def __init__(self, h, name):
        self.h = h
        self.name = name

    def __getitem__(self, k):
        return self.h[k]


class Op:
    __slots__ = ("eng", "fn", "deps", "odeps", "sig", "sem", "val", "dma", "idx", "cc", "region", "cost", "users", "tail", "nwait", "rt", "fin", "st", "prevdma")

    def __init__(self, eng, fn, dma):
        self.cc = False
        self.eng = eng
        self.fn = fn
        self.deps = set()
        self.odeps = set()
        self.sig = False
        self.sem = None
        self.val = 0
        self.dma = dma
        self.prevdma = None


DEF_COST = {"tensor": 0.25, "vector": 0.7, "scalar": 0.7, "gpsimd": 1.0, "sync": 0.1}
DMA_ISSUE = {"sync": 0.15, "gpsimd": 1.2}
DMA_LAT = 5.0
SYNC_LAT = 1.2


class Prog:
    def __init__(self, nc, stack, n_dma_sems=24, schedule=True):
        self.nc = nc
        self.stack = stack
        self.schedule = schedule
        self.regions = [[]]
        self.region_sched = [schedule]
        self.last_w = {}
        self.readers = {}
        self.engsem = {e: stack.enter_context(nc.semaphore("s_" + e)) for e in ENGS}
        self.dma_pool = {}
        for q in ("sync", "gpsimd"):
            self.dma_pool[q] = [stack.enter_context(nc.semaphore("d_%s%d" % (q, i))) for i in range(n_dma_sems)]
        self.n = 0
        self.final_wait = None

    def add(self, eng, fn, reads=(), writes=(), dma=False, cc=False, cost=None):
        op = Op(eng, fn, dma or cc)
        op.cc = cc
        op.idx = self.n
        self.n += 1
        op.region = len(self.regions) - 1
        op.cost = cost if cost is not None else DEF_COST[eng]
        deps = set()
        for r in reads:
            w = self.last_w.get(r)
            if w is not None:
                deps.add(w)
        for w_ in writes:
            lw = self.last_w.get(w_)
            if lw is not None:
                deps.add(lw)
            for rd in self.readers.get(w_, ()):
                deps.add(rd)
        if eng == "tensor" and not dma:
            for d_ in deps:
                if d_.eng == "tensor" and not d_.dma:
                    op.odeps.add(d_)
                else:
                    op.deps.add(d_)
        else:
            op.deps = deps
        if cc:
            op.sem = self.stack.enter_context(self.nc.semaphore("cc%d" % op.idx))
            op.val = 1
        for r in reads:
            self.readers.setdefault(r, []).append(op)
        for w_ in writes:
            self.last_w[w_] = op
            self.readers[w_] = []
        self.regions[-1].append(op)
        return op

    def barrier(self, schedule=None):
        self.regions.append([])
        self.region_sched.append(self.schedule if schedule is None else schedule)
        self.last_w = {}
        self.readers = {}

    def wait_all_dma(self, eng="sync"):
        self.final_wait = eng

    def _schedule(self, ops, do_sched):
        import heapq
        order = {e: [] for e in ENGS}
        if not do_sched:
            for op in ops:
                order[op.eng].append(op)
            return order
        for op in ops:
            op.users = []
            op.nwait = len(op.deps) + len(op.odeps)
            op.rt = 0.0
        for op in ops:
            for d_ in op.deps:
                d_.users.append((op, True))
            for d_ in op.odeps:
                d_.users.append((op, False))
        for op in reversed(ops):
            t = 0.0
            for u, sy in op.users:
                t = max(t, u.tail + (SYNC_LAT if sy else 0.0))
            op.tail = t + (DMA_LAT if op.dma else op.cost)
        pend = {e: [] for e in ENGS}
        avail = {e: [] for e in ENGS}
        tfree = {e: 0.0 for e in ENGS}
        for op in ops:
            if op.nwait == 0:
                heapq.heappush(pend[op.eng], (0.0, op.idx, op))
        left = len(ops)
        while left:
            best = None
            for e in ENGS:
                pe, av = pend[e], avail[e]
                t = tfree[e]
                while pe and pe[0][0] <= t:
                    _, _, o = heapq.heappop(pe)
                    heapq.heappush(av, (-o.tail, o.idx, o))
                if av:
                    cand = (t, av[0][0], e, True)
                elif pe:
                    cand = (pe[0][0], -pe[0][2].tail, e, False)
                else:
                    continue
                if best is None or cand[:2] < best[:2]:
                    best = cand
            start, _, e, from_av = best
            if from_av:
                _, _, op = heapq.heappop(avail[e])
            else:
                _, _, op = heapq.heappop(pend[e])
            order[e].append(op)
            left -= 1
            if op.dma:
                busy = DMA_ISSUE.get(e, 0.2)
                fin = start + busy + DMA_LAT
            else:
                busy = op.cost
                fin = start + busy
            tfree[e] = start + busy
            for u, sy in op.users:
                r = fin + SYNC_LAT if sy else start
                if r > u.rt:
                    u.rt = r
                u.nwait -= 1
                if u.nwait == 0:
                    heapq.heappush(pend[u.eng], (u.rt, u.idx, u))
        return order

    def emit(self):
        nc = self.nc
        queues = {e: [] for e in ENGS}
        bar_deps = []
        for ri, ops in enumerate(self.regions):
            order = self._schedule(ops, self.region_sched[ri])
            lasts = set()
            for e in ENGS:
                if order[e]:
                    for op in reversed(order[e]):
                        if not op.dma and op.fn is not None:
                            lasts.add(op)
                            break
                for op in order[e]:
                    if op.dma:
                        lasts.add(op)
            bar_deps.append(lasts)
            for e in ENGS:
                first = True
                for op in order[e]:
                    if first and len(bar_deps) > 1:
                        op.deps = set(op.deps) | bar_deps[-2]
                    first = False
                    queues[e].append(op)
        if self.final_wait is not None:
            op = Op(self.final_wait, None, False)
            op.deps = set(o for r in self.regions for o in r if o.dma)
            queues[self.final_wait].append(op)
        for q, pool in self.dma_pool.items():
            k = 0
            hist = []
            for op in queues[q]:
                if op.dma and not op.cc:
                    op.sem = pool[k % len(pool)]
                    op.val = 16 * (k // len(pool) + 1)
                    if k >= len(pool):
                        op.prevdma = hist[k - len(pool)]
                    hist.append(op)
                    k += 1
        pos = {}
        for e in ENGS:
            for i, op in enumerate(queues[e]):
                pos[op] = i
        for e in ENGS:
            for op in queues[e]:
                best = {}
                nd = set()
                for d in op.deps:
                    if d.dma:
                        nd.add(d)
                    else:
                        b_ = best.get(d.eng)
                        if b_ is None or pos[d] > pos[b_]:
                            best[d.eng] = d
                for d in best.values():
                    d.sig = True
                    nd.add(d)
                op.deps = nd
        for e in ENGS:
            cnt = 0
            for op in queues[e]:
                if op.dma:
                    continue
                if op.sig:
                    cnt += 1
                    op.sem = self.engsem[e]
                    op.val = cnt
        self.queues = queues
        with nc.Block() as block:
            def mk(e):
                def body(eng):
                    waited = {}
                    for op in queues[e]:
                        need = {}
                        for d in op.deps:
                            if need.get(d.sem, 0) < d.val:
                                need[d.sem] = d.val
                        if op.prevdma is not None:
                            p_ = op.prevdma
                            if need.get(p_.sem, 0) < p_.val:
                                need[p_.sem] = p_.val
                        for s, v in need.items():
                            if waited.get(s, 0) < v:
                                eng.wait_ge(s, v)
                                waited[s] = v
                        if op.fn is None:
                            continue
                        ins = op.fn(eng)
                        if op.cc:
                            ins.then_inc(op.sem, 1)
                        elif op.dma:
                            ins.then_inc(op.sem, 16)
                        elif op.sig:
                            ins.then_inc(op.sem, 1)
                return body
            block.tensor(mk("tensor"))
            block.vector(mk("vector"))
            block.scalar(mk("scalar"))
            block.gpsimd(mk("gpsimd"))
            block.sync(mk("sync"))


SBW = 52000


class Arena:
    def __init__(self, nc, st):
        self.sb_t = st.enter_context(nc.sbuf_tensor("arena_sb", [128, SBW], F32))
        self.ps_t = st.enter_context(nc.psum_tensor("arena_ps", [128, 4096], F32))
        self.reset()

    def reset(self):
        self.off = 0
        self.pbank = 0

    @staticmethod
    def _shape(v, shape):
        if len(shape) == 2:
            return v
        if len(shape) == 3:
            return v.rearrange("p (a b) -> p a b", a=shape[1])
        if len(shape) == 4:
            return v.rearrange("p (a b c) -> p a b c", a=shape[1], b=shape[2])
        raise ValueError(shape)

    def sb(self, name, shape, dt):
        p = shape[0]
        n = int(np.prod(shape[1:]))
        words = n if dt == F32 else (n + 1) // 2
        words = (words + 7) // 8 * 8
        assert self.off + words <= SBW, ("SBUF arena overflow", name, self.off, words)
        base = self.sb_t[0:p, self.off:self.off + words]
        self.off += words
        v = base if dt == F32 else base.bitcast(BF16)
        return T(self._shape(v[:, 0:n], shape), name)

    def ps(self, name, shape, dt):
        p = shape[0]
        n = int(np.prod(shape[1:]))
        nbytes = n * (4 if dt == F32 else 2)
        banks = (nbytes + 2047) // 2048
        assert self.pbank + banks <= 8, ("PSUM arena overflow", name)
        base = self.ps_t[0:p, self.pbank * 512:(self.pbank + banks) * 512]
        self.pbank += banks
        v = base if dt == F32 else base.bitcast(BF16)
        return T(self._shape(v[:, 0:n], shape), name)


D = 2048
EPS = 1e-6
NEG = -1.0e30
REPL = -3.0e38


def merge_gens(gens):
    gens = [g for g in gens if g is not None]
    while gens:
        for g in list(gens):
            try:
                next(g)
            except StopIteration:
                gens.remove(g)


def build_att(P, nc, sb, ps, d):
    def V(fn, r, w):
        return P.add("vector", fn, reads=r, writes=w)

    def A(fn, r, w):
        return P.add("scalar", fn, reads=r, writes=w)

    def G(fn, r, w):
        return P.add("gpsimd", fn, reads=r, writes=w)

    def TE(fn, r, w):
        return P.add("tensor", fn, reads=r, writes=w)

    def LD(t, src, q="sync", key=None):
        return P.add(q, lambda e: e.dma_start(out=t[:] if key is None else key[1], in_=src), writes=[t if key is None else key[0]], dma=True)

    ident = sb("ident", [128, 128], BF16)
    LD(ident, d["ident"])
    epsT = sb("epsT", [128, 1], F32)
    V(lambda e: e.memset(epsT[:], EPS), [], [epsT])
    g_bc = sb("g_bc", [128, D], F32)
    LD(g_bc, d["gmix"].partition_broadcast(128))
    gq_bc = sb("gq_bc", [128, 128], F32)
    LD(gq_bc, d["gq"].partition_broadcast(128))
    gk_bc = sb("gk_bc", [128, 128], F32)
    LD(gk_bc, d["gk"].partition_broadcast(128))
    tabs = {}
    for nm, shp in (("cos_k", [128, 16, 16]), ("sin_k", [128, 16, 16]), ("cos_ki", [128, 16, 8]), ("sin_ki", [128, 16, 8]),
                    ("cos_q", [128, 8, 16]), ("sin_q", [128, 8, 16]), ("cos_qi", [128, 8, 8]), ("sin_qi", [128, 8, 8])):
        tabs[nm] = sb(nm, shp, F32)
        LD(tabs[nm], d[nm])
    cbias = sb("cbias", [128, 256], F32)
    LD(cbias, d["cbias"])

    NW = 2
    wt = [sb("wt%d" % i, [128, 16, 512], BF16) for i in range(NW)]
    wki = sb("wki", [128, 16, 80], BF16)
    xt = [sb("xt%d" % i, [128, D], F32) for i in range(2)]
    hb = [sb("hb%d" % i, [128, D], BF16) for i in range(2)]
    ss = [sb("ss%d" % i, [128, 1], F32) for i in range(2)]
    rstd = [sb("rstd%d" % i, [128, 1], F32) for i in range(2)]
    hnT = sb("hnT", [128, 16, 512], BF16)
    kT = sb("kT", [128, 4, 2048], BF16)
    Vt = sb("Vt", [128, 16, 4, 128], BF16)
    ones_bf = sb("ones_bf", [128, 128], BF16)
    rsum = sb("rsum", [128, 512], F32)
    kiT = sb("kiT", [128, 2048], BF16)
    qT = sb("qT", [128, 16, 512], BF16)
    qiT = sb("qiT", [128, 8, 512], BF16)
    wi_sb = sb("wi_sb", [128, 4, 16], F32)
    score = sb("score", [128, 2048], F32)
    work = sb("work", [128, 2048], F32)
    mask = sb("mask", [128, 2048], BF16)
    maskT = sb("maskT", [128, 16, 128], BF16)
    scoreb = [score, work]
    bis = [[sb("bis%d_%d" % (k, j), [128, 1], F32) for j in range(6)] for k in range(2)]
    stepsb = [sb("steps%d" % k, [128, 24], F32) for k in range(2)]
    ckrow = sb("ckrow", [128, 24], F32)
    for k_ in range(24):
        V(lambda e, k_=k_: e.memset(ckrow[:, k_:k_ + 1], 2.0 ** -(k_ + 1)), [], [ckrow])
    tmpA = [sb("tmpA%d" % i, [128, 512], F32) for i in range(2)]
    sqt = sb("sqt", [128, 512], F32)
    osb = sqt
    st1 = sb("st1", [128, 8], F32)
    st2 = sb("st2", [128, 8], F32)
    rtmp = [sb("rtmp%d" % i, [128, 64], F32) for i in range(4)]
    ob = [sb("ob%d" % i, [128, 512], BF16) for i in range(2)]
    relu_t = [sb("relu%d" % i, [128, 512], F32) for i in range(2)]
    pT = [sb("pT%d" % i, [128, 512], BF16) for i in range(3)]
    ot = [sb("ot%d" % i, [128, 512], F32) for i in range(2)]
    xres = [sb("xres%d" % i, [128, 512], F32) for i in range(2)]
    ps_tr = [ps("tr%d" % i, [128, 4, 128], BF16) for i in range(2)]
    ps_mm = [ps("mm%d" % i, [128, 512], F32) for i in range(2)]
    ps_s = [ps("s%d" % i, [128, 512], F32) for i in range(2)]
    ps_oa = ps("oa", [128, 512], F32)
    ps_os = ps("os", [128, 512], F32)

    cnt = {"w": 0, "x": 0, "tr": 0, "mm": 0, "tA": 0, "ob": 0, "relu": 0, "p": 0, "s": 0, "ot": 0}

    def nxt(k, n=2):
        v = cnt[k] % n
        cnt[k] += 1
        return v

    def load_w(dram, r0, c0, ncols=512, dst=None):
        w = dst if dst is not None else wt[nxt("w", NW)]
        for q in range(4):
            src = dram[r0 + q * 512:r0 + (q + 1) * 512, c0:c0 + ncols].rearrange("(c p) n -> p c n", p=128)
            P.add("gpsimd", lambda e, w=w, q=q, src=src: e.dma_start(out=w[:, q * 4:(q + 1) * 4, 0:ncols], in_=src),
                  writes=[(w, q)], dma=True)
        return w

    def wres(w):
        return [(w, q) for q in range(4)]

    def norm_tile(src_ap):
        k = nxt("x")
        x_, h_, s_, r_ = xt[k], hb[k], ss[k], rstd[k]
        P.add("sync", lambda e: e.dma_start(out=x_[:], in_=src_ap), writes=[x_], dma=True)
        A(lambda e: e.activation(out=h_[:], in_=x_[:], func=AF.Square, accum_out=s_[:]), [x_], [h_, s_])
        A(lambda e: e.activation(out=s_[:], in_=s_[:], func=AF.Sqrt, bias=epsT[:], scale=1.0 / D), [s_, epsT], [s_])
        V(lambda e: e.reciprocal(out=r_[:], in_=s_[:]), [s_], [r_])
        V(lambda e: e.scalar_tensor_tensor(out=h_[:], in0=x_[:], scalar=r_[:], in1=g_bc[:], op0=ALU.mult, op1=ALU.mult),
          [x_, r_, g_bc], [h_])
        return h_

    def tr16(h_, dstT, col0, dkey):
        for j in range(4):
            pt = ps_tr[nxt("tr")]
            for k in range(4):
                dc = j * 4 + k
                TE(lambda e, pt=pt, k=k, dc=dc: e.transpose(out=pt[:, k, :], in_=h_[:, dc * 128:(dc + 1) * 128], identity=ident[:]),
                   [h_, ident], [pt])
            if j % 2 == 0:
                A(lambda e, pt=pt, j=j: e.copy(out=dstT[:, j * 4:(j + 1) * 4, col0:col0 + 128], in_=pt[:]), [pt], [(dstT, dkey, j)])
            else:
                V(lambda e, pt=pt, j=j: e.tensor_copy(out=dstT[:, j * 4:(j + 1) * 4, col0:col0 + 128], in_=pt[:]), [pt], [(dstT, dkey, j)])

    def tres(dstT, dkey):
        return [(dstT, dkey, j) for j in range(4)]

    def proj(pm, ncols, i, w, c_lo):
        for dc in range(16):
            TE(lambda e, dc=dc: e.matmul(pm[:, 0:ncols], lhsT=hnT[:, dc, i * 128:(i + 1) * 128], rhs=w[:, dc, c_lo:c_lo + ncols],
                                         start=(dc == 0), stop=(dc == 15)),
               tres(hnT, i) + [(w, dc // 4)], [pm])

    def normrope(pm, nh, hd, hf, gain, cos_ap, sin_ap, tabres, out, dup=False):
        n = nh * hd
        tA = tmpA[nxt("tA")]
        tA3 = tA[:, 0:n].rearrange("p (a b) -> p a b", a=nh)
        A(lambda e: e.copy(out=tA[:, 0:n], in_=pm[:, 0:n]), [pm], [tA])
        if gain is not None:
            V(lambda e: e.tensor_tensor(out=sqt[:, 0:n], in0=tA[:, 0:n], in1=tA[:, 0:n], op=ALU.mult), [tA], [sqt])
            V(lambda e: e.reduce_sum(out=st1[:, 0:nh], in_=sqt[:, 0:n].rearrange("p (a b) -> p a b", a=nh), axis=AX.X), [sqt], [st1])
            A(lambda e: e.activation(out=st1[:, 0:nh], in_=st1[:, 0:nh], func=AF.Sqrt, bias=epsT[:], scale=1.0 / hd), [st1, epsT], [st1])
            V(lambda e: e.reciprocal(out=st2[:, 0:nh], in_=st1[:, 0:nh]), [st1], [st2])
            V(lambda e: e.tensor_tensor(out=tA3, in0=tA3, in1=st2[:, 0:nh].unsqueeze(2).to_broadcast([128, nh, hd]), op=ALU.mult), [tA, st2], [tA])
            V(lambda e: e.tensor_tensor(out=tA3, in0=tA3, in1=gain[:, 0:hd].unsqueeze(1).to_broadcast([128, nh, hd]), op=ALU.mult), [tA, gain], [tA])
        x1 = tA3[:, :, 0:hf]
        x2 = tA3[:, :, hf:2 * hf]
        cb = cos_ap.unsqueeze(1).to_broadcast([128, nh, hf])
        sn = sin_ap.unsqueeze(1).to_broadcast([128, nh, hf])
        r3 = [r[:, 0:nh * hf].rearrange("p (a b) -> p a b", a=nh) for r in rtmp]
        G(lambda e: e.tensor_tensor(out=r3[0], in0=x1, in1=cb, op=ALU.mult), [tA] + tabres, [rtmp[0]])
        G(lambda e: e.tensor_tensor(out=r3[1], in0=x2, in1=sn, op=ALU.mult), [tA] + tabres, [rtmp[1]])
        G(lambda e: e.tensor_tensor(out=r3[2], in0=x1, in1=sn, op=ALU.mult), [tA] + tabres, [rtmp[2]])
        G(lambda e: e.tensor_tensor(out=r3[3], in0=x2, in1=cb, op=ALU.mult), [tA] + tabres, [rtmp[3]])
        reps = 2 if dup else 1
        for rp in range(reps):
            o3 = out[:, rp * n:(rp + 1) * n].rearrange("p (a b) -> p a b", a=nh)
            G(lambda e, o3=o3: e.tensor_tensor(out=o3[:, :, 0:hf], in0=r3[0], in1=r3[1], op=ALU.subtract), [rtmp[0], rtmp[1]], [(out, "a", rp)])
            G(lambda e, o3=o3: e.tensor_tensor(out=o3[:, :, hf:2 * hf], in0=r3[2], in1=r3[3], op=ALU.add), [rtmp[2], rtmp[3]], [(out, "b", rp)])
            A(lambda e, o3=o3: e.copy(out=o3[:, :, 2 * hf:hd], in_=tA3[:, :, 2 * hf:hd]), [tA], [(out, "c", rp)])

    def obres(out, dup=False):
        return [(out, k, rp) for k in "abc" for rp in range(2 if dup else 1)]

    V(lambda e: e.memset(ones_bf[:], 1.0), [], [ones_bf])
    w_in = d["w_in"]
    wk = load_w(w_in, 0, 2048)
    wv = load_w(w_in, 0, 2560)
    load_w(w_in, 0, 4096, ncols=80, dst=wki)
    for kb in range(4):
        for i in range(4):
            tI = kb * 4 + i
            h_ = norm_tile(d["xk"][tI * 128:(tI + 1) * 128, :])
            tr16(h_, hnT, i * 128, i)
        for i in range(4):
            tI = kb * 4 + i
            pm = ps_mm[nxt("mm")]
            proj(pm, 512, i, wk, 0)
            o_ = ob[nxt("ob")]
            normrope(pm, 4, 128, 16, gk_bc, tabs["cos_k"][:, tI, :], tabs["sin_k"][:, tI, :], [tabs["cos_k"], tabs["sin_k"]], o_)
            pt = ps_tr[nxt("tr")]
            for h in range(4):
                TE(lambda e, pt=pt, h=h, o_=o_: e.transpose(out=pt[:, h, :], in_=o_[:, h * 128:(h + 1) * 128], identity=ident[:]),
                   obres(o_) + [ident], [pt])
            V(lambda e, pt=pt, tI=tI: e.tensor_copy(out=kT[:, :, tI * 128:(tI + 1) * 128], in_=pt[:]), [pt], [(kT, tI)])
            pm = ps_mm[nxt("mm")]
            proj(pm, 512, i, wv, 0)
            A(lambda e, pm=pm, tI=tI: e.copy(out=Vt[:, tI, :, :], in_=pm[:].rearrange("p (a b) -> p a b", a=4)), [pm], [(Vt, tI)])
            pm = ps_mm[nxt("mm")]
            proj(pm, 64, i, wki, 0)
            o_ = ob[nxt("ob")]
            normrope(pm, 1, 64, 8, None, tabs["cos_ki"][:, tI, :], tabs["sin_ki"][:, tI, :], [tabs["cos_ki"], tabs["sin_ki"]], o_, dup=True)
            pt = ps_tr[nxt("tr")]
            TE(lambda e, pt=pt, o_=o_: e.transpose(out=pt[:, 0, :], in_=o_[:, 0:128], identity=ident[:]), obres(o_, True) + [ident], [pt])
            V(lambda e, pt=pt, tI=tI: e.tensor_copy(out=kiT[:, tI * 128:(tI + 1) * 128], in_=pt[:, 0, :]), [pt], [(kiT, tI)])

    SCALE = 128 ** -0.5

    KBIS = 22

    def index_steps(half, i):
        s_ = half * 4 + i
        nkt = 2 * s_ + 2
        S = nkt * 128
        nch = (S + 511) // 512
        sc = scoreb[s_ % 2]
        lo, Wd, nmid, cntt, gg, rmx = bis[s_ % 2]
        steps = stepsb[s_ % 2]
        for h in range(16):
            pair = h // 2
            off = (h % 2) * 64
            for c in range(nch):
                cols = min(512, S - c * 512)
                pm = ps_mm[nxt("mm")]
                TE(lambda e, pm=pm, cols=cols, c=c, off=off, pair=pair: e.matmul(pm[:, 0:cols], lhsT=qiT[off:off + 64, pair, i * 128:(i + 1) * 128],
                                                                                 rhs=kiT[off:off + 64, c * 512:c * 512 + cols], start=True, stop=True),
                   [(qiT, i, pair)] + [(kiT, t) for t in range(c * 4, min(c * 4 + 4, nkt))], [pm])
                rl = relu_t[nxt("relu")]
                A(lambda e, pm=pm, rl=rl, cols=cols: e.activation(out=rl[:, 0:cols], in_=pm[:, 0:cols], func=AF.Relu), [pm], [rl])
                if h == 0:
                    V(lambda e, rl=rl, cols=cols, c=c: e.tensor_scalar(out=sc[:, c * 512:c * 512 + cols], in0=rl[:, 0:cols], scalar1=wi_sb[:, i, 0:1], scalar2=None, op0=ALU.mult),
                      [rl, (wi_sb, i)], [(sc, c)])
                else:
                    V(lambda e, rl=rl, cols=cols, c=c, h=h: e.scalar_tensor_tensor(out=sc[:, c * 512:c * 512 + cols], in0=rl[:, 0:cols], scalar=wi_sb[:, i, h:h + 1],
                                                                                 in1=sc[:, c * 512:c * 512 + cols], op0=ALU.mult, op1=ALU.add),
                      [rl, (wi_sb, i), (sc, c)], [(sc, c)])
                yield
        sres = [(sc, c) for c in range(4)]
        V(lambda e: e.reduce_max(out=rmx[:], in_=sc[:, 0:S], axis=AX.X), sres, [rmx])
        V(lambda e: e.tensor_reduce(out=lo[:], in_=sc[:, 0:S], axis=AX.X, op=ALU.min), sres, [lo])
        V(lambda e: e.tensor_tensor(out=Wd[:], in0=rmx[:], in1=lo[:], op=ALU.subtract), [rmx, lo], [Wd])
        V(lambda e: e.tensor_scalar(out=steps[:], in0=ckrow[:], scalar1=Wd[:, 0:1], scalar2=None, op0=ALU.mult), [ckrow, Wd], [steps])
        V(lambda e: e.scalar_tensor_tensor(out=nmid[:], in0=lo[:], scalar=-1.0, in1=steps[:, 0:1], op0=ALU.mult, op1=ALU.subtract), [lo, steps], [nmid])
        V(lambda e: e.tensor_tensor(out=sc[:, S - 256:S], in0=sc[:, S - 256:S], in1=cbias[:], op=ALU.add), sres + [cbias], sres)
        yield
        for k in range(KBIS):
            A(lambda e: e.activation(out=mask[:, 0:S], in_=sc[:, 0:S], func=AF.Sign, bias=nmid[:], scale=1.0, accum_out=cntt[:]), sres + [nmid], [mask, cntt])
            V(lambda e: e.tensor_scalar(out=gg[:], in0=cntt[:], scalar1=510.5 - S, scalar2=0.5, op0=ALU.is_lt, op1=ALU.subtract), [cntt], [gg])
            V(lambda e, k=k: e.scalar_tensor_tensor(out=nmid[:], in0=gg[:], scalar=steps[:, k:k + 1], in1=nmid[:], op0=ALU.mult, op1=ALU.add), [gg, steps, nmid], [nmid])
            yield
        V(lambda e: e.scalar_tensor_tensor(out=lo[:], in0=steps[:, KBIS - 1:KBIS], scalar=-0.5, in1=nmid[:], op0=ALU.mult, op1=ALU.subtract), [steps, nmid], [lo])
        V(lambda e: e.tensor_scalar(out=mask[:, 0:S], in0=sc[:, 0:S], scalar1=lo[:, 0:1], scalar2=None, op0=ALU.is_ge), sres + [lo], [mask])
        yield
        for j0 in range(0, nkt, 4):
            pt = ps_tr[nxt("tr")]
            nj = min(4, nkt - j0)
            for jj in range(nj):
                j = j0 + jj
                TE(lambda e, pt=pt, jj=jj, j=j: e.transpose(out=pt[:, jj, :], in_=mask[:, j * 128:(j + 1) * 128], identity=ident[:]), [mask, ident], [pt])
            V(lambda e, pt=pt, j0=j0, nj=nj: e.tensor_scalar(out=maskT[:, j0:j0 + nj, :], in0=pt[:, 0:nj, :], scalar1=1.0, scalar2=30000.0, op0=ALU.subtract, op1=ALU.mult),
              [pt], [(maskT, j0 // 4)])
            yield

    def attn_steps(half, i):
        s_ = half * 4 + i
        nkt = 2 * s_ + 2
        for g in range(4):
            for j in range(nkt):
                pS = ps_s[nxt("s")]
                TE(lambda e, pS=pS, g=g, j=j: e.matmul(pS[:].rearrange("p (a b) -> p a b", a=4), lhsT=kT[:, g, j * 128:(j + 1) * 128],
                                                       rhs=qT[:, 4 * g:4 * g + 4, i * 128:(i + 1) * 128], start=True, stop=False),
                   [(kT, j), (qT, i, g)], [pS])
                TE(lambda e, pS=pS, j=j: e.matmul(pS[:].rearrange("p (a b) -> p a b", a=4), lhsT=ident[:],
                                                  rhs=maskT[:, j, :].unsqueeze(1).to_broadcast([128, 4, 128]), start=False, stop=True),
                   [ident, (maskT, j // 4)], [pS])
                pm_ = pT[nxt("p", 3)]
                A(lambda e, pS=pS, pm_=pm_: e.activation(out=pm_[:], in_=pS[:], func=AF.Exp, scale=SCALE), [pS], [pm_])
                TE(lambda e, pm_=pm_, j=j, g=g: e.matmul(ps_oa[:], lhsT=Vt[:, j, g, :], rhs=pm_[:], start=(j == 0), stop=(j == nkt - 1)),
                   [pm_, (Vt, j)], [ps_oa])
                TE(lambda e, pm_=pm_, j=j: e.matmul(ps_os[:], lhsT=ones_bf[:], rhs=pm_[:], start=(j == 0), stop=(j == nkt - 1)),
                   [pm_, ones_bf], [ps_os])
                yield
            V(lambda e: e.reciprocal(out=rsum[:], in_=ps_os[:]), [ps_os], [rsum])
            V(lambda e, g=g: e.tensor_tensor(out=hnT[:, 4 * g:4 * g + 4, i * 128:(i + 1) * 128], in0=ps_oa[:].rearrange("p (a b) -> p a b", a=4),
                                             in1=rsum[:].rearrange("p (a b) -> p a b", a=4), op=ALU.mult),
              [ps_oa, rsum], [(hnT, i, g)])
            yield

    w_out = d["w_out"]
    for half in range(2):
        for i in range(4):
            s_ = half * 4 + i
            h_ = norm_tile(d["xq"][s_ * 128:(s_ + 1) * 128, :])
            tr16(h_, hnT, i * 128, i)
        for u in range(4):
            w = load_w(w_in, 0, u * 512)
            for i in range(4):
                s_ = half * 4 + i
                pm = ps_mm[nxt("mm")]
                proj(pm, 512, i, w, 0)
                o_ = ob[nxt("ob")]
                normrope(pm, 4, 128, 16, gq_bc, tabs["cos_q"][:, s_, :], tabs["sin_q"][:, s_, :], [tabs["cos_q"], tabs["sin_q"]], o_)
                pt = ps_tr[nxt("tr")]
                for h in range(4):
                    TE(lambda e, pt=pt, h=h, o_=o_: e.transpose(out=pt[:, h, :], in_=o_[:, h * 128:(h + 1) * 128], identity=ident[:]),
                       obres(o_) + [ident], [pt])
                V(lambda e, pt=pt, u=u, i=i: e.tensor_copy(out=qT[:, 4 * u:4 * u + 4, i * 128:(i + 1) * 128], in_=pt[:]), [pt], [(qT, i, u)])
        for u in range(2):
            w = load_w(w_in, 0, 3072 + u * 512)
            for i in range(4):
                s_ = half * 4 + i
                pm = ps_mm[nxt("mm")]
                proj(pm, 512, i, w, 0)
                o_ = ob[nxt("ob")]
                normrope(pm, 8, 64, 8, None, tabs["cos_qi"][:, s_, :], tabs["sin_qi"][:, s_, :], [tabs["cos_qi"], tabs["sin_qi"]], o_)
                pt = ps_tr[nxt("tr")]
                for pr in range(4):
                    TE(lambda e, pt=pt, pr=pr, o_=o_: e.transpose(out=pt[:, pr, :], in_=o_[:, pr * 128:(pr + 1) * 128], identity=ident[:]),
                       obres(o_) + [ident], [pt])
                V(lambda e, pt=pt, u=u, i=i: e.tensor_copy(out=qiT[:, 4 * u:4 * u + 4, i * 128:(i + 1) * 128], in_=pt[:]), [pt],
                  [(qiT, i, 4 * u + k) for k in range(4)])
        for i in range(4):
            pm = ps_mm[nxt("mm")]
            proj(pm, 16, i, wki, 64)
            V(lambda e, pm=pm, i=i: e.tensor_scalar(out=wi_sb[:, i, :], in0=pm[:, 0:16], scalar1=1.0 / 32.0, scalar2=None, op0=ALU.mult), [pm], [(wi_sb, i)])
        prev = None
        for i in range(4):
            merge_gens([index_steps(half, i), prev])
            prev = attn_steps(half, i)
        merge_gens([prev])
        for u in range(4):
            w = load_w(w_out, 0, u * 512)
            for i in range(4):
                s_ = half * 4 + i
                pm = ps_mm[nxt("mm")]
                proj(pm, 512, i, w, 0)
                k = nxt("ot")
                xr, o = xres[k], ot[k]
                P.add("sync", lambda e, xr=xr, s_=s_, u=u: e.dma_start(out=xr[:], in_=d["xq"][s_ * 128:(s_ + 1) * 128, u * 512:(u + 1) * 512]), writes=[xr], dma=True)
                V(lambda e, pm=pm, xr=xr, o=o: e.tensor_tensor(out=o[:], in0=pm[:], in1=xr[:], op=ALU.add), [pm, xr], [o])
                P.add("sync", lambda e, o=o, s_=s_, u=u: e.dma_start(out=d["out"][s_ * 128:(s_ + 1) * 128, u * 512:(u + 1) * 512], in_=o[:]),
                      reads=[o], writes=[("out", s_, u)], dma=True)


def rope_tables(pos, rot):
    half = rot // 2
    inv = 500000.0 ** (-2.0 * np.arange(half, dtype=np.float32) / rot)
    ang = pos.astype(np.float32)[:, None] * inv[None, :].astype(np.float32)
    return np.cos(ang).astype(np.float32), np.sin(ang).astype(np.float32)


def att_consts(p):
    import ml_dtypes
    c = {}
    kpos = np.arange(2048)
    ck, sk = rope_tables(kpos, 32)
    c["cos_k"] = np.ascontiguousarray(ck.reshape(16, 128, 16).transpose(1, 0, 2))
    c["sin_k"] = np.ascontiguousarray(sk.reshape(16, 128, 16).transpose(1, 0, 2))
    ck, sk = rope_tables(kpos, 16)
    c["cos_ki"] = np.ascontiguousarray(ck.reshape(16, 128, 8).transpose(1, 0, 2))
    c["sin_ki"] = np.ascontiguousarray(sk.reshape(16, 128, 8).transpose(1, 0, 2))
    qpos = np.concatenate([np.arange(128) + (2 * i + p) * 128 for i in range(8)])
    cq, sq = rope_tables(qpos, 32)
    c["cos_q"] = np.ascontiguousarray(cq.reshape(8, 128, 16).transpose(1, 0, 2))
    c["sin_q"] = np.ascontiguousarray(sq.reshape(8, 128, 16).transpose(1, 0, 2))
    cq, sq = rope_tables(qpos, 16)
    c["cos_qi"] = np.ascontiguousarray(cq.reshape(8, 128, 8).transpose(1, 0, 2))
    c["sin_qi"] = np.ascontiguousarray(sq.reshape(8, 128, 8).transpose(1, 0, 2))
    t = np.arange(128)[:, None]
    col = np.arange(256)[None, :]
    c["cbias"] = np.where(col <= t + 128 * p, 0.0, NEG).astype(np.float32)
    c["ident"] = np.eye(128).astype(ml_dtypes.bfloat16)
    return c


ATT_SHAPES = {"xk": ([2048, D], F32), "xq": ([1024, D], F32), "gmix": ([D], F32), "gq": ([128], F32), "gk": ([128], F32),
              "w_in": ([D, 4176], F32), "w_out": ([D, D], F32),
              "cos_k": ([128, 16, 16], F32), "sin_k": ([128, 16, 16], F32), "cos_ki": ([128, 16, 8], F32), "sin_ki": ([128, 16, 8], F32),
              "cos_q": ([128, 8, 16], F32), "sin_q": ([128, 8, 16], F32), "cos_qi": ([128, 8, 8], F32), "sin_qi": ([128, 8, 8], F32),
              "cbias": ([128, 256], F32), "ident": ([128, 128], BF16)}


def q_tiles(x_b, p):
    return np.concatenate([x_b[(2 * i + p) * 128:(2 * i + p + 1) * 128] for i in range(8)], axis=0)


D = 2048
DFF = 8192
EPS = 1e-6


def build_ffn(P, nc, sb, ps, h_dram, g_dram, wu_dram, wd_dram, out_dram, ident_dram, ntok):
    NB = ntok // 512
    ident = sb("ident", [128, 128], BF16)
    g_bc = sb("g_bc", [128, D], F32)
    h_t = [sb("h_t%d" % i, [128, D], F32) for i in range(4)]
    hn = [sb("hn%d" % i, [128, D], BF16) for i in range(2)]
    sq = sb("sq", [128, D], BF16)
    ss = [sb("ss%d" % i, [128, 1], F32) for i in range(2)]
    rstd = [sb("rstd%d" % i, [128, 1], F32) for i in range(2)]
    hnT = sb("hnT", [128, 16, 512], BF16)
    rT = sb("rT", [128, 64, 512], BF16)
    NW = 3
    wt = [sb("wt%d" % i, [128, 16, 512], BF16) for i in range(NW)]
    ot = [sb("ot%d" % i, [128, 512], F32) for i in range(2)]
    relu_t = [sb("relu%d" % i, [128, 512], F32) for i in range(2)]
    ps_tr = [ps("ps_tr%d" % i, [128, 4, 128], BF16) for i in range(2)]
    ps_u = [ps("ps_u%d" % i, [128, 512], F32) for i in range(2)]
    ps_d = [ps("ps_d%d" % i, [128, 512], F32) for i in range(4)]

    epsT = sb("epsT", [128, 1], F32)
    P.add("vector", lambda e: e.memset(epsT[:], EPS), writes=[epsT])
    P.add("sync", lambda e: e.dma_start(out=ident[:], in_=ident_dram), writes=[ident], dma=True)
    P.add("sync", lambda e: e.dma_start(out=g_bc[:], in_=g_dram.partition_broadcast(128)), writes=[g_bc], dma=True)

    wcnt = [0]

    def load_w(dram, r0, c0):
        i = wcnt[0] % NW
        wcnt[0] += 1
        w = wt[i]
        for q in range(4):
            src = dram[r0 + q * 512:r0 + (q + 1) * 512, c0:c0 + 512].rearrange("(c p) n -> p c n", p=128)
            P.add("gpsimd", lambda e, w=w, q=q, src=src: e.dma_start(out=w[:, q * 4:(q + 1) * 4, :], in_=src),
                  writes=[(w, q)], dma=True)
        return w

    trc = [0]
    uc = [0]
    oc = [0]
    for tb in range(NB):
        for i in range(4):
            t0 = tb * 512 + i * 128
            ht = h_t[i]
            P.add("sync", lambda e, ht=ht, t0=t0: e.dma_start(out=ht[:], in_=h_dram[t0:t0 + 128, :]), writes=[ht], dma=True)
            s_ = ss[i % 2]
            r_ = rstd[i % 2]
            hb = hn[i % 2]
            P.add("scalar", lambda e, ht=ht, s_=s_: e.activation(out=sq[:], in_=ht[:], func=AF.Square, accum_out=s_[:]),
                  reads=[ht], writes=[sq, s_])
            P.add("scalar", lambda e, s_=s_: e.activation(out=s_[:], in_=s_[:], func=AF.Sqrt, bias=epsT[:], scale=1.0 / D),
                  reads=[s_, epsT], writes=[s_])
            P.add("vector", lambda e, s_=s_, r_=r_: e.reciprocal(out=r_[:], in_=s_[:]),
                  reads=[s_], writes=[r_])
            P.add("vector", lambda e, ht=ht, r_=r_, hb=hb: e.scalar_tensor_tensor(out=hb[:], in0=ht[:], scalar=r_[:], in1=g_bc[:], op0=ALU.mult, op1=ALU.mult),
                  reads=[ht, r_, g_bc], writes=[hb])
            for j in range(4):
                pt = ps_tr[trc[0] % 2]
                trc[0] += 1
                for k in range(4):
                    dc = j * 4 + k
                    P.add("tensor", lambda e, pt=pt, k=k, hb=hb, dc=dc: e.transpose(out=pt[:, k, :], in_=hb[:, dc * 128:(dc + 1) * 128], identity=ident[:]),
                          reads=[hb, ident], writes=[pt])
                eng = "scalar" if (j % 2 == 0) else "vector"
                if eng == "scalar":
                    P.add("scalar", lambda e, pt=pt, j=j, i=i: e.copy(out=hnT[:, j * 4:(j + 1) * 4, i * 128:(i + 1) * 128], in_=pt[:]),
                          reads=[pt], writes=[(hnT, i, j)])
                else:
                    P.add("vector", lambda e, pt=pt, j=j, i=i: e.tensor_copy(out=hnT[:, j * 4:(j + 1) * 4, i * 128:(i + 1) * 128], in_=pt[:]),
                          reads=[pt], writes=[(hnT, i, j)])
        hnT_res = [(hnT, i, j) for i in range(4) for j in range(4)]
        for fg in range(16):
            w = load_w(wu_dram, 0, fg * 512)
            for fcl in range(4):
                fc = fg * 4 + fcl
                pu = ps_u[uc[0] % 2]
                uc[0] += 1
                for dc in range(16):
                    P.add("tensor", lambda e, pu=pu, w=w, fcl=fcl, dc=dc: e.matmul(pu[:], lhsT=w[:, dc, fcl * 128:(fcl + 1) * 128], rhs=hnT[:, dc, :], start=(dc == 0), stop=(dc == 15)),
                          reads=[(w, dc // 4)] + hnT_res, writes=[pu])
                ru = relu_t[uc[0] % 2]
                P.add("scalar", lambda e, pu=pu, ru=ru: e.activation(out=ru[:], in_=pu[:], func=AF.Relu),
                      reads=[pu], writes=[ru])
                P.add("vector", lambda e, ru=ru, fc=fc: e.tensor_tensor(out=rT[:, fc, :], in0=ru[:], in1=ru[:], op=ALU.mult),
                      reads=[ru], writes=[(rT, fc)])
        for db in range(4):
            for fq in range(4):
                w = load_w(wd_dram, fq * 2048, db * 512)
                for i in range(4):
                    for k in range(16):
                        fc = fq * 16 + k
                        P.add("tensor", lambda e, i=i, w=w, k=k, fc=fc, fq=fq: e.matmul(ps_d[i][:], lhsT=rT[:, fc, i * 128:(i + 1) * 128], rhs=w[:, k, :], start=(fq == 0 and k == 0), stop=(fq == 3 and k == 15)),
                              reads=[(w, k // 4), (rT, fc)], writes=[ps_d[i]])
            for i in range(4):
                o = ot[oc[0] % 2]
                oc[0] += 1
                t0 = tb * 512 + i * 128
                P.add("vector", lambda e, o=o, i=i, db=db: e.tensor_tensor(out=o[:], in0=ps_d[i][:], in1=h_t[i][:, db * 512:(db + 1) * 512], op=ALU.add),
                      reads=[ps_d[i], h_t[i]], writes=[o])
                P.add("sync", lambda e, o=o, t0=t0, db=db: e.dma_start(out=out_dram[t0:t0 + 128, db * 512:(db + 1) * 512], in_=o[:]),
                      reads=[o], writes=[("out", t0, db)], dma=True)


D = 2048
EPS = 1e-6


def build_ml(P, nc, sb, ps, d):
    def V(fn, r, w):
        return P.add("vector", fn, reads=r, writes=w)

    def A(fn, r, w):
        return P.add("scalar", fn, reads=r, writes=w)

    def G(fn, r, w):
        return P.add("gpsimd", fn, reads=r, writes=w)

    def TE(fn, r, w):
        return P.add("tensor", fn, reads=r, writes=w)

    def LD(t, src):
        return P.add("sync", lambda e: e.dma_start(out=t[:], in_=src), writes=[t], dma=True)

    ident = sb("ident", [128, 128], BF16)
    LD(ident, d["ident"])
    i8 = sb("i8", [8, 8], F32)
    LD(i8, d["i8"])
    hsel = sb("hsel", [8, 8, 128], F32)
    LD(hsel, d["hsel"])
    bmask = sb("bmask", [128, 128], BF16)
    LD(bmask, d["bmask"])
    epsT = sb("epsT", [128, 1], F32)
    V(lambda e: e.memset(epsT[:], EPS), [], [epsT])
    one8 = sb("one8", [8, 1], F32)
    V(lambda e: e.memset(one8[:], 1.0), [], [one8])
    g_bc = sb("g_bc", [128, D], F32)
    LD(g_bc, d["gmix"].partition_broadcast(128))
    hg_bc = sb("hg_bc", [128, 256], F32)
    LD(hg_bc, d["hgain"].partition_broadcast(128))
    bi = sb("bi", [8, 1], F32)
    bf = sb("bf", [8, 1], F32)
    LD(bi, d["bgate"][0:8].rearrange("(a b) -> a b", b=1))
    LD(bf, d["bgate"][8:16].rearrange("(a b) -> a b", b=1))
    V(lambda e: e.tensor_scalar(out=bi[:], in0=bi[:], scalar1=1.0 / 15.0, scalar2=None, op0=ALU.mult), [bi], [bi])
    V(lambda e: e.tensor_scalar(out=bf[:], in0=bf[:], scalar1=1.0 / 15.0, scalar2=None, op0=ALU.mult), [bf], [bf])

    NW = 2
    wt = [sb("wt%d" % i, [128, 16, 512], BF16) for i in range(NW)]
    wg = sb("wg", [128, 16, 16], BF16)
    xt = [sb("xt%d" % i, [128, D], F32) for i in range(2)]
    hb = [sb("hb%d" % i, [128, D], BF16) for i in range(2)]
    ss = [sb("ss%d" % i, [128, 1], F32) for i in range(2)]
    rstd = [sb("rstd%d" % i, [128, 1], F32) for i in range(2)]
    hnT = sb("hnT", [128, 16, 512], BF16)

    class V3:
        def __init__(self, t):
            self.t = t

        def __getitem__(self, k):
            return self.t[:].rearrange("p (h v) -> p h v", h=8)[k]
    sq, sq3 = xt[0], V3(xt[0])
    hsb, hsb3 = xt[1], V3(xt[1])
    gated = hb[0]
    k_tok = sb("k_tok", [128, 4, 1024], BF16)
    v_tok = sb("v_tok", [128, 4, 2048], BF16)
    kT = sb("kT", [128, 8, 512], BF16)
    qT = sb("qT", [128, 8, 512], BF16)
    qA = sb("qA", [128, 8, 512], BF16)
    qB = sb("qB", [128, 8, 512], BF16)
    o_sig = sb("o_sig", [128, 4, 2048], BF16)
    ig_r = sb("ig_r", [8, 512], F32)
    fg_r = sb("fg_r", [8, 512], F32)
    lf = sb("lf", [8, 512], F32)
    lf2 = sb("lf2", [8, 512], F32)
    u_r = sb("u_r", [8, 512], F32)
    e_r = sb("e_r", [8, 512], F32)
    thr_r = sb("thr_r", [8, 512], F32)
    ux = sb("ux", [8, 8], F32)
    Mx = sb("Mx", [8, 8], F32)
    fpre = sb("fpre", [8, 8], F32)
    f_r = sb("f_r", [8, 8], F32)
    mcur = sb("mcur", [8, 1], F32)
    V(lambda e: e.memset(mcur[:], 0.0), [], [mcur])
    if "pflag" in d:
        flg = sb("flg", [128, 2], F32)
        LD(flg, d["pflag"])
    e_tok = sb("e_tok", [128, 4, 8], F32)
    e_bf = sb("e_bf", [128, 4, 8], BF16)
    thr_tok = sb("thr_tok", [128, 4, 8], F32)
    F_bc = sb("F_bc", [128, 8, 8], F32)
    Cs = sb("Cs", [128, 8, 256], F32)
    ns = sb("ns", [128, 8], F32)
    V(lambda e: e.memset(Cs[:], 0.0), [], [(Cs, 0), (Cs, 1)])
    V(lambda e: e.memset(ns[:], 0.0), [], [(ns, 0), (ns, 1)])
    Cbf = [sb("Cbf%d" % i, [128, 4, 256], BF16) for i in range(2)]
    nbf = [sb("nbf%d" % i, [128, 4], BF16) for i in range(2)]
    ve = [sb("ve%d" % i, [128, 4, 256], BF16) for i in range(2)]
    Sm = sb("Sm", [128, 4, 128], BF16)
    dtmp = sb("dtmp", [128, 4], F32)
    rd = sb("rd", [128, 4], F32)
    st1 = sb("st1", [128, 8], F32)
    st2 = sb("st2", [128, 8], F32)
    ot = [sb("ot%d" % i, [128, 512], F32) for i in range(2)]
    xres = [sb("xres%d" % i, [128, 512], F32) for i in range(2)]
    ps_tr = ps("tr", [128, 4, 128], BF16)
    ps_mm = [ps("mm%d" % i, [128, 512], F32) for i in range(2)]
    psN = ps("N", [128, 4, 256], F32)
    psC = ps("C", [128, 4, 256], F32)
    pss = ps("small", [128, 128], F32)

    cnt = {"w": 0, "x": 0, "mm": 0, "ot": 0, "ve": 0}

    def nxt(k, n=2):
        v = cnt[k] % n
        cnt[k] += 1
        return v

    def load_w(dram, c0, ncols=512, dst=None):
        w = dst if dst is not None else wt[nxt("w", NW)]
        for q in range(4):
            src = dram[q * 512:(q + 1) * 512, c0:c0 + ncols].rearrange("(c p) n -> p c n", p=128)
            P.add("gpsimd", lambda e, w=w, q=q, src=src: e.dma_start(out=w[:, q * 4:(q + 1) * 4, 0:ncols], in_=src),
                  writes=[(w, q)], dma=True)
        return w

    def norm_tile(src_ap):
        k = nxt("x")
        x_, h_, s_, r_ = xt[k], hb[k], ss[k], rstd[k]
        P.add("sync", lambda e: e.dma_start(out=x_[:], in_=src_ap), writes=[x_], dma=True)
        A(lambda e: e.activation(out=h_[:], in_=x_[:], func=AF.Square, accum_out=s_[:]), [x_], [h_, s_])
        A(lambda e: e.activation(out=s_[:], in_=s_[:], func=AF.Sqrt, bias=epsT[:], scale=1.0 / D), [s_, epsT], [s_])
        V(lambda e: e.reciprocal(out=r_[:], in_=s_[:]), [s_], [r_])
        V(lambda e: e.scalar_tensor_tensor(out=h_[:], in0=x_[:], scalar=r_[:], in1=g_bc[:], op0=ALU.mult, op1=ALU.mult),
          [x_, r_, g_bc], [h_])
        return h_

    def tr16(h_, hres, i):
        for j in range(4):
            for k in range(4):
                dc = j * 4 + k
                TE(lambda e, k=k, dc=dc: e.transpose(out=ps_tr[:, k, :], in_=h_[:, dc * 128:(dc + 1) * 128], identity=ident[:]),
                   hres + [ident], [ps_tr])
            if j % 2 == 0:
                A(lambda e, j=j: e.copy(out=hnT[:, j * 4:(j + 1) * 4, i * 128:(i + 1) * 128], in_=ps_tr[:]), [ps_tr], [(hnT, i, j)])
            else:
                V(lambda e, j=j: e.tensor_copy(out=hnT[:, j * 4:(j + 1) * 4, i * 128:(i + 1) * 128], in_=ps_tr[:]), [ps_tr], [(hnT, i, j)])

    def tres(i):
        return [(hnT, i, j) for j in range(4)]

    def allT():
        return [(hnT, i, j) for i in range(4) for j in range(4)]

    def proj_tok(pm, ncols, i, w, c_lo):
        for dc in range(16):
            TE(lambda e, dc=dc: e.matmul(pm[:, 0:ncols], lhsT=hnT[:, dc, i * 128:(i + 1) * 128], rhs=w[:, dc, c_lo:c_lo + ncols],
                                         start=(dc == 0), stop=(dc == 15)),
               tres(i) + [(w, dc // 4)], [pm])

    def proj_feat(pm, m, w, c_lo):
        for dc in range(16):
            TE(lambda e, dc=dc: e.matmul(pm[0:m, :], lhsT=w[:, dc, c_lo:c_lo + m], rhs=hnT[:, dc, :], start=(dc == 0), stop=(dc == 15)),
               allT() + [(w, dc // 4)], [pm])

    w_in = d["w_in"]
    w_out = d["w_out"]
    KS = 128 ** -0.5
    for blk in range(4):
        own = blk >= 2
        r0 = (blk % 2) * 512
        if blk == 2 and "pflag" in d:
            for hg_ in range(2):
                hs_ = slice(4 * hg_, 4 * hg_ + 4)
                V(lambda e, hs_=hs_: e.tensor_scalar(out=Cs[:, hs_, :], in0=Cs[:, hs_, :], scalar1=flg[:, 1:2], scalar2=None, op0=ALU.mult), [(Cs, hg_), flg], [(Cs, hg_)])
                V(lambda e, hs_=hs_: e.tensor_scalar(out=ns[:, hs_], in0=ns[:, hs_], scalar1=flg[:, 1:2], scalar2=None, op0=ALU.mult), [(ns, hg_), flg], [(ns, hg_)])
        for i in range(4):
            if own:
                src_ap = d["ho"][r0 + i * 128:r0 + (i + 1) * 128, :]
            elif "hp_tile" in d:
                src_ap = d["hp_tile"](blk * 4 + i)
            else:
                src_ap = d["hp"][r0 + i * 128:r0 + (i + 1) * 128, :]
            h_ = norm_tile(src_ap)
            tr16(h_, [h_], i)
        load_w(w_in, 6144, ncols=16, dst=wg)
        for gi, (dst, bb) in enumerate(((ig_r, bi), (fg_r, bf))):
            pm = ps_mm[nxt("mm")]
            proj_feat(pm, 8, wg, gi * 8)
            A(lambda e, pm=pm, dst=dst, bb=bb: e.activation(out=dst[:], in_=pm[0:8, :], func=AF.Tanh, bias=bb[:], scale=1.0 / 15.0), [pm, bb], [dst])
            V(lambda e, dst=dst: e.tensor_scalar(out=dst[:], in0=dst[:], scalar1=15.0, scalar2=None, op0=ALU.mult), [dst], [dst])
        A(lambda e: e.activation(out=lf2[:], in_=fg_r[:], func=AF.Exp, scale=-1.0), [fg_r], [lf2])
        A(lambda e: e.activation(out=lf[:], in_=lf2[:], func=AF.Ln, bias=one8[:], scale=1.0), [lf2, one8], [lf])
        V(lambda e: e.tensor_scalar(out=lf[:], in0=lf[:], scalar1=-1.0, scalar2=None, op0=ALU.mult), [lf], [lf])
        bufs = [lf, lf2]
        for si, dd in enumerate((1, 2, 4, 8, 16, 32)):
            s_, d_ = bufs[si % 2], bufs[(si + 1) % 2]
            s3 = s_[:].rearrange("p (c l) -> p c l", l=64)
            d3 = d_[:].rearrange("p (c l) -> p c l", l=64)
            V(lambda e, s3=s3, d3=d3, dd=dd: e.tensor_copy(out=d3[:, :, 0:dd], in_=s3[:, :, 0:dd]), [s_], [d_])
            V(lambda e, s3=s3, d3=d3, dd=dd: e.tensor_tensor(out=d3[:, :, dd:64], in0=s3[:, :, dd:64], in1=s3[:, :, 0:64 - dd], op=ALU.add), [s_], [d_])
        lf3 = lf[:].rearrange("p (c l) -> p c l", l=64)
        lres = [lf]
        V(lambda e: e.tensor_tensor(out=u_r[:], in0=ig_r[:], in1=lf[:], op=ALU.subtract), [ig_r] + lres, [u_r])
        u3 = u_r[:].rearrange("p (c l) -> p c l", l=64)
        V(lambda e: e.reduce_max(out=ux[:], in_=u3, axis=AX.X), [u_r], [ux])
        for c in range(8):
            V(lambda e, c=c: e.tensor_tensor(out=Mx[:, c:c + 1], in0=mcur[:], in1=ux[:, c:c + 1], op=ALU.max), [mcur, ux], [(Mx, c)])
            V(lambda e, c=c: e.tensor_tensor(out=fpre[:, c:c + 1], in0=mcur[:], in1=Mx[:, c:c + 1], op=ALU.subtract), [mcur, (Mx, c)], [(fpre, c)])
            V(lambda e, c=c: e.tensor_tensor(out=mcur[:], in0=lf3[:, c, 63:64], in1=Mx[:, c:c + 1], op=ALU.add), lres + [(Mx, c)], [mcur])
        Mres = [(Mx, c) for c in range(8)]
        A(lambda e: e.activation(out=f_r[:], in_=fpre[:], func=AF.Exp), [(fpre, c) for c in range(8)], [f_r])
        V(lambda e: e.tensor_tensor(out=u3, in0=u3, in1=Mx[:, 0:8].unsqueeze(2).to_broadcast([8, 8, 64]), op=ALU.subtract), [u_r] + Mres, [u_r])
        A(lambda e: e.activation(out=e_r[:], in_=u_r[:], func=AF.Exp), [u_r], [e_r])
        if own:
            V(lambda e: e.tensor_tensor(out=lf3, in0=lf3, in1=Mx[:, 0:8].unsqueeze(2).to_broadcast([8, 8, 64]), op=ALU.add), lres + Mres, lres)
            A(lambda e: e.activation(out=thr_r[:], in_=lf[:], func=AF.Exp, scale=-1.0), lres, [thr_r])
        for t in range(4):
            TE(lambda e, t=t: e.matmul(pss[:, 0:8], lhsT=e_r[:, t * 128:(t + 1) * 128], rhs=i8[:], start=True, stop=True), [e_r, i8], [pss])
            V(lambda e, t=t: e.tensor_copy(out=e_tok[:, t, :], in_=pss[:, 0:8]), [pss], [(e_tok, t)])
            A(lambda e, t=t: e.copy(out=e_bf[:, t, :], in_=pss[:, 0:8]), [pss], [(e_bf, t)])
            if own:
                TE(lambda e, t=t: e.matmul(pss[:, 8:16], lhsT=thr_r[:, t * 128:(t + 1) * 128], rhs=i8[:], start=True, stop=True), [thr_r, i8], [pss])
                V(lambda e, t=t: e.tensor_copy(out=thr_tok[:, t, :], in_=pss[:, 8:16]), [pss], [(thr_tok, t)])
        for h in range(8):
            TE(lambda e, h=h: e.matmul(pss[:, 32 + h * 8:40 + h * 8], lhsT=hsel[:, h, :], rhs=f_r[:], start=True, stop=True), [hsel, f_r], [pss])
        V(lambda e: e.tensor_copy(out=F_bc[:], in_=pss[:, 32:96].rearrange("p (h c) -> p h c", h=8)), [pss], [F_bc])
        for u in range(2):
            w = load_w(w_in, 1024 + u * 512)
            for i in range(4):
                pm = ps_mm[nxt("mm")]
                proj_tok(pm, 512, i, w, 0)
                A(lambda e, pm=pm, i=i, u=u: e.mul(out=k_tok[:, i, u * 512:(u + 1) * 512], in_=pm[:], mul=KS), [pm], [(k_tok, i, u)])
            if own:
                for hh in range(4):
                    pm = ps_mm[nxt("mm")]
                    proj_feat(pm, 128, w, hh * 128)
                    A(lambda e, pm=pm, u=u, hh=hh: e.mul(out=kT[:, 4 * u + hh, :], in_=pm[:], mul=KS), [pm], [(kT, 4 * u + hh)])
        if own:
            for u in range(2):
                w = load_w(w_in, u * 512)
                for hh in range(4):
                    pm = ps_mm[nxt("mm")]
                    proj_feat(pm, 128, w, hh * 128)
                    h = 4 * u + hh
                    A(lambda e, pm=pm, h=h: e.copy(out=qT[:, h, :], in_=pm[:]), [pm], [(qT, h)])
            qres = [(qT, h) for h in range(8)]
            q4 = lambda t_: t_[:].rearrange("p h (t a l) -> p h t a l", t=4, a=2)
            A(lambda e: e.copy(out=qA[:], in_=qT[:]), qres, [qA])
            G(lambda e: e.memset(q4(qA)[:, :, :, 1, :], 0.0), [qA], [qA])
            A(lambda e: e.copy(out=qB[:], in_=qT[:]), qres, [qB])
            G(lambda e: e.memset(q4(qB)[:, :, :, 0, :], 0.0), [qB], [qB])
        for u in range(4):
            w = load_w(w_in, 2048 + u * 512)
            for i in range(4):
                pm = ps_mm[nxt("mm")]
                proj_tok(pm, 512, i, w, 0)
                V(lambda e, pm=pm, i=i, u=u: e.tensor_copy(out=v_tok[:, i, u * 512:(u + 1) * 512], in_=pm[:]), [pm], [(v_tok, i, u)])
        if own:
            for u in range(4):
                w = load_w(w_in, 4096 + u * 512)
                for i in range(4):
                    pm = ps_mm[nxt("mm")]
                    proj_tok(pm, 512, i, w, 0)
                    A(lambda e, pm=pm, i=i, u=u: e.activation(out=o_sig[:, i, u * 512:(u + 1) * 512], in_=pm[:], func=AF.Sigmoid), [pm], [(o_sig, i, u)])
        def do_half(t, hg, half, ve_, own=own):
            hs = slice(4 * hg, 4 * hg + 4)
            c = 2 * t + half
            pl = slice(64 * half, 64 * half + 64)
            cb, nb = Cbf[half], nbf[half]
            V(lambda e, hs=hs, c=c: e.tensor_tensor(out=Cs[:, hs, :], in0=Cs[:, hs, :], in1=F_bc[:, hs, c:c + 1].to_broadcast([128, 4, 256]), op=ALU.mult),
              [(Cs, hg), F_bc], [(Cs, hg)])
            V(lambda e, hs=hs, c=c: e.tensor_tensor(out=ns[:, hs], in0=ns[:, hs], in1=F_bc[:, hs, c], op=ALU.mult), [(ns, hg), F_bc], [(ns, hg)])
            if own:
                A(lambda e, cb=cb, hs=hs: e.copy(out=cb[:], in_=Cs[:, hs, :]), [(Cs, hg)], [cb])
                A(lambda e, nb=nb, hs=hs: e.copy(out=nb[:], in_=ns[:, hs]), [(ns, hg)], [nb])
            for hh in range(4):
                h = 4 * hg + hh
                TE(lambda e, hh=hh, h=h, pl=pl, ve_=ve_: e.matmul(psC[:, hh, :], lhsT=k_tok[pl, t, h * 128:(h + 1) * 128], rhs=ve_[pl, hh, :], start=True, stop=True),
                   [(k_tok, t, h // 4), ve_], [psC])
                TE(lambda e, hh=hh, h=h, pl=pl: e.matmul(pss[:, 16 + hh:17 + hh], lhsT=k_tok[pl, t, h * 128:(h + 1) * 128], rhs=e_bf[pl, t, h:h + 1], start=True, stop=True),
                   [(k_tok, t, h // 4), (e_bf, t)], [pss])
            V(lambda e, hs=hs: e.tensor_tensor(out=Cs[:, hs, :], in0=Cs[:, hs, :], in1=psC[:], op=ALU.add), [(Cs, hg), psC], [(Cs, hg)])
            V(lambda e, hs=hs: e.tensor_tensor(out=ns[:, hs], in0=ns[:, hs], in1=pss[:, 16:20], op=ALU.add), [(ns, hg), pss], [(ns, hg)])

        def do_group(t, hg, own=own):
            tc_ = slice(t * 128, (t + 1) * 128)
            hs = slice(4 * hg, 4 * hg + 4)
            ve_ = ve[nxt("ve")]
            V(lambda e, ve_=ve_, t=t, hs=hs: e.tensor_tensor(out=ve_[:], in0=v_tok[:, t, hg * 1024:(hg + 1) * 1024].rearrange("p (h v) -> p h v", h=4),
                                                            in1=e_tok[:, t, hs].unsqueeze(2).to_broadcast([128, 4, 256]), op=ALU.mult),
              [(v_tok, t, 2 * hg), (v_tok, t, 2 * hg + 1), (e_tok, t)], [ve_])
            if own:
                for hh in range(4):
                    h = 4 * hg + hh
                    TE(lambda e, hh=hh, h=h: e.matmul(psN[:, hh, 0:128], lhsT=kT[:, h, tc_], rhs=qT[:, h, tc_], start=True, stop=True),
                       [(kT, h), (qT, h)], [psN])
                V(lambda e: e.tensor_tensor(out=Sm[:], in0=psN[:, :, 0:128], in1=bmask[:, :].unsqueeze(1).to_broadcast([128, 4, 128]), op=ALU.mult), [psN, bmask], [Sm])
            for half in range(2):
                do_half(t, hg, half, ve_)
            if own:
                for hh in range(4):
                    h = 4 * hg + hh
                    TE(lambda e, hh=hh, ve_=ve_: e.matmul(psN[:, hh, :], lhsT=Sm[:, hh, :], rhs=ve_[:, hh, :], start=True, stop=False), [Sm, ve_], [psN])
                    TE(lambda e, hh=hh, h=h: e.matmul(psN[:, hh, :], lhsT=qA[:, h, tc_], rhs=Cbf[0][:, hh, :], start=False, stop=False), [qA, Cbf[0]], [psN])
                    TE(lambda e, hh=hh, h=h: e.matmul(psN[:, hh, :], lhsT=qB[:, h, tc_], rhs=Cbf[1][:, hh, :], start=False, stop=True), [qB, Cbf[1]], [psN])
                    TE(lambda e, hh=hh, h=h: e.matmul(pss[:, 24 + hh:25 + hh], lhsT=Sm[:, hh, :], rhs=e_bf[:, t, h:h + 1], start=True, stop=False), [Sm, (e_bf, t)], [pss])
                    TE(lambda e, hh=hh, h=h: e.matmul(pss[:, 24 + hh:25 + hh], lhsT=qA[:, h, tc_], rhs=nbf[0][:, hh:hh + 1], start=False, stop=False), [qA, nbf[0]], [pss])
                    TE(lambda e, hh=hh, h=h: e.matmul(pss[:, 24 + hh:25 + hh], lhsT=qB[:, h, tc_], rhs=nbf[1][:, hh:hh + 1], start=False, stop=True), [qB, nbf[1]], [pss])
                A(lambda e: e.activation(out=dtmp[:], in_=pss[:, 24:28], func=AF.Abs), [pss], [dtmp])
                V(lambda e, hs=hs: e.tensor_tensor(out=dtmp[:], in0=dtmp[:], in1=thr_tok[:, t, hs], op=ALU.max), [dtmp, (thr_tok, t)], [dtmp])
                V(lambda e: e.reciprocal(out=rd[:], in_=dtmp[:]), [dtmp], [rd])
                V(lambda e, hs=hs: e.tensor_tensor(out=hsb3[:, hs, :], in0=psN[:], in1=rd[:, :].unsqueeze(2).to_broadcast([128, 4, 256]), op=ALU.mult),
                  [psN, rd], [hsb])

        def do_tile(t, own=own):
            for hg in range(2):
                do_group(t, hg)
            if own:
                hres = [hsb]
                V(lambda e: e.tensor_tensor(out=sq[:], in0=hsb[:], in1=hsb[:], op=ALU.mult), hres, [sq])
                V(lambda e: e.reduce_sum(out=st1[:], in_=sq3[:], axis=AX.X), [sq], [st1])
                A(lambda e: e.activation(out=st1[:], in_=st1[:], func=AF.Sqrt, bias=epsT[:], scale=1.0 / 256), [st1, epsT], [st1])
                V(lambda e: e.reciprocal(out=st2[:], in_=st1[:]), [st1], [st2])
                V(lambda e: e.tensor_tensor(out=hsb3[:], in0=hsb3[:], in1=st2[:, :].unsqueeze(2).to_broadcast([128, 8, 256]), op=ALU.mult), hres + [st2], hres)
                V(lambda e: e.tensor_tensor(out=hsb3[:], in0=hsb3[:], in1=hg_bc[:, :].unsqueeze(1).to_broadcast([128, 8, 256]), op=ALU.mult), hres + [hg_bc], hres)
                V(lambda e, t=t: e.tensor_tensor(out=gated[:], in0=hsb[:], in1=o_sig[:, t, :], op=ALU.mult),
                  hres + [(o_sig, t, u) for u in range(4)], [gated])
                tr16(gated, [gated], t)

        for t in range(4):
            do_tile(t)
        if own:
            for u in range(4):
                w = load_w(w_out, u * 512)
                for i in range(4):
                    pm = ps_mm[nxt("mm")]
                    proj_tok(pm, 512, i, w, 0)
                    k = nxt("ot")
                    xr, o = xres[k], ot[k]
                    rr = r0 + i * 128
                    P.add("sync", lambda e, xr=xr, rr=rr, u=u: e.dma_start(out=xr[:], in_=d["ho"][rr:rr + 128, u * 512:(u + 1) * 512]), writes=[xr], dma=True)
                    V(lambda e, pm=pm, xr=xr, o=o: e.tensor_tensor(out=o[:], in0=pm[:], in1=xr[:], op=ALU.add), [pm, xr], [o])
                    P.add("sync", lambda e, o=o, rr=rr, u=u: e.dma_start(out=d["out"][rr:rr + 128, u * 512:(u + 1) * 512], in_=o[:]),
                          reads=[o], writes=[("out", rr, u)], dma=True)


def ml_consts():
    import ml_dtypes
    c = {}
    c["ident"] = np.eye(128).astype(ml_dtypes.bfloat16)
    c["i8"] = np.eye(8).astype(np.float32)
    hs = np.zeros((8, 8, 128), np.float32)
    for h in range(8):
        hs[h, h, :] = 1.0
    c["hsel"] = hs
    s = np.arange(128)[:, None]
    l = np.arange(128)[None, :]
    c["bmask"] = ((s <= l) & (s // 64 == l // 64)).astype(ml_dtypes.bfloat16)
    return c


ML_SHAPES = {"hp": ([1024, D], F32), "ho": ([1024, D], F32), "gmix": ([D], F32), "hgain": ([256], F32), "bgate": ([16], F32),
             "w_in": ([D, 6160], F32), "w_out": ([D, D], F32), "ident": ([128, 128], BF16), "i8": ([8, 8], F32),
             "hsel": ([8, 8, 128], F32), "bmask": ([128, 128], BF16)}


def rb(g):
    i, p = g // 2, g % 2
    return (i // 2) * 4 + p * 2 + (i % 2)


def build_select(P, nc, sb, ps, hg, ho, pflag):
    fl = sb("fl", [128, 2], F32)
    P.add("sync", lambda e: e.dma_start(out=fl[:], in_=pflag), writes=[fl], dma=True)
    c0 = [sb("c0_%d" % i, [128, D], F32) for i in range(3)]
    c1 = [sb("c1_%d" % i, [128, D], F32) for i in range(3)]
    for j in range(8):
        a, b = c0[j % 3], c1[j % 3]
        P.add("sync", lambda e, a=a, j=j: e.dma_start(out=a[:], in_=hg[rb(j) * 128:(rb(j) + 1) * 128, :]), reads=[("hg", rb(j) // 4)], writes=[a], dma=True)
        P.add("sync", lambda e, b=b, j=j: e.dma_start(out=b[:], in_=hg[rb(8 + j) * 128:(rb(8 + j) + 1) * 128, :]), reads=[("hg", rb(8 + j) // 4)], writes=[b], dma=True)
        P.add("vector", lambda e, a=a: e.tensor_scalar(out=a[:], in0=a[:], scalar1=fl[:, 0:1], scalar2=None, op0=ALU.mult), reads=[a, fl], writes=[a])
        P.add("vector", lambda e, a=a, b=b: e.scalar_tensor_tensor(out=b[:], in0=b[:], scalar=fl[:, 1:2], in1=a[:], op0=ALU.mult, op1=ALU.add), reads=[a, b, fl], writes=[b])
        P.add("sync", lambda e, b=b, j=j: e.dma_start(out=ho[j * 128:(j + 1) * 128, :], in_=b[:]), reads=[b], writes=[("ho", j)], dma=True)


IN_SHAPES = {
    "xk": ([2048, D], F32), "xq": ([1024, D], F32), "pflag": ([128, 2], F32),
    "norm_mix": ([2, D], F32), "norm_ffn": ([2, D], F32),
    "att_w_in": ([D, 4176], F32), "att_q_gain": ([128], F32), "att_k_gain": ([128], F32), "att_w_out": ([D, D], F32),
    "ml_w_in": ([D, 6160], F32), "ml_b_gate": ([16], F32), "ml_h_gain": ([256], F32), "ml_w_out": ([D, D], F32),
    "ffn_w_up": ([2, D, DFF], F32), "ffn_w_down": ([2, DFF, D], F32),
    "cos_k": ([128, 16, 16], F32), "sin_k": ([128, 16, 16], F32), "cos_ki": ([128, 16, 8], F32), "sin_ki": ([128, 16, 8], F32),
    "cos_q": ([128, 8, 16], F32), "sin_q": ([128, 8, 16], F32), "cos_qi": ([128, 8, 8], F32), "sin_qi": ([128, 8, 8], F32),
    "cbias": ([128, 256], F32), "ident": ([128, 128], BF16), "i8": ([8, 8], F32), "hsel": ([8, 8, 128], F32), "bmask": ([128, 128], BF16),
}
PAIRS = [[0, 1], [2, 3], [4, 5], [6, 7]]


SCHED_ATT = True
SCHED_ML = True


def build_fused(use_cc=True):
    nc = bass.Bass("TRN2", target_bir_lowering=False)
    di = {k: nc.dram_tensor(k, s, dt, kind="ExternalInput").ap() for k, (s, dt) in IN_SHAPES.items()}
    out = nc.dram_tensor("out", [1024, D], F32, kind="ExternalOutput").ap()
    h_a = nc.dram_tensor("h_a_i", [1024, D], F32).ap()
    h_0 = nc.dram_tensor("h_0_i", [1024, D], F32).ap()
    hg = nc.dram_tensor("hg_i", [2048, D], F32).ap()
    hp = nc.dram_tensor("hp_i", [1024, D], F32).ap()
    ho = nc.dram_tensor("ho_i", [1024, D], F32).ap()
    h_m = nc.dram_tensor("h_m_i", [1024, D], F32).ap()
    with ExitStack() as st:
        P = Prog(nc, st, schedule=SCHED_ATT)
        ar = Arena(nc, st)
        d = {k: di[k] for k in ("xk", "xq", "cos_k", "sin_k", "cos_ki", "sin_ki", "cos_q", "sin_q", "cos_qi", "sin_qi", "cbias", "ident")}
        d.update({"gmix": di["norm_mix"][0], "gq": di["att_q_gain"], "gk": di["att_k_gain"], "w_in": di["att_w_in"], "w_out": di["att_w_out"], "out": h_a})
        build_att(P, nc, ar.sb, ar.ps, d)
        P.barrier(schedule=False)
        ar.reset()
        build_ffn(P, nc, ar.sb, ar.ps, h_a, di["norm_ffn"][0], di["ffn_w_up"][0], di["ffn_w_down"][0], h_0, di["ident"], 1024)
        if use_cc:
            for j in range(4):
                P.add("gpsimd", lambda e, j=j: e.collective_compute("AllGather", ALU.bypass, replica_groups=PAIRS,
                                                                    ins=[h_0[j * 256:(j + 1) * 256, :]], outs=[hg[j * 512:(j + 1) * 512, :]]),
                      reads=[("out", j * 256 + t * 128, db) for t in range(2) for db in range(4)], writes=[("hg", j)], cc=True)
        else:
            allout = [("out", t * 128, db) for t in range(8) for db in range(4)]
            P.add("sync", lambda e: e.dma_start(out=hg[0:1024, :], in_=h_0), reads=allout, writes=["hg"], dma=True)
            P.add("sync", lambda e: e.dma_start(out=hg[1024:2048, :], in_=h_0), reads=allout, writes=["hg2"], dma=True)
        P.barrier(schedule=False)
        ar.reset()
        build_select(P, nc, ar.sb, ar.ps, hg, ho, di["pflag"])
        P.barrier(schedule=SCHED_ML)
        ar.reset()
        d = {"hp_tile": (lambda t: hg[rb(t) * 128:(rb(t) + 1) * 128, :]), "pflag": di["pflag"], "ho": ho, "gmix": di["norm_mix"][1], "hgain": di["ml_h_gain"], "bgate": di["ml_b_gate"], "w_in": di["ml_w_in"],
             "w_out": di["ml_w_out"], "ident": di["ident"], "i8": di["i8"], "hsel": di["hsel"], "bmask": di["bmask"], "out": h_m}
        build_ml(P, nc, ar.sb, ar.ps, d)
        P.barrier(schedule=False)
        ar.reset()
        build_ffn(P, nc, ar.sb, ar.ps, h_m, di["norm_ffn"][1], di["ffn_w_up"][1], di["ffn_w_down"][1], out, di["ident"], 1024)
        P.wait_all_dma("sync")
        P.emit()
    return nc


def core_inputs(c, x, shared):
    b, p = c // 2, c % 2
    m = dict(shared)
    m["xk"] = x[b]
    m["xq"] = q_tiles(x[b], p)
    fl = np.zeros((128, 2), np.float32)
    fl[:, p] = 1.0
    m["pflag"] = fl
    m.update(att_consts(p))
    m.update(ml_consts())
    return m


_NC = {}


def kernel(x, norm_mix, norm_ffn, att_w_in, att_q_gain, att_k_gain, att_w_out,
           ml_w_in, ml_b_gate, ml_h_gain, ml_w_out, ffn_w_up, ffn_w_down):
    f32 = lambda a: np.ascontiguousarray(np.asarray(a, dtype=np.float32))
    x = f32(x)
    shared = {"norm_mix": f32(norm_mix), "norm_ffn": f32(norm_ffn), "att_w_in": f32(att_w_in)[0], "att_q_gain": f32(att_q_gain)[0],
              "att_k_gain": f32(att_k_gain)[0], "att_w_out": f32(att_w_out)[0], "ml_w_in": f32(ml_w_in)[0], "ml_b_gate": f32(ml_b_gate)[0],
              "ml_h_gain": f32(ml_h_gain)[0], "ml_w_out": f32(ml_w_out)[0], "ffn_w_up": f32(ffn_w_up), "ffn_w_down": f32(ffn_w_down)}
    if "nc" not in _NC:
        _NC["nc"] = build_fused()
    cores = list(range(8))
    in_maps = [core_inputs(c, x, shared) for c in cores]
    res = run_bass_kernel_spmd(_NC["nc"], in_maps, core_ids=cores)
    out = np.empty_like(x)
    for c in cores:
        b, p = c // 2, c % 2
        out[b, p * 1024:(p + 1) * 1024] = np.asarray(res.results[c]["out"])
    return out
```

```python
import sys, time
import numpy as np
from contextlib import ExitStack
import ml_dtypes
import concourse.bass as bass
import concourse.mybir as mybir
from concourse.bass_utils import run_bass_kernel_spmd


F32 = mybir.dt.float32
BF16 = mybir.dt.bfloat16
ALU = mybir.AluOpType
AF = mybir.ActivationFunctionType
AX = mybir.AxisListType

ENGS = ["tensor", "vector", "scalar", "gpsimd", "sync"]


class T:
    __slots__ = ("h", "name")

    def __init__(self, h, name):
        self.h = h
        self.name = name

    def __getitem__(self, k):
        return self.h[k]


class Op:
    __slots__ = ("eng", "fn", "deps", "odeps", "sig", "sem", "val", "dma", "idx", "cc", "region", "cost", "users", "tail", "nwait", "rt", "fin", "st", "prevdma")

    def __init__(self, eng, fn, dma):
        self.cc = False
        self.eng = eng
        self.fn = fn
        self.deps = set()
        self.odeps = set()
        self.sig = False
        self.sem = None
        self.val = 0
        self.dma = dma
        self.prevdma = None


DEF_COST = {"tensor": 0.25, "vector": 0.7, "scalar": 0.7, "gpsimd": 1.0, "sync": 0.1}
DMA_ISSUE = {"sync": 0.15, "gpsimd": 1.2}
DMA_LAT = 5.0
SYNC_LAT = 1.2


class Prog:
    def __init__(self, nc, stack, n_dma_sems=24, schedule=True):
        self.nc = nc
        self.stack = stack
        self.schedule = schedule
        self.regions = [[]]
        self.region_sched = [schedule]
        self.last_w = {}
        self.readers = {}
        self.engsem = {e: stack.enter_context(nc.semaphore("s_" + e)) for e in ENGS}
        self.dma_pool = {}
        for q in ("sync", "gpsimd"):
            self.dma_pool[q] = [stack.enter_context(nc.semaphore("d_%s%d" % (q, i))) for i in range(n_dma_sems)]
        self.n = 0
        self.final_wait = None

    def add(self, eng, fn, reads=(), writes=(), dma=False, cc=False, cost=None):
        op = Op(eng, fn, dma or cc)
        op.cc = cc
        op.idx = self.n
        self.n += 1
        op.region = len(self.regions) - 1
        op.cost = cost if cost is not None else DEF_COST[eng]
        deps = set()
        for r in reads:
            w = self.last_w.get(r)
            if w is not None:
                deps.add(w)
        for w_ in writes:
            lw = self.last_w.get(w_)
            if lw is not None:
                deps.add(lw)
            for rd in self.readers.get(w_, ()):
                deps.add(rd)
        if eng == "tensor" and not dma:
            for d_ in deps:
                if d_.eng == "tensor" and not d_.dma:
                    op.odeps.add(d_)
                else:
                    op.deps.add(d_)
        else:
            op.deps = deps
        if cc:
            op.sem = self.stack.enter_context(self.nc.semaphore("cc%d" % op.idx))
            op.val = 1
        for r in reads:
            self.readers.setdefault(r, []).append(op)
        for w_ in writes:
            self.last_w[w_] = op
            self.readers[w_] = []
        self.regions[-1].append(op)
        return op

    def barrier(self, schedule=None):
        self.regions.append([])
        self.region_sched.append(self.schedule if schedule is None else schedule)
        self.last_w = {}
        self.readers = {}

    def wait_all_dma(self, eng="sync"):
        self.final_wait = eng

    def _schedule(self, ops, do_sched):
        import heapq
        order = {e: [] for e in ENGS}
        if not do_sched:
            for op in ops:
                order[op.eng].append(op)
            return order
        for op in ops:
            op.users = []
            op.nwait = len(op.deps) + len(op.odeps)
            op.rt = 0.0
        for op in ops:
            for d_ in op.deps:
                d_.users.append((op, True))
            for d_ in op.odeps:
                d_.users.append((op, False))
        for op in reversed(ops):
            t = 0.0
            for u, sy in op.users:
                t = max(t, u.tail + (SYNC_LAT if sy else 0.0))
            op.tail = t + (DMA_LAT if op.dma else op.cost)
        pend = {e: [] for e in ENGS}
        avail = {e: [] for e in ENGS}
        tfree = {e: 0.0 for e in ENGS}
        for op in ops:
            if op.nwait == 0:
                heapq.heappush(pend[op.eng], (0.0, op.idx, op))
        left = len(ops)
        while left:
            best = None
            for e in ENGS:
                pe, av = pend[e], avail[e]
                t = tfree[e]
                while pe and pe[0][0] <= t:
                    _, _, o = heapq.heappop(pe)
                    heapq.heappush(av, (-o.tail, o.idx, o))
                if av:
                    cand = (t, av[0][0], e, True)
                elif pe:
                    cand = (pe[0][0], -pe[0][2].tail, e, False)
                else:
                    continue
                if best is None or cand[:2] < best[:2]:
                    best = cand
            start, _, e, from_av = best
            if from_av:
                _, _, op = heapq.heappop(avail[e])
            else:
                _, _, op = heapq.heappop(pend[e])
            order[e].append(op)
            left -= 1
            if op.dma:
                busy = DMA_ISSUE.get(e, 0.2)
                fin = start + busy + DMA_LAT
            else:
                busy = op.cost
                fin = start + busy
            tfree[e] = start + busy
            for u, sy in op.users:
                r = fin + SYNC_LAT if sy else start
                if r > u.rt:
                    u.rt = r
                u.nwait -= 1
                if u.nwait == 0:
                    heapq.heappush(pend[u.eng], (u.rt, u.idx, u))
        return order

    def emit(self):
        nc = self.nc
        queues = {e: [] for e in ENGS}
        bar_deps = []
        for ri, ops in enumerate(self.regions):
            order = self._schedule(ops, self.region_sched[ri])
            lasts = set()
            for e in ENGS:
                if order[e]:
                    for op in reversed(order[e]):
                        if not op.dma and op.fn is not None:
                            lasts.add(op)
                            break
                for op in order[e]:
                    if op.dma:
                        lasts.add(op)
            bar_deps.append(lasts)
            for e in ENGS:
                first = True
                for op in order[e]:
                    if first and len(bar_deps) > 1:
                        op.deps = set(op.deps) | bar_deps[-2]
                    first = False
                    queues[e].append(op)
        if self.final_wait is not None:
            op = Op(self.final_wait, None, False)
            op.deps = set(o for r in self.regions for o in r if o.dma)
            queues[self.final_wait].append(op)
        for q, pool in self.dma_pool.items():
            k = 0
            hist = []
            for op in queues[q]:
                if op.dma and not op.cc:
                    op.sem = pool[k % len(pool)]
                    op.val = 16 * (k // len(pool) + 1)
                    if k >= len(pool):
                        op.prevdma = hist[k - len(pool)]
                    hist.append(op)
                    k += 1
        pos = {}
        for e in ENGS:
            for i, op in enumerate(queues[e]):
                pos[op] = i
        for e in ENGS:
            for op in queues[e]:
                best = {}
                nd = set()
                for d in op.deps:
                    if d.dma:
                        nd.add(d)
                    else:
                        b_ = best.get(d.eng)
                        if b_ is None or pos[d] > pos[b_]:
                            best[d.eng] = d
                for d in best.values():
                    d.sig = True
                    nd.add(d)
                op.deps = nd
        for e in ENGS:
            cnt = 0
            for op in queues[e]:
                if op.dma:
                    continue
                if op.sig:
                    cnt += 1
                    op.sem = self.engsem[e]
                    op.val = cnt
        self.queues = queues
        with nc.Block() as block:
            def mk(e):
                def body(eng):
                    waited = {}
                    for op in queues[e]:
                        need = {}
                        for d in op.deps:
                            if need.get(d.sem, 0) < d.val:
                                need[d.sem] = d.val
                        if op.prevdma is not None:
                            p_ = op.prevdma
                            if need.get(p_.sem, 0) < p_.val:
                                need[p_.sem] = p_.val
                        for s, v in need.items():
                            if waited.get(s, 0) < v:
                                eng.wait_ge(s, v)
                                waited[s] = v
                        if op.fn is None:
                            continue
                        ins = op.fn(eng)
                        if op.cc:
                            ins.then_inc(op.sem, 1)
                        elif op.dma:
                            ins.then_inc(op.sem, 16)
                        elif op.sig:
                            ins.then_inc(op.sem, 1)
                return body
            block.tensor(mk("tensor"))
            block.vector(mk("vector"))
            block.scalar(mk("scalar"))
            block.gpsimd(mk("gpsimd"))
            block.sync(mk("sync"))


SBW = 52000


class Arena:
    def __init__(self, nc, st):
        self.sb_t = st.enter_context(nc.sbuf_tensor("arena_sb", [128, SBW], F32))
        self.ps_t = st.enter_context(nc.psum_tensor("arena_ps", [128, 4096], F32))
        self.reset()

    def reset(self):
        self.off = 0
        self.pbank = 0

    @staticmethod
    def _shape(v, shape):
        if len(shape) == 2:
            return v
        if len(shape) == 3:
            return v.rearrange("p (a b) -> p a b", a=shape[1])
        if len(shape) == 4:
            return v.rearrange("p (a b c) -> p a b c", a=shape[1], b=shape[2])
        raise ValueError(shape)

    def sb(self, name, shape, dt):
        p = shape[0]
        n = int(np.prod(shape[1:]))
        words = n if dt == F32 else (n + 1) // 2
        words = (words + 7) // 8 * 8
        assert self.off + words <= SBW, ("SBUF arena overflow", name, self.off, words)
        base = self.sb_t[0:p, self.off:self.off + words]
        self.off += words
        v = base if dt == F32 else base.bitcast(BF16)
        return T(self._shape(v[:, 0:n], shape), name)

    def ps(self, name, shape, dt):
        p = shape[0]
        n = int(np.prod(shape[1:]))
        nbytes = n * (4 if dt == F32 else 2)
        banks = (nbytes + 2047) // 2048
        assert self.pbank + banks <= 8, ("PSUM arena overflow", name)
        base = self.ps_t[0:p, self.pbank * 512:(self.pbank + banks) * 512]
        self.pbank += banks
        v = base if dt == F32 else base.bitcast(BF16)
        return T(self._shape(v[:, 0:n], shape), name)


D = 2048
EPS = 1e-6
NEG = -1.0e30
REPL = -3.0e38


def merge_gens(gens):
    gens = [g for g in gens if g is not None]
    while gens:
        for g in list(gens):
            try:
                next(g)
            except StopIteration:
                gens.remove(g)


def build_att(P, nc, sb, ps, d):
    def V(fn, r, w):
        return P.add("vector", fn, reads=r, writes=w)

    def A(fn, r, w):
        return P.add("scalar", fn, reads=r, writes=w)

    def G(fn, r, w):
        return P.add("gpsimd", fn, reads=r, writes=w)

    def TE(fn, r, w):
        return P.add("tensor", fn, reads=r, writes=w)

    def LD(t, src, q="sync", key=None):
        return P.add(q, lambda e: e.dma_start(out=t[:] if key is None else key[1], in_=src), writes=[t if key is None else key[0]], dma=True)

    ident = sb("ident", [128, 128], BF16)
    LD(ident, d["ident"])
    epsT = sb("epsT", [128, 1], F32)
    V(lambda e: e.memset(epsT[:], EPS), [], [epsT])
    g_bc = sb("g_bc", [128, D], F32)
    LD(g_bc, d["gmix"].partition_broadcast(128))
    gq_bc = sb("gq_bc", [128, 128], F32)
    LD(gq_bc, d["gq"].partition_broadcast(128))
    gk_bc = sb("gk_bc", [128, 128], F32)
    LD(gk_bc, d["gk"].partition_broadcast(128))
    tabs = {}
    for nm, shp in (("cos_k", [128, 16, 16]), ("sin_k", [128, 16, 16]), ("cos_ki", [128, 16, 8]), ("sin_ki", [128, 16, 8]),
                    ("cos_q", [128, 8, 16]), ("sin_q", [128, 8, 16]), ("cos_qi", [128, 8, 8]), ("sin_qi", [128, 8, 8])):
        tabs[nm] = sb(nm, shp, F32)
        LD(tabs[nm], d[nm])
    cbias = sb("cbias", [128, 256], F32)
    LD(cbias, d["cbias"])

    NW = 2
    wt = [sb("wt%d" % i, [128, 16, 512], BF16) for i in range(NW)]
    wki = sb("wki", [128, 16, 80], BF16)
    xt = [sb("xt%d" % i, [128, D], F32) for i in range(2)]
    hb = [sb("hb%d" % i, [128, D], BF16) for i in range(2)]
    ss = [sb("ss%d" % i, [128, 1], F32) for i in range(2)]
    rstd = [sb("rstd%d" % i, [128, 1], F32) for i in range(2)]
    hnT = sb("hnT", [128, 16, 512], BF16)
    kT = sb("kT", [128, 4, 2048], BF16)
    Vt = sb("Vt", [128, 16, 4, 128], BF16)
    ones_bf = sb("ones_bf", [128, 128], BF16)
    rsum = sb("rsum", [128, 512], F32)
    kiT = sb("kiT", [128, 2048], BF16)
    qT = sb("qT", [128, 16, 512], BF16)
    qiT = sb("qiT", [128, 8, 512], BF16)
    wi_sb = sb("wi_sb", [128, 4, 16], F32)
    score = sb("score", [128, 2048], F32)
    work = sb("work", [128, 2048], F32)
    mask = sb("mask", [128, 2048], BF16)
    maskT = sb("maskT", [128, 16, 128], BF16)
    scoreb = [score, work]
    bis = [[sb("bis%d_%d" % (k, j), [128, 1], F32) for j in range(6)] for k in range(2)]
    stepsb = [sb("steps%d" % k, [128, 24], F32) for k in range(2)]
    ckrow = sb("ckrow", [128, 24], F32)
    for k_ in range(24):
        V(lambda e, k_=k_: e.memset(ckrow[:, k_:k_ + 1], 2.0 ** -(k_ + 1)), [], [ckrow])
    tmpA = [sb("tmpA%d" % i, [128, 512], F32) for i in range(2)]
    sqt = sb("sqt", [128, 512], F32)
    osb = sqt
    st1 = sb("st1", [128, 8], F32)
    st2 = sb("st2", [128, 8], F32)
    rtmp = [sb("rtmp%d" % i, [128, 64], F32) for i in range(4)]
    ob = [sb("ob%d" % i, [128, 512], BF16) for i in range(2)]
    relu_t = [sb("relu%d" % i, [128, 512], F32) for i in range(2)]
    pT = [sb("pT%d" % i, [128, 512], BF16) for i in range(3)]
    ot = [sb("ot%d" % i, [128, 512], F32) for i in range(2)]
    xres = [sb("xres%d" % i, [128, 512], F32) for i in range(2)]
    ps_tr = [ps("tr%d" % i, [128, 4, 128], BF16) for i in range(2)]
    ps_mm = [ps("mm%d" % i, [128, 512], F32) for i in range(2)]
    ps_s = [ps("s%d" % i, [128, 512], F32) for i in range(2)]
    ps_oa = ps("oa", [128, 512], F32)
    ps_os = ps("os", [128, 512], F32)

    cnt = {"w": 0, "x": 0, "tr": 0, "mm": 0, "tA": 0, "ob": 0, "relu": 0, "p": 0, "s": 0, "ot": 0}

    def nxt(k, n=2):
        v = cnt[k] % n
        cnt[k] += 1
        return v

    def load_w(dram, r0, c0, ncols=512, dst=None):
        w = dst if dst is not None else wt[nxt("w", NW)]
        for q in range(4):
            src = dram[r0 + q * 512:r0 + (q + 1) * 512, c0:c0 + ncols].rearrange("(c p) n -> p c n", p=128)
            P.add("gpsimd", lambda e, w=w, q=q, src=src: e.dma_start(out=w[:, q * 4:(q + 1) * 4, 0:ncols], in_=src),
                  writes=[(w, q)], dma=True)
        return w

    def wres(w):
        return [(w, q) for q in range(4)]

    def norm_tile(src_ap):
        k = nxt("x")
        x_, h_, s_, r_ = xt[k], hb[k], ss[k], rstd[k]
        P.add("sync", lambda e: e.dma_start(out=x_[:], in_=src_ap), writes=[x_], dma=True)
        A(lambda e: e.activation(out=h_[:], in_=x_[:], func=AF.Square, accum_out=s_[:]), [x_], [h_, s_])
        A(lambda e: e.activation(out=s_[:], in_=s_[:], func=AF.Sqrt, bias=epsT[:], scale=1.0 / D), [s_, epsT], [s_])
        V(lambda e: e.reciprocal(out=r_[:], in_=s_[:]), [s_], [r_])
        V(lambda e: e.scalar_tensor_tensor(out=h_[:], in0=x_[:], scalar=r_[:], in1=g_bc[:], op0=ALU.mult, op1=ALU.mult),
          [x_, r_, g_bc], [h_])
        return h_

    def tr16(h_, dstT, col0, dkey):
        for j in range(4):
            pt = ps_tr[nxt("tr")]
            for k in range(4):
                dc = j * 4 + k
                TE(lambda e, pt=pt, k=k, dc=dc: e.transpose(out=pt[:, k, :], in_=h_[:, dc * 128:(dc + 1) * 128], identity=ident[:]),
                   [h_, ident], [pt])
            if j % 2 == 0:
                A(lambda e, pt=pt, j=j: e.copy(out=dstT[:, j * 4:(j + 1) * 4, col0:col0 + 128], in_=pt[:]), [pt], [(dstT, dkey, j)])
            else:
                V(lambda e, pt=pt, j=j: e.tensor_copy(out=dstT[:, j * 4:(j + 1) * 4, col0:col0 + 128], in_=pt[:]), [pt], [(dstT, dkey, j)])

    def tres(dstT, dkey):
        return [(dstT, dkey, j) for j in range(4)]

    def proj(pm, ncols, i, w, c_lo):
        for dc in range(16):
            TE(lambda e, dc=dc: e.matmul(pm[:, 0:ncols], lhsT=hnT[:, dc, i * 128:(i + 1) * 128], rhs=w[:, dc, c_lo:c_lo + ncols],
                                         start=(dc == 0), stop=(dc == 15)),
               tres(hnT, i) + [(w, dc // 4)], [pm])

    def normrope(pm, nh, hd, hf, gain, cos_ap, sin_ap, tabres, out, dup=False):
        n = nh * hd
        tA = tmpA[nxt("tA")]
        tA3 = tA[:, 0:n].rearrange("p (a b) -> p a b", a=nh)
        A(lambda e: e.copy(out=tA[:, 0:n], in_=pm[:, 0:n]), [pm], [tA])
        if gain is not None:
            V(lambda e: e.tensor_tensor(out=sqt[:, 0:n], in0=tA[:, 0:n], in1=tA[:, 0:n], op=ALU.mult), [tA], [sqt])
            V(lambda e: e.reduce_sum(out=st1[:, 0:nh], in_=sqt[:, 0:n].rearrange("p (a b) -> p a b", a=nh), axis=AX.X), [sqt], [st1])
            A(lambda e: e.activation(out=st1[:, 0:nh], in_=st1[:, 0:nh], func=AF.Sqrt, bias=epsT[:], scale=1.0 / hd), [st1, epsT], [st1])
            V(lambda e: e.reciprocal(out=st2[:, 0:nh], in_=st1[:, 0:nh]), [st1], [st2])
            V(lambda e: e.tensor_tensor(out=tA3, in0=tA3, in1=st2[:, 0:nh].unsqueeze(2).to_broadcast([128, nh, hd]), op=ALU.mult), [tA, st2], [tA])
            V(lambda e: e.tensor_tensor(out=tA3, in0=tA3, in1=gain[:, 0:hd].unsqueeze(1).to_broadcast([128, nh, hd]), op=ALU.mult), [tA, gain], [tA])
        x1 = tA3[:, :, 0:hf]
        x2 = tA3[:, :, hf:2 * hf]
        cb = cos_ap.unsqueeze(1).to_broadcast([128, nh, hf])
        sn = sin_ap.unsqueeze(1).to_broadcast([128, nh, hf])
        r3 = [r[:, 0:nh * hf].rearrange("p (a b) -> p a b", a=nh) for r in rtmp]
        G(lambda e: e.tensor_tensor(out=r3[0], in0=x1, in1=cb, op=ALU.mult), [tA] + tabres, [rtmp[0]])
        G(lambda e: e.tensor_tensor(out=r3[1], in0=x2, in1=sn, op=ALU.mult), [tA] + tabres, [rtmp[1]])
        G(lambda e: e.tensor_tensor(out=r3[2], in0=x1, in1=sn, op=ALU.mult), [tA] + tabres, [rtmp[2]])
        G(lambda e: e.tensor_tensor(out=r3[3], in0=x2, in1=cb, op=ALU.mult), [tA] + tabres, [rtmp[3]])
        reps = 2 if dup else 1
        for rp in range(reps):
            o3 = out[:, rp * n:(rp + 1) * n].rearrange("p (a b) -> p a b", a=nh)
            G(lambda e, o3=o3: e.tensor_tensor(out=o3[:, :, 0:hf], in0=r3[0], in1=r3[1], op=ALU.subtract), [rtmp[0], rtmp[1]], [(out, "a", rp)])
            G(lambda e, o3=o3: e.tensor_tensor(out=o3[:, :, hf:2 * hf], in0=r3[2], in1=r3[3], op=ALU.add), [rtmp[2], rtmp[3]], [(out, "b", rp)])
            A(lambda e, o3=o3: e.copy(out=o3[:, :, 2 * hf:hd], in_=tA3[:, :, 2 * hf:hd]), [tA], [(out, "c", rp)])

    def obres(out, dup=False):
        return [(out, k, rp) for k in "abc" for rp in range(2 if dup else 1)]

    V(lambda e: e.memset(ones_bf[:], 1.0), [], [ones_bf])
    w_in = d["w_in"]
    wk = load_w(w_in, 0, 2048)
    wv = load_w(w_in, 0, 2560)
    load_w(w_in, 0, 4096, ncols=80, dst=wki)
    for kb in range(4):
        for i in range(4):
            tI = kb * 4 + i
            h_ = norm_tile(d["xk"][tI * 128:(tI + 1) * 128, :])
            tr16(h_, hnT, i * 128, i)
        for i in range(4):
            tI = kb * 4 + i
            pm = ps_mm[nxt("mm")]
            proj(pm, 512, i, wk, 0)
            o_ = ob[nxt("ob")]
            normrope(pm, 4, 128, 16, gk_bc, tabs["cos_k"][:, tI, :], tabs["sin_k"][:, tI, :], [tabs["cos_k"], tabs["sin_k"]], o_)
            pt = ps_tr[nxt("tr")]
            for h in range(4):
                TE(lambda e, pt=pt, h=h, o_=o_: e.transpose(out=pt[:, h, :], in_=o_[:, h * 128:(h + 1) * 128], identity=ident[:]),
                   obres(o_) + [ident], [pt])
            V(lambda e, pt=pt, tI=tI: e.tensor_copy(out=kT[:, :, tI * 128:(tI + 1) * 128], in_=pt[:]), [pt], [(kT, tI)])
            pm = ps_mm[nxt("mm")]
            proj(pm, 512, i, wv, 0)
            A(lambda e, pm=pm, tI=tI: e.copy(out=Vt[:, tI, :, :], in_=pm[:].rearrange("p (a b) -> p a b", a=4)), [pm], [(Vt, tI)])
            pm = ps_mm[nxt("mm")]
            proj(pm, 64, i, wki, 0)
            o_ = ob[nxt("ob")]
            normrope(pm, 1, 64, 8, None, tabs["cos_ki"][:, tI, :], tabs["sin_ki"][:, tI, :], [tabs["cos_ki"], tabs["sin_ki"]], o_, dup=True)
            pt = ps_tr[nxt("tr")]
            TE(lambda e, pt=pt, o_=o_: e.transpose(out=pt[:, 0, :], in_=o_[:, 0:128], identity=ident[:]), obres(o_, True) + [ident], [pt])
            V(lambda e, pt=pt, tI=tI: e.tensor_copy(out=kiT[:, tI * 128:(tI + 1) * 128], in_=pt[:, 0, :]), [pt], [(kiT, tI)])

    SCALE = 128 ** -0.5

    KBIS = 22

    def index_steps(half, i):
        s_ = half * 4 + i
        nkt = 2 * s_ + 2
        S = nkt * 128
        nch = (S + 511) // 512
        sc = scoreb[s_ % 2]
        lo, Wd, nmid, cntt, gg, rmx = bis[s_ % 2]
        steps = stepsb[s_ % 2]
        for h in range(16):
            pair = h // 2
            off = (h % 2) * 64
            for c in range(nch):
                cols = min(512, S - c * 512)
                pm = ps_mm[nxt("mm")]
                TE(lambda e, pm=pm, cols=cols, c=c, off=off, pair=pair: e.matmul(pm[:, 0:cols], lhsT=qiT[off:off + 64, pair, i * 128:(i + 1) * 128],
                                                                                 rhs=kiT[off:off + 64, c * 512:c * 512 + cols], start=True, stop=True),
                   [(qiT, i, pair)] + [(kiT, t) for t in range(c * 4, min(c * 4 + 4, nkt))], [pm])
                rl = relu_t[nxt("relu")]
                A(lambda e, pm=pm, rl=rl, cols=cols: e.activation(out=rl[:, 0:cols], in_=pm[:, 0:cols], func=AF.Relu), [pm], [rl])
                if h == 0:
                    V(lambda e, rl=rl, cols=cols, c=c: e.tensor_scalar(out=sc[:, c * 512:c * 512 + cols], in0=rl[:, 0:cols], scalar1=wi_sb[:, i, 0:1], scalar2=None, op0=ALU.mult),
                      [rl, (wi_sb, i)], [(sc, c)])
                else:
                    V(lambda e, rl=rl, cols=cols, c=c, h=h: e.scalar_tensor_tensor(out=sc[:, c * 512:c * 512 + cols], in0=rl[:, 0:cols], scalar=wi_sb[:, i, h:h + 1],
                                                                                 in1=sc[:, c * 512:c * 512 + cols], op0=ALU.mult, op1=ALU.add),
                      [rl, (wi_sb, i), (sc, c)], [(sc, c)])
                yield
        sres = [(sc, c) for c in range(4)]
        V(lambda e: e.reduce_max(out=rmx[:], in_=sc[:, 0:S], axis=AX.X), sres, [rmx])
        V(lambda e: e.tensor_reduce(out=lo[:], in_=sc[:, 0:S], axis=AX.X, op=ALU.min), sres, [lo])
        V(lambda e: e.tensor_tensor(out=Wd[:], in0=rmx[:], in1=lo[:], op=ALU.subtract), [rmx, lo], [Wd])
        V(lambda e: e.tensor_scalar(out=steps[:], in0=ckrow[:], scalar1=Wd[:, 0:1], scalar2=None, op0=ALU.mult), [ckrow, Wd], [steps])
        V(lambda e: e.tensor_tensor(out=nmid[:], in0=lo[:], in1=steps[:, 0:1], op=ALU.add), [lo, steps], [nmid])
        V(lambda e: e.tensor_tensor(out=sc[:, S - 256:S], in0=sc[:, S - 256:S], in1=cbias[:], op=ALU.add), sres + [cbias], sres)
        yield
        jt = xt[s_ % 2]
        jres = [jt, hb[s_ % 2]]
        junk = jt[:].bitcast(BF16)
        for k in range(KBIS):
            if k % 2 == 0:
                A(lambda e: e.activation(out=junk[:, 0:S], in_=sc[:, 0:S], func=AF.Sign, bias=nmid[:], scale=-1.0, accum_out=cntt[:]), sres + [nmid] + jres, [cntt])
                V(lambda e: e.tensor_scalar(out=gg[:], in0=cntt[:], scalar1=S - 510.5, scalar2=0.5, op0=ALU.is_le, op1=ALU.subtract), [cntt], [gg])
            else:
                V(lambda e: e.tensor_scalar(out=junk[:, 0:S], in0=sc[:, 0:S], scalar1=nmid[:, 0:1], scalar2=0.0, op0=ALU.is_ge, op1=ALU.add, accum_out=cntt[:]),
                  sres + [nmid] + jres, [cntt])
                V(lambda e: e.tensor_scalar(out=gg[:], in0=cntt[:], scalar1=255.5, scalar2=0.5, op0=ALU.is_ge, op1=ALU.subtract), [cntt], [gg])
            V(lambda e, k=k: e.scalar_tensor_tensor(out=nmid[:], in0=gg[:], scalar=steps[:, k:k + 1], in1=nmid[:], op0=ALU.mult, op1=ALU.add), [gg, steps, nmid], [nmid])
            yield
        V(lambda e: e.scalar_tensor_tensor(out=lo[:], in0=steps[:, KBIS - 1:KBIS], scalar=-0.5, in1=nmid[:], op0=ALU.mult, op1=ALU.add), [steps, nmid], [lo])
        V(lambda e: e.tensor_scalar(out=mask[:, 0:S], in0=sc[:, 0:S], scalar1=lo[:, 0:1], scalar2=None, op0=ALU.is_ge), sres + [lo], [mask])
        yield
        for j0 in range(0, nkt, 4):
            pt = ps_tr[nxt("tr")]
            nj = min(4, nkt - j0)
            for jj in range(nj):
                j = j0 + jj
                TE(lambda e, pt=pt, jj=jj, j=j: e.transpose(out=pt[:, jj, :], in_=mask[:, j * 128:(j + 1) * 128], identity=ident[:]), [mask, ident], [pt])
            V(lambda e, pt=pt, j0=j0, nj=nj: e.tensor_scalar(out=maskT[:, j0:j0 + nj, :], in0=pt[:, 0:nj, :], scalar1=1.0, scalar2=30000.0, op0=ALU.subtract, op1=ALU.mult),
              [pt], [(maskT, j0 // 4)])
            yield

    def attn_steps(half, i):
        s_ = half * 4 + i
        nkt = 2 * s_ + 2
        for g in range(4):
            for j in range(nkt):
                pS = ps_s[nxt("s")]
                TE(lambda e, pS=pS, g=g, j=j: e.matmul(pS[:].rearrange("p (a b) -> p a b", a=4), lhsT=kT[:, g, j * 128:(j + 1) * 128],
                                                       rhs=qT[:, 4 * g:4 * g + 4, i * 128:(i + 1) * 128], start=True, stop=False),
                   [(kT, j), (qT, i, g)], [pS])
                TE(lambda e, pS=pS, j=j: e.matmul(pS[:].rearrange("p (a b) -> p a b", a=4), lhsT=ident[:],
                                                  rhs=maskT[:, j, :].unsqueeze(1).to_broadcast([128, 4, 128]), start=False, stop=True),
                   [ident, (maskT, j // 4)], [pS])
                pm_ = pT[nxt("p", 3)]
                A(lambda e, pS=pS, pm_=pm_: e.activation(out=pm_[:], in_=pS[:], func=AF.Exp, scale=SCALE), [pS], [pm_])
                TE(lambda e, pm_=pm_, j=j, g=g: e.matmul(ps_oa[:], lhsT=Vt[:, j, g, :], rhs=pm_[:], start=(j == 0), stop=(j == nkt - 1)),
                   [pm_, (Vt, j)], [ps_oa])
                TE(lambda e, pm_=pm_, j=j: e.matmul(ps_os[:], lhsT=ones_bf[:], rhs=pm_[:], start=(j == 0), stop=(j == nkt - 1)),
                   [pm_, ones_bf], [ps_os])
                yield
            V(lambda e: e.reciprocal(out=rsum[:], in_=ps_os[:]), [ps_os], [rsum])
            V(lambda e, g=g: e.tensor_tensor(out=hnT[:, 4 * g:4 * g + 4, i * 128:(i + 1) * 128], in0=ps_oa[:].rearrange("p (a b) -> p a b", a=4),
                                             in1=rsum[:].rearrange("p (a b) -> p a b", a=4), op=ALU.mult),
              [ps_oa, rsum], [(hnT, i, g)])
            yield

    w_out = d["w_out"]
    for half in range(2):
        for i in range(4):
            s_ = half * 4 + i
            h_ = norm_tile(d["xq"][s_ * 128:(s_ + 1) * 128, :])
            tr16(h_, hnT, i * 128, i)
        for u in range(4):
            w = load_w(w_in, 0, u * 512)
            for i in range(4):
                s_ = half * 4 + i
                pm = ps_mm[nxt("mm")]
                proj(pm, 512, i, w, 0)
                o_ = ob[nxt("ob")]
                normrope(pm, 4, 128, 16, gq_bc, tabs["cos_q"][:, s_, :], tabs["sin_q"][:, s_, :], [tabs["cos_q"], tabs["sin_q"]], o_)
                pt = ps_tr[nxt("tr")]
                for h in range(4):
                    TE(lambda e, pt=pt, h=h, o_=o_: e.transpose(out=pt[:, h, :], in_=o_[:, h * 128:(h + 1) * 128], identity=ident[:]),
                       obres(o_) + [ident], [pt])
                V(lambda e, pt=pt, u=u, i=i: e.tensor_copy(out=qT[:, 4 * u:4 * u + 4, i * 128:(i + 1) * 128], in_=pt[:]), [pt], [(qT, i, u)])
        for u in range(2):
            w = load_w(w_in, 0, 3072 + u * 512)
            for i in range(4):
                s_ = half * 4 + i
                pm = ps_mm[nxt("mm")]
                proj(pm, 512, i, w, 0)
                o_ = ob[nxt("ob")]
                normrope(pm, 8, 64, 8, None, tabs["cos_qi"][:, s_, :], tabs["sin_qi"][:, s_, :], [tabs["cos_qi"], tabs["sin_qi"]], o_)
                pt = ps_tr[nxt("tr")]
                for pr in range(4):
                    TE(lambda e, pt=pt, pr=pr, o_=o_: e.transpose(out=pt[:, pr, :], in_=o_[:, pr * 128:(pr + 1) * 128], identity=ident[:]),
                       obres(o_) + [ident], [pt])
                V(lambda e, pt=pt, u=u, i=i: e.tensor_copy(out=qiT[:, 4 * u:4 * u + 4, i * 128:(i + 1) * 128], in_=pt[:]), [pt],
                  [(qiT, i, 4 * u + k) for k in range(4)])
        for i in range(4):
            pm = ps_mm[nxt("mm")]
            proj(pm, 16, i, wki, 64)
            V(lambda e, pm=pm, i=i: e.tensor_scalar(out=wi_sb[:, i, :], in0=pm[:, 0:16], scalar1=1.0 / 32.0, scalar2=None, op0=ALU.mult), [pm], [(wi_sb, i)])
        prev = None
        for i in range(4):
            merge_gens([index_steps(half, i), prev])
            prev = attn_steps(half, i)
        merge_gens([prev])
        for u in range(4):
            w = load_w(w_out, 0, u * 512)
            for i in range(4):
                s_ = half * 4 + i
                pm = ps_mm[nxt("mm")]
                proj(pm, 512, i, w, 0)
                k = nxt("ot")
                xr, o = xres[k], ot[k]
                P.add("sync", lambda e, xr=xr, s_=s_, u=u: e.dma_start(out=xr[:], in_=d["xq"][s_ * 128:(s_ + 1) * 128, u * 512:(u + 1) * 512]), writes=[xr], dma=True)
                V(lambda e, pm=pm, xr=xr, o=o: e.tensor_tensor(out=o[:], in0=pm[:], in1=xr[:], op=ALU.add), [pm, xr], [o])
                P.add("sync", lambda e, o=o, s_=s_, u=u: e.dma_start(out=d["out"][s_ * 128:(s_ + 1) * 128, u * 512:(u + 1) * 512], in_=o[:]),
                      reads=[o], writes=[("out", s_, u)], dma=True)


def rope_tables(pos, rot):
    half = rot // 2
    inv = 500000.0 ** (-2.0 * np.arange(half, dtype=np.float32) / rot)
    ang = pos.astype(np.float32)[:, None] * inv[None, :].astype(np.float32)
    return np.cos(ang).astype(np.float32), np.sin(ang).astype(np.float32)


def att_consts(p):
    import ml_dtypes
    c = {}
    kpos = np.arange(2048)
    ck, sk = rope_tables(kpos, 32)
    c["cos_k"] = np.ascontiguousarray(ck.reshape(16, 128, 16).transpose(1, 0, 2))
    c["sin_k"] = np.ascontiguousarray(sk.reshape(16, 128, 16).transpose(1, 0, 2))
    ck, sk = rope_tables(kpos, 16)
    c["cos_ki"] = np.ascontiguousarray(ck.reshape(16, 128, 8).transpose(1, 0, 2))
    c["sin_ki"] = np.ascontiguousarray(sk.reshape(16, 128, 8).transpose(1, 0, 2))
    qpos = np.concatenate([np.arange(128) + (2 * i + p) * 128 for i in range(8)])
    cq, sq = rope_tables(qpos, 32)
    c["cos_q"] = np.ascontiguousarray(cq.reshape(8, 128, 16).transpose(1, 0, 2))
    c["sin_q"] = np.ascontiguousarray(sq.reshape(8, 128, 16).transpose(1, 0, 2))
    cq, sq = rope_tables(qpos, 16)
    c["cos_qi"] = np.ascontiguousarray(cq.reshape(8, 128, 8).transpose(1, 0, 2))
    c["sin_qi"] = np.ascontiguousarray(sq.reshape(8, 128, 8).transpose(1, 0, 2))
    t = np.arange(128)[:, None]
    col = np.arange(256)[None, :]
    c["cbias"] = np.where(col <= t + 128 * p, 0.0, NEG).astype(np.float32)
    c["ident"] = np.eye(128).astype(ml_dtypes.bfloat16)
    return c


ATT_SHAPES = {"xk": ([2048, D], F32), "xq": ([1024, D], F32), "gmix": ([D], F32), "gq": ([128], F32), "gk": ([128], F32),
              "w_in": ([D, 4176], F32), "w_out": ([D, D], F32),
              "cos_k": ([128, 16, 16], F32), "sin_k": ([128, 16, 16], F32), "cos_ki": ([128, 16, 8], F32), "sin_ki": ([128, 16, 8], F32),
              "cos_q": ([128, 8, 16], F32), "sin_q": ([128, 8, 16], F32), "cos_qi": ([128, 8, 8], F32), "sin_qi": ([128, 8, 8], F32),
              "cbias": ([128, 256], F32), "ident": ([128, 128], BF16)}


def q_tiles(x_b, p):
    return np.concatenate([x_b[(2 * i + p) * 128:(2 * i + p + 1) * 128] for i in range(8)], axis=0)


D = 2048
DFF = 8192
EPS = 1e-6


def build_ffn(P, nc, sb, ps, h_dram, g_dram, wu_dram, wd_dram, out_dram, ident_dram, ntok):
    NB = ntok // 512
    ident = sb("ident", [128, 128], BF16)
    g_bc = sb("g_bc", [128, D], F32)
    h_t = [sb("h_t%d" % i, [128, D], F32) for i in range(4)]
    hn = [sb("hn%d" % i, [128, D], BF16) for i in range(2)]
    sq = sb("sq", [128, D], BF16)
    ss = [sb("ss%d" % i, [128, 1], F32) for i in range(2)]
    rstd = [sb("rstd%d" % i, [128, 1], F32) for i in range(2)]
    hnT = sb("hnT", [128, 16, 512], BF16)
    rT = sb("rT", [128, 64, 512], BF16)
    NW = 3
    wt = [sb("wt%d" % i, [128, 16, 512], BF16) for i in range(NW)]
    ot = [sb("ot%d" % i, [128, 512], F32) for i in range(2)]
    relu_t = [sb("relu%d" % i, [128, 512], F32) for i in range(2)]
    ps_tr = [ps("ps_tr%d" % i, [128, 4, 128], BF16) for i in range(2)]
    ps_u = [ps("ps_u%d" % i, [128, 512], F32) for i in range(2)]
    ps_d = [ps("ps_d%d" % i, [128, 512], F32) for i in range(4)]

    epsT = sb("epsT", [128, 1], F32)
    P.add("vector", lambda e: e.memset(epsT[:], EPS), writes=[epsT])
    P.add("sync", lambda e: e.dma_start(out=ident[:], in_=ident_dram), writes=[ident], dma=True)
    P.add("sync", lambda e: e.dma_start(out=g_bc[:], in_=g_dram.partition_broadcast(128)), writes=[g_bc], dma=True)

    wcnt = [0]

    def load_w(dram, r0, c0):
        i = wcnt[0] % NW
        wcnt[0] += 1
        w = wt[i]
        for q in range(4):
            src = dram[r0 + q * 512:r0 + (q + 1) * 512, c0:c0 + 512].rearrange("(c p) n -> p c n", p=128)
            P.add("gpsimd", lambda e, w=w, q=q, src=src: e.dma_start(out=w[:, q * 4:(q + 1) * 4, :], in_=src),
                  writes=[(w, q)], dma=True)
        return w

    trc = [0]
    uc = [0]
    oc = [0]
    for tb in range(NB):
        for i in range(4):
            t0 = tb * 512 + i * 128
            ht = h_t[i]
            P.add("sync", lambda e, ht=ht, t0=t0: e.dma_start(out=ht[:], in_=h_dram[t0:t0 + 128, :]), writes=[ht], dma=True)
            s_ = ss[i % 2]
            r_ = rstd[i % 2]
            hb = hn[i % 2]
            P.add("scalar", lambda e, ht=ht, s_=s_: e.activation(out=sq[:], in_=ht[:], func=AF.Square, accum_out=s_[:]),
                  reads=[ht], writes=[sq, s_])
            P.add("scalar", lambda e, s_=s_: e.activation(out=s_[:], in_=s_[:], func=AF.Sqrt, bias=epsT[:], scale=1.0 / D),
                  reads=[s_, epsT], writes=[s_])
            P.add("vector", lambda e, s_=s_, r_=r_: e.reciprocal(out=r_[:], in_=s_[:]),
                  reads=[s_], writes=[r_])
            P.add("vector", lambda e, ht=ht, r_=r_, hb=hb: e.scalar_tensor_tensor(out=hb[:], in0=ht[:], scalar=r_[:], in1=g_bc[:], op0=ALU.mult, op1=ALU.mult),
                  reads=[ht, r_, g_bc], writes=[hb])
            for j in range(4):
                pt = ps_tr[trc[0] % 2]
                trc[0] += 1
                for k in range(4):
                    dc = j * 4 + k
                    P.add("tensor", lambda e, pt=pt, k=k, hb=hb, dc=dc: e.transpose(out=pt[:, k, :], in_=hb[:, dc * 128:(dc + 1) * 128], identity=ident[:]),
                          reads=[hb, ident], writes=[pt])
                eng = "scalar" if (j % 2 == 0) else "vector"
                if eng == "scalar":
                    P.add("scalar", lambda e, pt=pt, j=j, i=i: e.copy(out=hnT[:, j * 4:(j + 1) * 4, i * 128:(i + 1) * 128], in_=pt[:]),
                          reads=[pt], writes=[(hnT, i, j)])
                else:
                    P.add("vector", lambda e, pt=pt, j=j, i=i: e.tensor_copy(out=hnT[:, j * 4:(j + 1) * 4, i * 128:(i + 1) * 128], in_=pt[:]),
                          reads=[pt], writes=[(hnT, i, j)])
        hnT_res = [(hnT, i, j) for i in range(4) for j in range(4)]
        for fg in range(16):
            w = load_w(wu_dram, 0, fg * 512)
            for fcl in range(4):
                fc = fg * 4 + fcl
                pu = ps_u[uc[0] % 2]
                uc[0] += 1
                for dc in range(16):
                    P.add("tensor", lambda e, pu=pu, w=w, fcl=fcl, dc=dc: e.matmul(pu[:], lhsT=w[:, dc, fcl * 128:(fcl + 1) * 128], rhs=hnT[:, dc, :], start=(dc == 0), stop=(dc == 15)),
                          reads=[(w, dc // 4)] + hnT_res, writes=[pu])
                ru = relu_t[uc[0] % 2]
                P.add("scalar", lambda e, pu=pu, ru=ru: e.activation(out=ru[:], in_=pu[:], func=AF.Relu),
                      reads=[pu], writes=[ru])
                P.add("vector", lambda e, ru=ru, fc=fc: e.tensor_tensor(out=rT[:, fc, :], in0=ru[:], in1=ru[:], op=ALU.mult),
                      reads=[ru], writes=[(rT, fc)])
        for db in range(4):
            for fq in range(4):
                w = load_w(wd_dram, fq * 2048, db * 512)
                for i in range(4):
                    for k in range(16):
                        fc = fq * 16 + k
                        P.add("tensor", lambda e, i=i, w=w, k=k, fc=fc, fq=fq: e.matmul(ps_d[i][:], lhsT=rT[:, fc, i * 128:(i + 1) * 128], rhs=w[:, k, :], start=(fq == 0 and k == 0), stop=(fq == 3 and k == 15)),
                              reads=[(w, k // 4), (rT, fc)], writes=[ps_d[i]])
            for i in range(4):
                o = ot[oc[0] % 2]
                oc[0] += 1
                t0 = tb * 512 + i * 128
                P.add("vector", lambda e, o=o, i=i, db=db: e.tensor_tensor(out=o[:], in0=ps_d[i][:], in1=h_t[i][:, db * 512:(db + 1) * 512], op=ALU.add),
                      reads=[ps_d[i], h_t[i]], writes=[o])
                P.add("sync", lambda e, o=o, t0=t0, db=db: e.dma_start(out=out_dram[t0:t0 + 128, db * 512:(db + 1) * 512], in_=o[:]),
                      reads=[o], writes=[("out", t0, db)], dma=True)


D = 2048
EPS = 1e-6


def build_ml(P, nc, sb, ps, d):
    def V(fn, r, w):
        return P.add("vector", fn, reads=r, writes=w)

    def A(fn, r, w):
        return P.add("scalar", fn, reads=r, writes=w)

    def G(fn, r, w):
        return P.add("gpsimd", fn, reads=r, writes=w)

    def TE(fn, r, w):
        return P.add("tensor", fn, reads=r, writes=w)

    def LD(t, src):
        return P.add("sync", lambda e: e.dma_start(out=t[:], in_=src), writes=[t], dma=True)

    ident = sb("ident", [128, 128], BF16)
    LD(ident, d["ident"])
    i8 = sb("i8", [8, 8], F32)
    LD(i8, d["i8"])
    hsel = sb("hsel", [8, 8, 128], F32)
    LD(hsel, d["hsel"])
    bmask = sb("bmask", [128, 128], BF16)
    LD(bmask, d["bmask"])
    epsT = sb("epsT", [128, 1], F32)
    V(lambda e: e.memset(epsT[:], EPS), [], [epsT])
    one8 = sb("one8", [8, 1], F32)
    V(lambda e: e.memset(one8[:], 1.0), [], [one8])
    g_bc = sb("g_bc", [128, D], F32)
    LD(g_bc, d["gmix"].partition_broadcast(128))
    hg_bc = sb("hg_bc", [128, 256], F32)
    LD(hg_bc, d["hgain"].partition_broadcast(128))
    bi = sb("bi", [8, 1], F32)
    bf = sb("bf", [8, 1], F32)
    LD(bi, d["bgate"][0:8].rearrange("(a b) -> a b", b=1))
    LD(bf, d["bgate"][8:16].rearrange("(a b) -> a b", b=1))
    V(lambda e: e.tensor_scalar(out=bi[:], in0=bi[:], scalar1=1.0 / 15.0, scalar2=None, op0=ALU.mult), [bi], [bi])
    V(lambda e: e.tensor_scalar(out=bf[:], in0=bf[:], scalar1=1.0 / 15.0, scalar2=None, op0=ALU.mult), [bf], [bf])

    NW = 2
    wt = [sb("wt%d" % i, [128, 16, 512], BF16) for i in range(NW)]
    wg = sb("wg", [128, 16, 16], BF16)
    xt = [sb("xt%d" % i, [128, D], F32) for i in range(2)]
    hb = [sb("hb%d" % i, [128, D], BF16) for i in range(2)]
    ss = [sb("ss%d" % i, [128, 1], F32) for i in range(2)]
    rstd = [sb("rstd%d" % i, [128, 1], F32) for i in range(2)]
    hnT = sb("hnT", [128, 16, 512], BF16)

    class V3:
        def __init__(self, t):
            self.t = t

        def __getitem__(self, k):
            return self.t[:].rearrange("p (h v) -> p h v", h=8)[k]
    sq, sq3 = xt[0], V3(xt[0])
    hsb, hsb3 = xt[1], V3(xt[1])
    gated = hb[0]
    k_tok = sb("k_tok", [128, 4, 1024], BF16)
    v_tok = sb("v_tok", [128, 4, 2048], BF16)
    kT = sb("kT", [128, 8, 512], BF16)
    qT = sb("qT", [128, 8, 512], BF16)
    qA = sb("qA", [128, 8, 512], BF16)
    qB = sb("qB", [128, 8, 512], BF16)
    o_sig = sb("o_sig", [128, 4, 2048], BF16)
    ig_r = sb("ig_r", [8, 512], F32)
    fg_r = sb("fg_r", [8, 512], F32)
    lf = sb("lf", [8, 512], F32)
    lf2 = sb("lf2", [8, 512], F32)
    u_r = sb("u_r", [8, 512], F32)
    e_r = sb("e_r", [8, 512], F32)
    thr_r = sb("thr_r", [8, 512], F32)
    ux = sb("ux", [8, 8], F32)
    Mx = sb("Mx", [8, 8], F32)
    fpre = sb("fpre", [8, 8], F32)
    f_r = sb("f_r", [8, 8], F32)
    mcur = sb("mcur", [8, 1], F32)
    V(lambda e: e.memset(mcur[:], 0.0), [], [mcur])
    if "pflag" in d:
        flg = sb("flg", [128, 2], F32)
        LD(flg, d["pflag"])
    e_tok = sb("e_tok", [128, 4, 8], F32)
    e_bf = sb("e_bf", [128, 4, 8], BF16)
    thr_tok = sb("thr_tok", [128, 4, 8], F32)
    F_bc = sb("F_bc", [128, 8, 8], F32)
    Cs = sb("Cs", [128, 8, 256], F32)
    ns = sb("ns", [128, 8], F32)
    V(lambda e: e.memset(Cs[:], 0.0), [], [(Cs, 0), (Cs, 1)])
    V(lambda e: e.memset(ns[:], 0.0), [], [(ns, 0), (ns, 1)])
    Cbf = [sb("Cbf%d" % i, [128, 4, 256], BF16) for i in range(2)]
    nbf = [sb("nbf%d" % i, [128, 4], BF16) for i in range(2)]
    ve = [sb("ve%d" % i, [128, 4, 256], BF16) for i in range(2)]
    Sm = sb("Sm", [128, 4, 128], BF16)
    dtmp = sb("dtmp", [128, 4], F32)
    rd = sb("rd", [128, 4], F32)
    st1 = sb("st1", [128, 8], F32)
    st2 = sb("st2", [128, 8], F32)
    ot = [sb("ot%d" % i, [128, 512], F32) for i in range(2)]
    xres = [sb("xres%d" % i, [128, 512], F32) for i in range(2)]
    ps_tr = ps("tr", [128, 4, 128], BF16)
    ps_mm = [ps("mm%d" % i, [128, 512], F32) for i in range(2)]
    psN = ps("N", [128, 4, 256], F32)
    psC = ps("C", [128, 4, 256], F32)
    pss = ps("small", [128, 128], F32)

    cnt = {"w": 0, "x": 0, "mm": 0, "ot": 0, "ve": 0}

    def nxt(k, n=2):
        v = cnt[k] % n
        cnt[k] += 1
        return v

    def load_w(dram, c0, ncols=512, dst=None):
        w = dst if dst is not None else wt[nxt("w", NW)]
        for q in range(4):
            src = dram[q * 512:(q + 1) * 512, c0:c0 + ncols].rearrange("(c p) n -> p c n", p=128)
            P.add("gpsimd", lambda e, w=w, q=q, src=src: e.dma_start(out=w[:, q * 4:(q + 1) * 4, 0:ncols], in_=src),
                  writes=[(w, q)], dma=True)
        return w

    def norm_tile(src_ap):
        k = nxt("x")
        x_, h_, s_, r_ = xt[k], hb[k], ss[k], rstd[k]
        P.add("sync", lambda e: e.dma_start(out=x_[:], in_=src_ap), writes=[x_], dma=True)
        A(lambda e: e.activation(out=h_[:], in_=x_[:], func=AF.Square, accum_out=s_[:]), [x_], [h_, s_])
        A(lambda e: e.activation(out=s_[:], in_=s_[:], func=AF.Sqrt, bias=epsT[:], scale=1.0 / D), [s_, epsT], [s_])
        V(lambda e: e.reciprocal(out=r_[:], in_=s_[:]), [s_], [r_])
        V(lambda e: e.scalar_tensor_tensor(out=h_[:], in0=x_[:], scalar=r_[:], in1=g_bc[:], op0=ALU.mult, op1=ALU.mult),
          [x_, r_, g_bc], [h_])
        return h_

    def tr16(h_, hres, i):
        for j in range(4):
            for k in range(4):
                dc = j * 4 + k
                TE(lambda e, k=k, dc=dc: e.transpose(out=ps_tr[:, k, :], in_=h_[:, dc * 128:(dc + 1) * 128], identity=ident[:]),
                   hres + [ident], [ps_tr])
            if j % 2 == 0:
                A(lambda e, j=j: e.copy(out=hnT[:, j * 4:(j + 1) * 4, i * 128:(i + 1) * 128], in_=ps_tr[:]), [ps_tr], [(hnT, i, j)])
            else:
                V(lambda e, j=j: e.tensor_copy(out=hnT[:, j * 4:(j + 1) * 4, i * 128:(i + 1) * 128], in_=ps_tr[:]), [ps_tr], [(hnT, i, j)])

    def tres(i):
        return [(hnT, i, j) for j in range(4)]

    def allT():
        return [(hnT, i, j) for i in range(4) for j in range(4)]

    def proj_tok(pm, ncols, i, w, c_lo):
        for dc in range(16):
            TE(lambda e, dc=dc: e.matmul(pm[:, 0:ncols], lhsT=hnT[:, dc, i * 128:(i + 1) * 128], rhs=w[:, dc, c_lo:c_lo + ncols],
                                         start=(dc == 0), stop=(dc == 15)),
               tres(i) + [(w, dc // 4)], [pm])

    def proj_feat(pm, m, w, c_lo):
        for dc in range(16):
            TE(lambda e, dc=dc: e.matmul(pm[0:m, :], lhsT=w[:, dc, c_lo:c_lo + m], rhs=hnT[:, dc, :], start=(dc == 0), stop=(dc == 15)),
               allT() + [(w, dc // 4)], [pm])

    w_in = d["w_in"]
    w_out = d["w_out"]
    KS = 128 ** -0.5
    for blk in range(4):
        own = blk >= 2
        r0 = (blk % 2) * 512
        if blk == 2 and "pflag" in d:
            for hg_ in range(2):
                hs_ = slice(4 * hg_, 4 * hg_ + 4)
                V(lambda e, hs_=hs_: e.tensor_scalar(out=Cs[:, hs_, :], in0=Cs[:, hs_, :], scalar1=flg[:, 1:2], scalar2=None, op0=ALU.mult), [(Cs, hg_), flg], [(Cs, hg_)])
                V(lambda e, hs_=hs_: e.tensor_scalar(out=ns[:, hs_], in0=ns[:, hs_], scalar1=flg[:, 1:2], scalar2=None, op0=ALU.mult), [(ns, hg_), flg], [(ns, hg_)])
        for i in range(4):
            if own:
                src_ap = d["ho"][r0 + i * 128:r0 + (i + 1) * 128, :]
            elif "hp_tile" in d:
                src_ap = d["hp_tile"](blk * 4 + i)
            else:
                src_ap = d["hp"][r0 + i * 128:r0 + (i + 1) * 128, :]
            h_ = norm_tile(src_ap)
            tr16(h_, [h_], i)
        load_w(w_in, 6144, ncols=16, dst=wg)
        for gi, (dst, bb) in enumerate(((ig_r, bi), (fg_r, bf))):
            pm = ps_mm[nxt("mm")]
            proj_feat(pm, 8, wg, gi * 8)
            A(lambda e, pm=pm, dst=dst, bb=bb: e.activation(out=dst[:], in_=pm[0:8, :], func=AF.Tanh, bias=bb[:], scale=1.0 / 15.0), [pm, bb], [dst])
            V(lambda e, dst=dst: e.tensor_scalar(out=dst[:], in0=dst[:], scalar1=15.0, scalar2=None, op0=ALU.mult), [dst], [dst])
        A(lambda e: e.activation(out=lf2[:], in_=fg_r[:], func=AF.Exp, scale=-1.0), [fg_r], [lf2])
        A(lambda e: e.activation(out=lf[:], in_=lf2[:], func=AF.Ln, bias=one8[:], scale=1.0), [lf2, one8], [lf])
        V(lambda e: e.tensor_scalar(out=lf[:], in0=lf[:], scalar1=-1.0, scalar2=None, op0=ALU.mult), [lf], [lf])
        bufs = [lf, lf2]
        for si, dd in enumerate((1, 2, 4, 8, 16, 32)):
            s_, d_ = bufs[si % 2], bufs[(si + 1) % 2]
            s3 = s_[:].rearrange("p (c l) -> p c l", l=64)
            d3 = d_[:].rearrange("p (c l) -> p c l", l=64)
            V(lambda e, s3=s3, d3=d3, dd=dd: e.tensor_copy(out=d3[:, :, 0:dd], in_=s3[:, :, 0:dd]), [s_], [d_])
            V(lambda e, s3=s3, d3=d3, dd=dd: e.tensor_tensor(out=d3[:, :, dd:64], in0=s3[:, :, dd:64], in1=s3[:, :, 0:64 - dd], op=ALU.add), [s_], [d_])
        lf3 = lf[:].rearrange("p (c l) -> p c l", l=64)
        lres = [lf]
        V(lambda e: e.tensor_tensor(out=u_r[:], in0=ig_r[:], in1=lf[:], op=ALU.subtract), [ig_r] + lres, [u_r])
        u3 = u_r[:].rearrange("p (c l) -> p c l", l=64)
        V(lambda e: e.reduce_max(out=ux[:], in_=u3, axis=AX.X), [u_r], [ux])
        for c in range(8):
            V(lambda e, c=c: e.tensor_tensor(out=Mx[:, c:c + 1], in0=mcur[:], in1=ux[:, c:c + 1], op=ALU.max), [mcur, ux], [(Mx, c)])
            V(lambda e, c=c: e.tensor_tensor(out=fpre[:, c:c + 1], in0=mcur[:], in1=Mx[:, c:c + 1], op=ALU.subtract), [mcur, (Mx, c)], [(fpre, c)])
            V(lambda e, c=c: e.tensor_tensor(out=mcur[:], in0=lf3[:, c, 63:64], in1=Mx[:, c:c + 1], op=ALU.add), lres + [(Mx, c)], [mcur])
        Mres = [(Mx, c) for c in range(8)]
        A(lambda e: e.activation(out=f_r[:], in_=fpre[:], func=AF.Exp), [(fpre, c) for c in range(8)], [f_r])
        V(lambda e: e.tensor_tensor(out=u3, in0=u3, in1=Mx[:, 0:8].unsqueeze(2).to_broadcast([8, 8, 64]), op=ALU.subtract), [u_r] + Mres, [u_r])
        A(lambda e: e.activation(out=e_r[:], in_=u_r[:], func=AF.Exp), [u_r], [e_r])
        if own:
            V(lambda e: e.tensor_tensor(out=lf3, in0=lf3, in1=Mx[:, 0:8].unsqueeze(2).to_broadcast([8, 8, 64]), op=ALU.add), lres + Mres, lres)
            A(lambda e: e.activation(out=thr_r[:], in_=lf[:], func=AF.Exp, scale=-1.0), lres, [thr_r])
        for t in range(4):
            TE(lambda e, t=t: e.matmul(pss[:, 0:8], lhsT=e_r[:, t * 128:(t + 1) * 128], rhs=i8[:], start=True, stop=True), [e_r, i8], [pss])
            V(lambda e, t=t: e.tensor_copy(out=e_tok[:, t, :], in_=pss[:, 0:8]), [pss], [(e_tok, t)])
            A(lambda e, t=t: e.copy(out=e_bf[:, t, :], in_=pss[:, 0:8]), [pss], [(e_bf, t)])
            if own:
                TE(lambda e, t=t: e.matmul(pss[:, 8:16], lhsT=thr_r[:, t * 128:(t + 1) * 128], rhs=i8[:], start=True, stop=True), [thr_r, i8], [pss])
                V(lambda e, t=t: e.tensor_copy(out=thr_tok[:, t, :], in_=pss[:, 8:16]), [pss], [(thr_tok, t)])
        for h in range(8):
            TE(lambda e, h=h: e.matmul(pss[:, 32 + h * 8:40 + h * 8], lhsT=hsel[:, h, :], rhs=f_r[:], start=True, stop=True), [hsel, f_r], [pss])
        V(lambda e: e.tensor_copy(out=F_bc[:], in_=pss[:, 32:96].rearrange("p (h c) -> p h c", h=8)), [pss], [F_bc])
        for u in range(2):
            w = load_w(w_in, 1024 + u * 512)
            for i in range(4):
                pm = ps_mm[nxt("mm")]
                proj_tok(pm, 512, i, w, 0)
                A(lambda e, pm=pm, i=i, u=u: e.mul(out=k_tok[:, i, u * 512:(u + 1) * 512], in_=pm[:], mul=KS), [pm], [(k_tok, i, u)])
            if own:
                for hh in range(4):
                    pm = ps_mm[nxt("mm")]
                    proj_feat(pm, 128, w, hh * 128)
                    A(lambda e, pm=pm, u=u, hh=hh: e.mul(out=kT[:, 4 * u + hh, :], in_=pm[:], mul=KS), [pm], [(kT, 4 * u + hh)])
        if own:
            for u in range(2):
                w = load_w(w_in, u * 512)
                for hh in range(4):
                    pm = ps_mm[nxt("mm")]
                    proj_feat(pm, 128, w, hh * 128)
                    h = 4 * u + hh
                    A(lambda e, pm=pm, h=h: e.copy(out=qT[:, h, :], in_=pm[:]), [pm], [(qT, h)])
            qres = [(qT, h) for h in range(8)]
            q4 = lambda t_: t_[:].rearrange("p h (t a l) -> p h t a l", t=4, a=2)
            A(lambda e: e.copy(out=qA[:], in_=qT[:]), qres, [qA])
            G(lambda e: e.memset(q4(qA)[:, :, :, 1, :], 0.0), [qA], [qA])
            A(lambda e: e.copy(out=qB[:], in_=qT[:]), qres, [qB])
            G(lambda e: e.memset(q4(qB)[:, :, :, 0, :], 0.0), [qB], [qB])
        for u in range(4):
            w = load_w(w_in, 2048 + u * 512)
            for i in range(4):
                pm = ps_mm[nxt("mm")]
                proj_tok(pm, 512, i, w, 0)
                V(lambda e, pm=pm, i=i, u=u: e.tensor_copy(out=v_tok[:, i, u * 512:(u + 1) * 512], in_=pm[:]), [pm], [(v_tok, i, u)])
        if own:
            for u in range(4):
                w = load_w(w_in, 4096 + u * 512)
                for i in range(4):
                    pm = ps_mm[nxt("mm")]
                    proj_tok(pm, 512, i, w, 0)
                    A(lambda e, pm=pm, i=i, u=u: e.activation(out=o_sig[:, i, u * 512:(u + 1) * 512], in_=pm[:], func=AF.Sigmoid), [pm], [(o_sig, i, u)])
        def do_half(t, hg, half, ve_, own=own):
            hs = slice(4 * hg, 4 * hg + 4)
            c = 2 * t + half
            pl = slice(64 * half, 64 * half + 64)
            cb, nb = Cbf[half], nbf[half]
            V(lambda e, hs=hs, c=c: e.tensor_tensor(out=Cs[:, hs, :], in0=Cs[:, hs, :], in1=F_bc[:, hs, c:c + 1].to_broadcast([128, 4, 256]), op=ALU.mult),
              [(Cs, hg), F_bc], [(Cs, hg)])
            V(lambda e, hs=hs, c=c: e.tensor_tensor(out=ns[:, hs], in0=ns[:, hs], in1=F_bc[:, hs, c], op=ALU.mult), [(ns, hg), F_bc], [(ns, hg)])
            if own:
                A(lambda e, cb=cb, hs=hs: e.copy(out=cb[:], in_=Cs[:, hs, :]), [(Cs, hg)], [cb])
                A(lambda e, nb=nb, hs=hs: e.copy(out=nb[:], in_=ns[:, hs]), [(ns, hg)], [nb])
            for hh in range(4):
                h = 4 * hg + hh
                TE(lambda e, hh=hh, h=h, pl=pl, ve_=ve_: e.matmul(psC[:, hh, :], lhsT=k_tok[pl, t, h * 128:(h + 1) * 128], rhs=ve_[pl, hh, :], start=True, stop=True),
                   [(k_tok, t, h // 4), ve_], [psC])
                TE(lambda e, hh=hh, h=h, pl=pl: e.matmul(pss[:, 16 + hh:17 + hh], lhsT=k_tok[pl, t, h * 128:(h + 1) * 128], rhs=e_bf[pl, t, h:h + 1], start=True, stop=True),
                   [(k_tok, t, h // 4), (e_bf, t)], [pss])
            V(lambda e, hs=hs: e.tensor_tensor(out=Cs[:, hs, :], in0=Cs[:, hs, :], in1=psC[:], op=ALU.add), [(Cs, hg), psC], [(Cs, hg)])
            V(lambda e, hs=hs: e.tensor_tensor(out=ns[:, hs], in0=ns[:, hs], in1=pss[:, 16:20], op=ALU.add), [(ns, hg), pss], [(ns, hg)])

        def do_group(t, hg, own=own):
            tc_ = slice(t * 128, (t + 1) * 128)
            hs = slice(4 * hg, 4 * hg + 4)
            ve_ = ve[nxt("ve")]
            V(lambda e, ve_=ve_, t=t, hs=hs: e.tensor_tensor(out=ve_[:], in0=v_tok[:, t, hg * 1024:(hg + 1) * 1024].rearrange("p (h v) -> p h v", h=4),
                                                            in1=e_tok[:, t, hs].unsqueeze(2).to_broadcast([128, 4, 256]), op=ALU.mult),
              [(v_tok, t, 2 * hg), (v_tok, t, 2 * hg + 1), (e_tok, t)], [ve_])
            if own:
                for hh in range(4):
                    h = 4 * hg + hh
                    TE(lambda e, hh=hh, h=h: e.matmul(psN[:, hh, 0:128], lhsT=kT[:, h, tc_], rhs=qT[:, h, tc_], start=True, stop=True),
                       [(kT, h), (qT, h)], [psN])
                V(lambda e: e.tensor_tensor(out=Sm[:], in0=psN[:, :, 0:128], in1=bmask[:, :].unsqueeze(1).to_broadcast([128, 4, 128]), op=ALU.mult), [psN, bmask], [Sm])
            for half in range(2):
                do_half(t, hg, half, ve_)
            if own:
                for hh in range(4):
                    h = 4 * hg + hh
                    TE(lambda e, hh=hh, ve_=ve_: e.matmul(psN[:, hh, :], lhsT=Sm[:, hh, :], rhs=ve_[:, hh, :], start=True, stop=False), [Sm, ve_], [psN])
                    TE(lambda e, hh=hh, h=h: e.matmul(psN[:, hh, :], lhsT=qA[:, h, tc_], rhs=Cbf[0][:, hh, :], start=False, stop=False), [qA, Cbf[0]], [psN])
                    TE(lambda e, hh=hh, h=h: e.matmul(psN[:, hh, :], lhsT=qB[:, h, tc_], rhs=Cbf[1][:, hh, :], start=False, stop=True), [qB, Cbf[1]], [psN])
                    TE(lambda e, hh=hh, h=h: e.matmul(pss[:, 24 + hh:25 + hh], lhsT=Sm[:, hh, :], rhs=e_bf[:, t, h:h + 1], start=True, stop=False), [Sm, (e_bf, t)], [pss])
                    TE(lambda e, hh=hh, h=h: e.matmul(pss[:, 24 + hh:25 + hh], lhsT=qA[:, h, tc_], rhs=nbf[0][:, hh:hh + 1], start=False, stop=False), [qA, nbf[0]], [pss])
                    TE(lambda e, hh=hh, h=h: e.matmul(pss[:, 24 + hh:25 + hh], lhsT=qB[:, h, tc_], rhs=nbf[1][:, hh:hh + 1], start=False, stop=True), [qB, nbf[1]], [pss])
                A(lambda e: e.activation(out=dtmp[:], in_=pss[:, 24:28], func=AF.Abs), [pss], [dtmp])
                V(lambda e, hs=hs: e.tensor_tensor(out=dtmp[:], in0=dtmp[:], in1=thr_tok[:, t, hs], op=ALU.max), [dtmp, (thr_tok, t)], [dtmp])
                V(lambda e: e.reciprocal(out=rd[:], in_=dtmp[:]), [dtmp], [rd])
                V(lambda e, hs=hs: e.tensor_tensor(out=hsb3[:, hs, :], in0=psN[:], in1=rd[:, :].unsqueeze(2).to_broadcast([128, 4, 256]), op=ALU.mult),
                  [psN, rd], [hsb])

        def do_tile(t, own=own):
            for hg in range(2):
                do_group(t, hg)
            if own:
                hres = [hsb]
                V(lambda e: e.tensor_tensor(out=sq[:], in0=hsb[:], in1=hsb[:], op=ALU.mult), hres, [sq])
                V(lambda e: e.reduce_sum(out=st1[:], in_=sq3[:], axis=AX.X), [sq], [st1])
                A(lambda e: e.activation(out=st1[:], in_=st1[:], func=AF.Sqrt, bias=epsT[:], scale=1.0 / 256), [st1, epsT], [st1])
                V(lambda e: e.reciprocal(out=st2[:], in_=st1[:]), [st1], [st2])
                V(lambda e: e.tensor_tensor(out=hsb3[:], in0=hsb3[:], in1=st2[:, :].unsqueeze(2).to_broadcast([128, 8, 256]), op=ALU.mult), hres + [st2], hres)
                V(lambda e: e.tensor_tensor(out=hsb3[:], in0=hsb3[:], in1=hg_bc[:, :].unsqueeze(1).to_broadcast([128, 8, 256]), op=ALU.mult), hres + [hg_bc], hres)
                V(lambda e, t=t: e.tensor_tensor(out=gated[:], in0=hsb[:], in1=o_sig[:, t, :], op=ALU.mult),
                  hres + [(o_sig, t, u) for u in range(4)], [gated])
                tr16(gated, [gated], t)

        for t in range(4):
            do_tile(t)
        if own:
            for u in range(4):
                w = load_w(w_out, u * 512)
                for i in range(4):
                    pm = ps_mm[nxt("mm")]
                    proj_tok(pm, 512, i, w, 0)
                    k = nxt("ot")
                    xr, o = xres[k], ot[k]
                    rr = r0 + i * 128
                    P.add("sync", lambda e, xr=xr, rr=rr, u=u: e.dma_start(out=xr[:], in_=d["ho"][rr:rr + 128, u * 512:(u + 1) * 512]), writes=[xr], dma=True)
                    V(lambda e, pm=pm, xr=xr, o=o: e.tensor_tensor(out=o[:], in0=pm[:], in1=xr[:], op=ALU.add), [pm, xr], [o])
                    P.add("sync", lambda e, o=o, rr=rr, u=u: e.dma_start(out=d["out"][rr:rr + 128, u * 512:(u + 1) * 512], in_=o[:]),
                          reads=[o], writes=[("out", rr, u)], dma=True)


def ml_consts():
    import ml_dtypes
    c = {}
    c["ident"] = np.eye(128).astype(ml_dtypes.bfloat16)
    c["i8"] = np.eye(8).astype(np.float32)
    hs = np.zeros((8, 8, 128), np.float32)
    for h in range(8):
        hs[h, h, :] = 1.0
    c["hsel"] = hs
    s = np.arange(128)[:, None]
    l = np.arange(128)[None, :]
    c["bmask"] = ((s <= l) & (s // 64 == l // 64)).astype(ml_dtypes.bfloat16)
    return c


ML_SHAPES = {"hp": ([1024, D], F32), "ho": ([1024, D], F32), "gmix": ([D], F32), "hgain": ([256], F32), "bgate": ([16], F32),
             "w_in": ([D, 6160], F32), "w_out": ([D, D], F32), "ident": ([128, 128], BF16), "i8": ([8, 8], F32),
             "hsel": ([8, 8, 128], F32), "bmask": ([128, 128], BF16)}


def rb(g):
    i, p = g // 2, g % 2
    return (i // 2) * 4 + p * 2 + (i % 2)


def build_select(P, nc, sb, ps, hg, ho, pflag):
    fl = sb("fl", [128, 2], F32)
    P.add("sync", lambda e: e.dma_start(out=fl[:], in_=pflag), writes=[fl], dma=True)
    c0 = [sb("c0_%d" % i, [128, D], F32) for i in range(3)]
    c1 = [sb("c1_%d" % i, [128, D], F32) for i in range(3)]
    for j in range(8):
        a, b = c0[j % 3], c1[j % 3]
        P.add("sync", lambda e, a=a, j=j: e.dma_start(out=a[:], in_=hg[rb(j) * 128:(rb(j) + 1) * 128, :]), reads=[("hg", rb(j) // 4)], writes=[a], dma=True)
        P.add("sync", lambda e, b=b, j=j: e.dma_start(out=b[:], in_=hg[rb(8 + j) * 128:(rb(8 + j) + 1) * 128, :]), reads=[("hg", rb(8 + j) // 4)], writes=[b], dma=True)
        P.add("vector", lambda e, a=a: e.tensor_scalar(out=a[:], in0=a[:], scalar1=fl[:, 0:1], scalar2=None, op0=ALU.mult), reads=[a, fl], writes=[a])
        P.add("vector", lambda e, a=a, b=b: e.scalar_tensor_tensor(out=b[:], in0=b[:], scalar=fl[:, 1:2], in1=a[:], op0=ALU.mult, op1=ALU.add), reads=[a, b, fl], writes=[b])
        P.add("sync", lambda e, b=b, j=j: e.dma_start(out=ho[j * 128:(j + 1) * 128, :], in_=b[:]), reads=[b], writes=[("ho", j)], dma=True)


IN_SHAPES = {
    "xk": ([2048, D], F32), "xq": ([1024, D], F32), "pflag": ([128, 2], F32),
    "norm_mix": ([2, D], F32), "norm_ffn": ([2, D], F32),
    "att_w_in": ([D, 4176], F32), "att_q_gain": ([128], F32), "att_k_gain": ([128], F32), "att_w_out": ([D, D], F32),
    "ml_w_in": ([D, 6160], F32), "ml_b_gate": ([16], F32), "ml_h_gain": ([256], F32), "ml_w_out": ([D, D], F32),
    "ffn_w_up": ([2, D, DFF], F32), "ffn_w_down": ([2, DFF, D], F32),
    "cos_k": ([128, 16, 16], F32), "sin_k": ([128, 16, 16], F32), "cos_ki": ([128, 16, 8], F32), "sin_ki": ([128, 16, 8], F32),
    "cos_q": ([128, 8, 16], F32), "sin_q": ([128, 8, 16], F32), "cos_qi": ([128, 8, 8], F32), "sin_qi": ([128, 8, 8], F32),
    "cbias": ([128, 256], F32), "ident": ([128, 128], BF16), "i8": ([8, 8], F32), "hsel": ([8, 8, 128], F32), "bmask": ([128, 128], BF16),
}
PAIRS = [[0, 1], [2, 3], [4, 5], [6, 7]]


SCHED_ATT = True
SCHED_ML = True


def build_fused(use_cc=True):
    nc = bass.Bass("TRN2", target_bir_lowering=False)
    di = {k: nc.dram_tensor(k, s, dt, kind="ExternalInput").ap() for k, (s, dt) in IN_SHAPES.items()}
    out = nc.dram_tensor("out", [1024, D], F32, kind="ExternalOutput").ap()
    h_a = nc.dram_tensor("h_a_i", [1024, D], F32).ap()
    h_0 = nc.dram_tensor("h_0_i", [1024, D], F32).ap()
    hg = nc.dram_tensor("hg_i", [2048, D], F32).ap()
    hp = nc.dram_tensor("hp_i", [1024, D], F32).ap()
    ho = nc.dram_tensor("ho_i", [1024, D], F32).ap()
    h_m = nc.dram_tensor("h_m_i", [1024, D], F32).ap()
    with ExitStack() as st:
        P = Prog(nc, st, schedule=SCHED_ATT)
        ar = Arena(nc, st)
        d = {k: di[k] for k in ("xk", "xq", "cos_k", "sin_k", "cos_ki", "sin_ki", "cos_q", "sin_q", "cos_qi", "sin_qi", "cbias", "ident")}
        d.update({"gmix": di["norm_mix"][0], "gq": di["att_q_gain"], "gk": di["att_k_gain"], "w_in": di["att_w_in"], "w_out": di["att_w_out"], "out": h_a})
        build_att(P, nc, ar.sb, ar.ps, d)
        P.barrier(schedule=False)
        ar.reset()
        build_ffn(P, nc, ar.sb, ar.ps, h_a, di["norm_ffn"][0], di["ffn_w_up"][0], di["ffn_w_down"][0], h_0, di["ident"], 1024)
        if use_cc:
            for j in range(4):
                P.add("gpsimd", lambda e, j=j: e.collective_compute("AllGather", ALU.bypass, replica_groups=PAIRS,
                                                                    ins=[h_0[j * 256:(j + 1) * 256, :]], outs=[hg[j * 512:(j + 1) * 512, :]]),
                      reads=[("out", j * 256 + t * 128, db) for t in range(2) for db in range(4)], writes=[("hg", j)], cc=True)
        else:
            allout = [("out", t * 128, db) for t in range(8) for db in range(4)]
            P.add("sync", lambda e: e.dma_start(out=hg[0:1024, :], in_=h_0), reads=allout, writes=["hg"], dma=True)
            P.add("sync", lambda e: e.dma_start(out=hg[1024:2048, :], in_=h_0), reads=allout, writes=["hg2"], dma=True)
        P.barrier(schedule=False)
        ar.reset()
        build_select(P, nc, ar.sb, ar.ps, hg, ho, di["pflag"])
        P.barrier(schedule=SCHED_ML)
        ar.reset()
        d = {"hp_tile": (lambda t: hg[rb(t) * 128:(rb(t) + 1) * 128, :]), "pflag": di["pflag"], "ho": ho, "gmix": di["norm_mix"][1], "hgain": di["ml_h_gain"], "bgate": di["ml_b_gate"], "w_in": di["ml_w_in"],
             "w_out": di["ml_w_out"], "ident": di["ident"], "i8": di["i8"], "hsel": di["hsel"], "bmask": di["bmask"], "out": h_m}
        build_ml(P, nc, ar.sb, ar.ps, d)
        P.barrier(schedule=False)
        ar.reset()
        build_ffn(P, nc, ar.sb, ar.ps, h_m, di["norm_ffn"][1], di["ffn_w_up"][1], di["ffn_w_down"][1], out, di["ident"], 1024)
        P.wait_all_dma("sync")
        P.emit()
    return nc


def core_inputs(c, x, shared):
    b, p = c // 2, c % 2
    m = dict(shared)
    m["xk"] = x[b]
    m["xq"] = q_tiles(x[b], p)
    fl = np.zeros((128, 2), np.float32)
    fl[:, p] = 1.0
    m["pflag"] = fl
    m.update(att_consts(p))
    m.update(ml_consts())
    return m


_NC = {}


def kernel(x, norm_mix, norm_ffn, att_w_in, att_q_gain, att_k_gain, att_w_out,
           ml_w_in, ml_b_gate, ml_h_gain, ml_w_out, ffn_w_up, ffn_w_down):
    f32 = lambda a: np.ascontiguousarray(np.asarray(a, dtype=np.float32))
    x = f32(x)
    shared = {"norm_mix": f32(norm_mix), "norm_ffn": f32(norm_ffn), "att_w_in": f32(att_w_in)[0], "att_q_gain": f32(att_q_gain)[0],
              "att_k_gain": f32(att_k_gain)[0], "att_w_out": f32(att_w_out)[0], "ml_w_in": f32(ml_w_in)[0], "ml_b_gate": f32(ml_b_gate)[0],
              "ml_h_gain": f32(ml_h_gain)[0], "ml_w_out": f32(ml_w_out)[0], "ffn_w_up": f32(ffn_w_up), "ffn_w_down": f32(ffn_w_down)}
    if "nc" not in _NC:
        _NC["nc"] = build_fused()
    cores = list(range(8))
    in_maps = [core_inputs(c, x, shared) for c in cores]
    res = run_bass_kernel_spmd(_NC["nc"], in_maps, core_ids=cores)
    out = np.empty_like(x)
    for c in cores:
        b, p = c // 2, c % 2
        out[b, p * 1024:(p + 1) * 1024] = np.asarray(res.results[c]["out"])
    return out
```
